# Optimizing a Trainium2 kernel written in Bass

```python
import math
import jax, jax.numpy as jnp
from jax import lax
import numpy as np

D_MODEL = 1024
BATCH = 16
SEQ = 2048
DEPTH = 2

GRID_W = 64
CTX_LEN = 256

D_HY = D_MODEL // 4
D_HG = D_MODEL // 2
D_FN = D_MODEL // 4
D_MIX = D_HY + D_HG + D_FN

HY_ORDER = 2
HY_SHORT_CONV = 3
HY_BANDS = 16
HY_EMB_DIM = 1 + 2 * HY_BANDS
HY_FILTER_HIDDEN = 64
HY_DECAY_TARGET = 1e-2
HY_FAST_DECAY_PCT = 0.3
HY_SLOW_DECAY_PCT = 1.5
D_HY_PROJ = (HY_ORDER + 1) * D_HY

HG_HEAD_DIM = 128
HG_HEADS = D_HG // HG_HEAD_DIM
HG_CHUNK = 64
HG_PROJ = 5 * D_HG

FN_GROUPS = 4
FN_GROUP_DIM = D_FN // FN_GROUPS

D_PROJ = D_HY_PROJ + HG_PROJ + D_FN

N_EXPERTS = 16
N_GROUPS = 4
EXPERTS_PER_GROUP = N_EXPERTS // N_GROUPS
TOP_K = 2
D_EXPERT = 512

N_MOD = 6
RMS_EPS = 1e-6
POS_BASE = 10000.0

kernel_name = "hybrid_hyena_hgrn2_fnet_moe_dit"


def rmsnorm(x, g):
    xf = x.astype(jnp.float32)
    y = xf * lax.rsqrt(jnp.mean(xf * xf, axis=-1, keepdims=True) + RMS_EPS)
    return (y * g.astype(jnp.float32)).astype(x.dtype)


def modulate(h, shift, scale):
    return h * (1 + scale) + shift


def ada_modulation(cond, w, b):
    return jnp.split(jax.nn.silu(cond) @ w + b, N_MOD, axis=-1)


def grid_sincos(n_tok):
    rows = n_tok // GRID_W
    row = jnp.repeat(jnp.arange(rows, dtype=jnp.float32), GRID_W)
    col = jnp.tile(jnp.arange(GRID_W, dtype=jnp.float32), rows)
    quarter = D_MODEL // 4
    omega = 1.0 / (POS_BASE ** (jnp.arange(quarter, dtype=jnp.float32) / quarter))
    ar = row[:, None] * omega
    ac = col[:, None] * omega
    return jnp.concatenate([jnp.sin(ar), jnp.cos(ar), jnp.sin(ac), jnp.cos(ac)], axis=-1)


def short_conv3(u, w, b):
    up = jnp.pad(u, ((0, 0), (1, 1), (0, 0)))
    return up[:, :-2] * w[0] + up[:, 1:-1] * w[1] + up[:, 2:] * w[2] + b


def hyena_deltas():
    max_decay = math.log(HY_DECAY_TARGET) / HY_FAST_DECAY_PCT
    min_decay = math.log(HY_DECAY_TARGET) / HY_SLOW_DECAY_PCT
    return jnp.linspace(min_decay, max_decay, D_HY, dtype=jnp.float32)


def hyena_filters(L, w1, b1, w2, b2, w3, freq):
    f32 = jnp.float32
    t = jnp.linspace(0.0, 1.0, L, dtype=f32)[:, None]
    w = 2.0 * math.pi * jnp.arange(L, dtype=f32)[:, None] / L
    bands = jnp.linspace(1e-4, HY_BANDS - 1, HY_BANDS, dtype=f32)[None, :]
    z = jnp.concatenate([t, jnp.cos(bands * w), -jnp.sin(bands * w)], axis=-1)
    fr = freq.astype(f32)
    h = jnp.sin(fr * (z @ w1.astype(f32) + b1.astype(f32)))
    h = jnp.sin(fr * (h @ w2.astype(f32) + b2.astype(f32)))
    h = (h @ w3.astype(f32)).reshape(L, 2 * HY_ORDER, D_HY)
    decay = jnp.exp(-t[:, :, None] * jnp.abs(hyena_deltas()))
    return h * decay


def bidir_fftconv(u, h_fwd, h_bwd, skip):
    L, C = h_fwd.shape
    k = jnp.concatenate([h_fwd, jnp.zeros((1, C), jnp.float32), h_bwd[1:][::-1]], axis=0)
    uf = jnp.fft.rfft(u.astype(jnp.float32), n=2 * L, axis=1)
    kf = jnp.fft.rfft(k, n=2 * L, axis=0)
    y = jnp.fft.irfft(uf * kf[None], n=2 * L, axis=1)[:, :L]
    return (y + u.astype(jnp.float32) * skip.astype(jnp.float32)).astype(u.dtype)


def hyena_mixer(z, conv_w, conv_b, w1, b1, w2, b2, w3, freq, bias, norm_g):
    L = z.shape[1]
    u = short_conv3(z, conv_w, conv_b)
    v, x1, x2 = jnp.split(u, HY_ORDER + 1, axis=-1)
    h = hyena_filters(L, w1, b1, w2, b2, w3, freq)
    y = x1 * bidir_fftconv(v, h[:, 0], h[:, 1], bias[0])
    y = x2 * bidir_fftconv(y, h[:, 2], h[:, 3], bias[1])
    return rmsnorm(y, norm_g)


def fourier_mixer(z, w, b, norm_g):
    B, L, _ = z.shape
    zg = z.astype(jnp.float32).reshape(B, L, FN_GROUPS, FN_GROUP_DIM)
    y = jnp.fft.fftn(zg, axes=(1, 3), norm="ortho").real
    y = jnp.einsum("blgc,gcd->blgd", y, w.astype(jnp.float32)).reshape(B, L, D_FN) + b.astype(jnp.float32)
    return rmsnorm(y.astype(z.dtype), norm_g)


def hgrn_lower_bounds(raw):
    cs = jnp.cumsum(jax.nn.softmax(raw.astype(jnp.float32), axis=0), axis=0)
    return cs - cs[0:1]


def hgrn2_inputs(z, lb):
    B, L, _ = z.shape
    q, f_fwd, f_bwd, i, g = jnp.split(z.astype(jnp.float32), 5, axis=-1)
    heads = lambda t: t.reshape(B, L, HG_HEADS, HG_HEAD_DIM)
    q = heads(jax.nn.silu(q)) * (HG_HEAD_DIM ** -0.5)
    dirs = []
    for d, f_raw in enumerate((f_fwd, f_bwd)):
        lbd = lb[d]
        forget = lbd + (1.0 - lbd) * jax.nn.sigmoid(f_raw)
        dirs.append((heads(1.0 - forget), heads(jnp.log(forget))))
    return q, heads(i), g, dirs


def to_chunks(t):
    B, L, H, d = t.shape
    return t.reshape(B, L // HG_CHUNK, HG_CHUNK, H, d).transpose(1, 0, 3, 2, 4)


def from_chunks(t):
    n, B, H, C, d = t.shape
    return t.transpose(1, 0, 3, 2, 4).reshape(B, n * C, H, d)


def gla_chunk_scan(q, k, v, logf, s0):
    causal = jnp.tril(jnp.ones((HG_CHUNK, HG_CHUNK), dtype=bool))

    def step(s, inp):
        qc, kc, vc, gc = inp
        b = jnp.cumsum(gc, axis=2)
        o_inter = jnp.einsum("bhtd,bhdv->bhtv", qc * jnp.exp(b), s)
        diff = b[:, :, :, None, :] - b[:, :, None, :, :]
        decay = jnp.exp(jnp.where(causal[:, :, None], diff, -jnp.inf))
        scores = jnp.einsum("bhtd,bhsd,bhtsd->bhts", qc, kc, decay)
        o_intra = jnp.einsum("bhts,bhsv->bhtv", scores, vc)
        b_end = b[:, :, -1, :]
        s_new = jnp.exp(b_end)[..., None] * s + jnp.einsum(
            "bhsd,bhsv->bhdv", kc * jnp.exp(b_end[:, :, None, :] - b), vc)
        return s_new, o_inter + o_intra

    s_fin, o = lax.scan(step, s0, (to_chunks(q), to_chunks(k), to_chunks(v), to_chunks(logf)))
    return s_fin, from_chunks(o)


def hgrn2_bidir(q, v, dirs, s0_fwd, s0_bwd):
    (k_f, g_f), (k_b, g_b) = dirs
    s_f, o_f = gla_chunk_scan(q, k_f, v, g_f, s0_fwd)
    flip = lambda t: t[:, ::-1]
    s_b, o_b = gla_chunk_scan(flip(q), flip(k_b), flip(v), flip(g_b), s0_bwd)
    return s_f, s_b, o_f + flip(o_b)


def hgrn2_readout(o, g, norm_g):
    B, L, H, d = o.shape
    on = o * lax.rsqrt(jnp.mean(o * o, axis=-1, keepdims=True) + RMS_EPS)
    return on.reshape(B, L, D_HG) * norm_g.astype(jnp.float32) * jax.nn.silu(g)


def mixer_output(z, o_hg, g_hg, w_out, conv_w, conv_b, w1, b1, w2, b2, w3, freq,
                 hy_bias, hy_norm_g, hg_norm_g, fn_w, fn_b, fn_norm_g):
    y_hy = hyena_mixer(z[..., :D_HY_PROJ], conv_w, conv_b, w1, b1, w2, b2, w3, freq, hy_bias, hy_norm_g)
    y_hg = hgrn2_readout(o_hg, g_hg, hg_norm_g).astype(z.dtype)
    y_fn = fourier_mixer(z[..., D_HY_PROJ + HG_PROJ:], fn_w, fn_b, fn_norm_g)
    return jnp.concatenate([y_hy, y_hg, y_fn], axis=-1) @ w_out


def moe_ffn(h, router_w, router_b, w_gate, w_up, w_down):
    N = h.shape[0]
    probs = jax.nn.softmax(h.astype(jnp.float32) @ router_w.astype(jnp.float32), axis=-1)
    sel = probs + router_b.astype(jnp.float32)
    group_score = lax.top_k(sel.reshape(N, N_GROUPS, EXPERTS_PER_GROUP), TOP_K)[0].sum(-1)
    best_group = jnp.argmax(group_score, axis=-1)
    in_group = (jnp.arange(N_EXPERTS) // EXPERTS_PER_GROUP)[None, :] == best_group[:, None]
    _, idx = lax.top_k(jnp.where(in_group, sel, -jnp.inf), TOP_K)
    gp = jnp.take_along_axis(probs, idx, axis=-1)
    gp = gp / jnp.sum(gp, axis=-1, keepdims=True)
    combine = jnp.sum(jax.nn.one_hot(idx, N_EXPERTS, dtype=jnp.float32) * gp[..., None], axis=1)
    out = jnp.zeros(h.shape, jnp.float32)
    for e in range(N_EXPERTS):
        he = (jax.nn.silu(h @ w_gate[e]) * (h @ w_up[e])) @ w_down[e]
        out = out + combine[:, e:e + 1] * he.astype(jnp.float32)
    return out.astype(h.dtype)


def setup_inputs(seed: int = 0) -> dict:
    key = jax.random.key(seed)
    ks = jax.random.split(key, 32)
    f32 = jnp.float32
    nrm = lambda k, shape, s: s * jax.random.normal(k, shape, f32)
    D = D_MODEL
    return {
        "x": nrm(ks[0], (BATCH, SEQ, D), 1.0),
        "c": nrm(ks[1], (BATCH, D), 1.0),
        "ctx": nrm(ks[2], (BATCH, CTX_LEN, D), 1.0),
        "c_ctx": nrm(ks[3], (D,), 1.0),
        "ada_w": nrm(ks[4], (DEPTH, D, N_MOD * D), 0.5 * D ** -0.5),
        "ada_b": nrm(ks[5], (DEPTH, N_MOD * D), 0.02),
        "norm1_g": 1.0 + nrm(ks[6], (DEPTH, D), 0.1),
        "norm2_g": 1.0 + nrm(ks[7], (DEPTH, D), 0.1),
        "w_in": nrm(ks[8], (DEPTH, D, D_PROJ), D ** -0.5),
        "w_out": nrm(ks[9], (DEPTH, D_MIX, D), D_MIX ** -0.5),
        "hy_conv_w": nrm(ks[10], (DEPTH, HY_SHORT_CONV, D_HY_PROJ), 0.5),
        "hy_conv_b": nrm(ks[11], (DEPTH, D_HY_PROJ), 0.02),
        "hy_filt_w1": nrm(ks[12], (DEPTH, HY_EMB_DIM, HY_FILTER_HIDDEN), HY_EMB_DIM ** -0.5),
        "hy_filt_b1": nrm(ks[13], (DEPTH, HY_FILTER_HIDDEN), 0.02),
        "hy_filt_w2": nrm(ks[14], (DEPTH, HY_FILTER_HIDDEN, HY_FILTER_HIDDEN), HY_FILTER_HIDDEN ** -0.5),
        "hy_filt_b2": nrm(ks[15], (DEPTH, HY_FILTER_HIDDEN), 0.02),
        "hy_filt_w3": nrm(ks[16], (DEPTH, HY_FILTER_HIDDEN, 2 * HY_ORDER * D_HY), HY_FILTER_HIDDEN ** -0.5),
        "hy_filt_freq": 1.0 + nrm(ks[17], (DEPTH, HY_FILTER_HIDDEN), 0.1),
        "hy_bias": nrm(ks[18], (DEPTH, HY_ORDER, D_HY), 0.5),
        "hy_norm_g": 1.0 + nrm(ks[19], (DEPTH, D_HY), 0.1),
        "hg_lower_bounds": nrm(ks[20], (DEPTH, 2, D_HG), 1.0),
        "hg_norm_g": 1.0 + nrm(ks[21], (DEPTH, D_HG), 0.1),
        "fn_w": nrm(ks[22], (DEPTH, FN_GROUPS, FN_GROUP_DIM, FN_GROUP_DIM), FN_GROUP_DIM ** -0.5),
        "fn_b": nrm(ks[23], (DEPTH, D_FN), 0.02),
        "fn_norm_g": 1.0 + nrm(ks[24], (DEPTH, D_FN), 0.1),
        "router_w": nrm(ks[25], (D, N_EXPERTS), D ** -0.5),
        "router_b": nrm(ks[26], (N_EXPERTS,), 0.01),
        "moe_w_gate": nrm(ks[27], (DEPTH, N_EXPERTS, D, D_EXPERT), D ** -0.5),
        "moe_w_up": nrm(ks[28], (DEPTH, N_EXPERTS, D, D_EXPERT), D ** -0.5),
        "moe_w_down": nrm(ks[29], (DEPTH, N_EXPERTS, D_EXPERT, D), D_EXPERT ** -0.5),
        "final_norm_g": 1.0 + nrm(ks[30], (D,), 0.1),
    }


def reference(x, c, ctx, c_ctx, ada_w, ada_b, norm1_g, norm2_g, w_in, w_out, hy_conv_w, hy_conv_b,
              hy_filt_w1, hy_filt_b1, hy_filt_w2, hy_filt_b2, hy_filt_w3, hy_filt_freq, hy_bias, hy_norm_g,
              hg_lower_bounds, hg_norm_g, fn_w, fn_b, fn_norm_g, router_w, router_b,
              moe_w_gate, moe_w_up, moe_w_down, final_norm_g):
    B, L, D = x.shape
    n_ctx = ctx.shape[1]
    lower_bounds = hgrn_lower_bounds(hg_lower_bounds)
    x = x + grid_sincos(L).astype(x.dtype)[None]
    cond_x = c[:, None, :]
    cond_c = c_ctx[None, None, :]
    s_zero = jnp.zeros((B, HG_HEADS, HG_HEAD_DIM, HG_HEAD_DIM), jnp.float32)
    hg_lo, hg_hi = D_HY_PROJ, D_HY_PROJ + HG_PROJ
    for layer in range(DEPTH):
        last = layer == DEPTH - 1
        mod_x = ada_modulation(cond_x, ada_w[layer], ada_b[layer])
        mod_c = ada_modulation(cond_c, ada_w[layer], ada_b[layer])
        mix_params = (w_out[layer], hy_conv_w[layer], hy_conv_b[layer], hy_filt_w1[layer], hy_filt_b1[layer],
                      hy_filt_w2[layer], hy_filt_b2[layer], hy_filt_w3[layer], hy_filt_freq[layer],
                      hy_bias[layer], hy_norm_g[layer], hg_norm_g[layer], fn_w[layer], fn_b[layer], fn_norm_g[layer])
        hx = modulate(rmsnorm(x, norm1_g[layer]), mod_x[0], mod_x[1])
        hc = modulate(rmsnorm(ctx, norm1_g[layer]), mod_c[0], mod_c[1])
        zx = hx @ w_in[layer]
        zc = hc @ w_in[layer]
        q_c, v_c, g_c, dirs_c = hgrn2_inputs(zc[..., hg_lo:hg_hi], lower_bounds[layer])
        s_f, s_b, o_c = hgrn2_bidir(q_c, v_c, dirs_c, s_zero, s_zero)
        q_x, v_x, g_x, dirs_x = hgrn2_inputs(zx[..., hg_lo:hg_hi], lower_bounds[layer])
        _, _, o_x = hgrn2_bidir(q_x, v_x, dirs_x, s_f, s_b)
        x = x + mod_x[2] * mixer_output(zx, o_x, g_x, *mix_params)
        hx2 = modulate(rmsnorm(x, norm2_g[layer]), mod_x[3], mod_x[4])
        if last:
            ff_x = moe_ffn(hx2.reshape(B * L, D), router_w, router_b,
                           moe_w_gate[layer], moe_w_up[layer], moe_w_down[layer])
        else:
            ctx = ctx + mod_c[2] * mixer_output(zc, o_c, g_c, *mix_params)
            hc2 = modulate(rmsnorm(ctx, norm2_g[layer]), mod_c[3], mod_c[4])
            ff = moe_ffn(jnp.concatenate([hc2.reshape(B * n_ctx, D), hx2.reshape(B * L, D)], axis=0),
                         router_w, router_b, moe_w_gate[layer], moe_w_up[layer], moe_w_down[layer])
            ctx = ctx + mod_c[5] * ff[:B * n_ctx].reshape(B, n_ctx, D)
            ff_x = ff[B * n_ctx:]
        x = x + mod_x[5] * ff_x.reshape(B, L, D)
    return rmsnorm(x, final_norm_g)
```

```python
import math
from contextlib import ExitStack
import numpy as np
import ml_dtypes
import concourse.bass as bass
import concourse.mybir as mybir
from concourse.bass_utils import run_bass_kernel_spmd

F32 = mybir.dt.float32
BF16 = mybir.dt.bfloat16
ALU = mybir.AluOpType
AF = mybir.ActivationFunctionType
AX = mybir.AxisListType

ENGS = ("pe", "act", "dve", "pool", "sp")
ROT = 12000
NDMASEM = 64
RING = {'sp': (0, 40), 'pool': (40, 20), 'act': (60, 4), 'dve': (60, 4), 'pe': (60, 4)}


class Trk:
    __slots__ = ("lw", "rd")

    def __init__(self):
        self.lw = None
        self.rd = []


class Op:
    __slots__ = ("eng", "fn", "deps", "mile", "isdma", "ev", "semi")


class Sched:
    def __init__(self, nc):
        self.nc = nc
        self.ops = {e: [] for e in ENGS}
        self.ndma = 0
        self.dma_cnt = [0] * NDMASEM
        self.bar_deps = []
        self.dma_open = []
        self.all_dma = []
        self.ring_pos = {e: 0 for e in ENGS}
        self.sem_last = [None] * NDMASEM

    def op(self, eng, fn, reads=(), writes=(), dma=False):
        o = Op()
        o.eng = eng
        o.fn = fn
        o.isdma = dma
        o.mile = False
        o.ev = None
        deps = list(self.bar_deps)
        for t in reads:
            if t.lw is not None:
                deps.append(t.lw)
        for t in writes:
            if t.lw is not None:
                deps.append(t.lw)
            deps.extend(t.rd)
        seen = set()
        dd = []
        for d in deps:
            if id(d) in seen or d is o:
                continue
            seen.add(id(d))
            if (not d.isdma) and (not dma) and d.eng == "pe" and eng == "pe":
                continue
            d.mile = True
            dd.append(d)
        o.deps = dd
        for t in reads:
            if (not dma) and t.rd and (not t.rd[-1].isdma) and t.rd[-1].eng == eng:
                t.rd[-1] = o
            else:
                t.rd.append(o)
        for t in writes:
            t.lw = o
            t.rd = []
        if dma:
            base, cnt_ = RING[eng]
            j = base + self.ring_pos[eng] % cnt_
            self.ring_pos[eng] += 1
            self.ndma += 1
            prev = self.sem_last[j]
            if prev is not None and all(prev is not d_ for d_ in o.deps):
                o.deps.append(prev)
            self.sem_last[j] = o
            self.dma_cnt[j] += 1
            o.semi = j
            o.ev = ("d", j, 16 * self.dma_cnt[j])
            self.dma_open.append(o)
            self.all_dma.append(o)
        self.ops[eng].append(o)
        return o

    def barrier(self):
        deps = []
        for e in ENGS:
            for o in reversed(self.ops[e]):
                if not o.isdma:
                    o.mile = True
                    deps.append(o)
                    break
        best = {}
        for o in self.dma_open:
            if o.semi not in best or best[o.semi].ev[2] < o.ev[2]:
                best[o.semi] = o
        deps.extend(best.values())
        self.dma_open = []
        self.bar_deps = deps

    def emit(self, es):
        nc = self.nc
        nsem = {}
        for e in ENGS:
            c = 0
            for o in self.ops[e]:
                if o.isdma:
                    continue
                if o.mile:
                    c += 1
                    o.ev = (e, (c - 1) // ROT, (c - 1) % ROT + 1)
            nsem[e] = (c + ROT - 1) // ROT if c else 0
        sems = {}
        for e in ENGS:
            for i in range(nsem[e]):
                sems[(e, i)] = es.enter_context(nc.semaphore(f"s_{e}{i}"))
        for j in range(NDMASEM):
            sems[("d", j)] = es.enter_context(nc.semaphore(f"s_d{j}"))
        block = es.enter_context(nc.Block())
        handles = {"pe": block.tensor, "act": block.scalar, "dve": block.vector,
                   "pool": block.gpsimd, "sp": block.sync}
        stats = {}
        for e in ENGS:
            ops = self.ops[e]
            last_dma = None
            if e == "sp":
                best = {}
                for o in self.all_dma:
                    if o.semi not in best or best[o.semi].ev[2] < o.ev[2]:
                        best[o.semi] = o
                last_dma = list(best.values())

            def body(eng, ops=ops, e=e, last_dma=last_dma):
                known = {}
                nw = 0
                for o in ops:
                    for d in o.deps:
                        k = (d.ev[0], d.ev[1])
                        v = d.ev[2]
                        if known.get(k, 0) < v:
                            eng.wait_ge(sems[k], v)
                            known[k] = v
                            nw += 1
                    ins = o.fn(eng)
                    if o.isdma:
                        ins.then_inc(sems[("d", o.semi)], 16)
                    elif o.mile:
                        ins.then_inc(sems[(o.ev[0], o.ev[1])], 1)
                if last_dma is not None:
                    for d in last_dma:
                        k = (d.ev[0], d.ev[1])
                        if known.get(k, 0) < d.ev[2]:
                            eng.wait_ge(sems[k], d.ev[2])
                            known[k] = d.ev[2]
                stats[e] = (len(ops), nw)

            handles[e](body)
        return stats


class Arena:
    def __init__(self, nc, es, nelem_f32):
        self.t = es.enter_context(nc.sbuf_tensor("arena", [128, nelem_f32], F32))
        self.n = nelem_f32
        self.off = 0
        self.peak = 0

    def mark(self):
        return self.off

    def release(self, m):
        self.off = m

    def alloc(self, shape_free, dtype=F32):
        n = int(np.prod(shape_free))
        nf = (n + 1) // 2 if dtype == BF16 else n
        nf = (nf + 7) // 8 * 8
        assert self.off + nf <= self.n, f"arena overflow {self.off}+{nf}>{self.n}"
        ap = self.t[:, self.off:self.off + nf]
        self.off += nf
        self.peak = max(self.peak, self.off)
        if dtype == BF16:
            ap = ap.bitcast(BF16)[:, 0:n]
        else:
            ap = ap[:, 0:n]
        if len(shape_free) > 1:
            names = " ".join(f"a{i}" for i in range(len(shape_free)))
            kw = {f"a{i}": int(s) for i, s in enumerate(shape_free)}
            ap = ap.rearrange(f"p ({names}) -> p {names}", **kw)
        return ap


D = 1024
KC = 8
NEXP = 16
DEXP = 512
DPROJ = 3584
EPS = 1e-6
TWO_PI = 2.0 * math.pi


def host_consts(L, LC):
    bf = ml_dtypes.bfloat16
    c = {}
    rows = L // 64
    row = np.repeat(np.arange(rows, dtype=np.float32), 64)
    col = np.tile(np.arange(64, dtype=np.float32), rows)
    quarter = D // 4
    omega = (1.0 / (10000.0 ** (np.arange(quarter, dtype=np.float32) / quarter))).astype(np.float32)
    ar = row[:, None] * omega
    ac = col[:, None] * omega
    c["pos"] = np.concatenate([np.sin(ar), np.cos(ar), np.sin(ac), np.cos(ac)], axis=-1).astype(np.float32)
    c["identb"] = np.eye(128).astype(bf)
    c["identf"] = np.eye(128).astype(np.float32)
    s = np.arange(128)[:, None]
    t = np.arange(128)[None, :]
    same = (s // 32) == (t // 32)
    c["maskf"] = (same & (s <= t)).astype(np.float32)
    c["maskb"] = (same & (s >= t)).astype(np.float32)
    c["rowm"] = (np.arange(128)[:, None] // 32 == np.arange(4)[None, :]).astype(np.float32)
    deltas = np.abs(np.linspace(math.log(1e-2) / 1.5, math.log(1e-2) / 0.3, 256, dtype=np.float32))
    m = np.arange(64)
    c64 = np.cos(2 * np.pi * np.outer(m, m) / 64) / 8.0
    s64 = -np.sin(2 * np.pi * np.outer(m, m) / 64) / 8.0
    z = np.zeros((64, 64))
    c["bdc"] = np.block([[c64, z], [z, c64]]).astype(np.float32)
    c["bds"] = np.block([[s64, z], [z, s64]]).astype(np.float32)
    for tag, n in (("l", L), ("c", LC)):
        nt = n // 128
        tt = np.linspace(0.0, 1.0, n, dtype=np.float32)[:, None]
        w = (2.0 * math.pi * np.arange(n, dtype=np.float32)[:, None] / n).astype(np.float32)
        bands = np.linspace(1e-4, 15, 16, dtype=np.float32)[None, :]
        zp = np.concatenate([tt, np.cos(bands * w), -np.sin(bands * w)], axis=-1).astype(np.float32)
        c["zpos" + tag] = np.ascontiguousarray(zp.T)
        dec = np.exp(-tt * deltas[None, :]).astype(np.float32)
        dec0 = dec.copy()
        dec0[0, :] = 0.0
        c["dec" + tag] = np.ascontiguousarray(np.concatenate([dec, dec0, dec, dec0], axis=1))
        k = np.arange(n, dtype=np.float64)
        tq = np.arange(n, dtype=np.float64)
        ang = 2 * np.pi * np.outer(tq, k + 0.5) / (2 * n)
        Fc = np.cos(ang)
        Fs = np.sin(ang)

        def tile_fwd(M):
            return M.reshape(nt, 128, nt, 128).transpose(2, 1, 0, 3)

        hyf = np.stack([tile_fwd(Fc), tile_fwd(Fs)], axis=2)
        c["hyf" + tag] = np.ascontiguousarray(hyf).astype(bf)
        Ic = (Fc.T / n)
        Is = (-Fs.T / n)
        hyi = np.stack([tile_fwd(Ic), tile_fwd(Is)], axis=2)
        c["hyi" + tag] = np.ascontiguousarray(hyi).astype(bf)
        ang2 = 2 * np.pi * np.outer(tq, k) / n
        fnf = np.stack([tile_fwd(np.cos(ang2) / math.sqrt(n)), tile_fwd(np.sin(ang2) / math.sqrt(n))], axis=2)
        c["fnf" + tag] = np.ascontiguousarray(fnf).astype(bf)
    return c


WEIGHT_SHAPES = {
    "ada_w": (2, D, 6 * D), "ada_b": (2, 6 * D), "norm1_g": (2, D), "norm2_g": (2, D),
    "w_in": (2, D, DPROJ), "w_out": (2, D, D), "hy_conv_w": (2, 3, 768), "hy_conv_b": (2, 768),
    "hy_filt_w1": (2, 33, 64), "hy_filt_b1": (2, 64), "hy_filt_w2": (2, 64, 64), "hy_filt_b2": (2, 64),
    "hy_filt_w3": (2, 64, 1024), "hy_filt_freq": (2, 64), "hy_bias": (2, 2, 256), "hy_norm_g": (2, 256),
    "hg_lower_bounds": (2, 2, 512), "hg_norm_g": (2, 512), "fn_w": (2, 4, 64, 64), "fn_b": (2, 256),
    "fn_norm_g": (2, 256), "router_w": (D, NEXP), "router_b": (NEXP,),
    "moe_w_gate": (2, NEXP, D, DEXP), "moe_w_up": (2, NEXP, D, DEXP), "moe_w_down": (2, NEXP, DEXP, D),
    "final_norm_g": (D,),
}


DEBUG = False
STOP_AFTER = None
HALF_EXP = False


def build(NB, L, LC, NL=2, arena_elems=53000):
    T = LC + L
    NT = T // 128
    NTC = LC // 128
    NTL = L // 128
    NCH = T // 32
    NCHC = LC // 32
    hc = host_consts(L, LC)
    nc = bass.Bass("TRN2", target_bir_lowering=False)

    def din(name, shape, dt=F32):
        return nc.dram_tensor(name, list(shape), dt, kind="ExternalInput").ap()

    def dscr(name, shape, dt=F32):
        return nc.dram_tensor(name, list(shape), dt, kind="Internal").ap()

    x_d = din("x", [NB, L, D])
    c_d = din("c", [NB, D])
    ctx_d = din("ctx", [NB, LC, D])
    cctx_d = din("c_ctx", [D])
    W = {k: din(k, v) for k, v in WEIGHT_SHAPES.items()}
    C = {k: din("k_" + k, v.shape, BF16 if v.dtype == ml_dtypes.bfloat16 else F32) for k, v in hc.items()}
    out_d = nc.dram_tensor("out", [NB, L, D], F32, kind="ExternalOutput").ap()

    xres = nc.dram_tensor("xres", [NB, T, D], F32, kind="ExternalOutput").ap() if DEBUG else dscr("xres", [NB, T, D])
    gate_d = dscr("gate_rows", [NL, 3, 2, D])
    kk_d = {"l": dscr("kk_l", [NL, NTL, 128, 2, 2, 256], BF16), "c": dscr("kk_c", [NL, NTC, 128, 2, 2, 256], BF16)}
    yT_d = nc.dram_tensor("yT", [128, 8, T], BF16, kind="ExternalOutput").ap() if DEBUG else dscr("yT", [128, 8, T], BF16)

    if DEBUG:
        dbg_acc = nc.dram_tensor("dbg_acc", [128, 16, D], F32, kind="ExternalOutput").ap()
        dbg_comb = nc.dram_tensor("dbg_comb", [128, 16, NEXP], F32, kind="ExternalOutput").ap()
        dbg_h = nc.dram_tensor("dbg_h", [128, KC, 256], BF16, kind="ExternalOutput").ap()
    es = ExitStack()
    S = Sched(nc)
    A = Arena(nc, es, arena_elems)
    PS = [es.enter_context(nc.psum_tensor(f"ps{i}", [128, 512], F32)) for i in range(8)]
    PT = [Trk() for _ in range(8)]

    def MM(out, lhsT, rhs, st, sp, R, Wt):
        S.op("pe", lambda e: e.matmul(out, lhsT=lhsT, rhs=rhs, start=st, stop=sp), reads=R, writes=Wt)

    def TRP(out, in_, ident, R, Wt):
        S.op("pe", lambda e: e.transpose(out=out, in_=in_, identity=ident), reads=R, writes=Wt)

    def ACT(out, in_, func, R, Wt, scale=1.0, bias=0.0, accum=None):
        if accum is None:
            S.op("act", lambda e: e.activation(out=out, in_=in_, func=func, scale=scale, bias=bias), reads=R, writes=Wt)
        else:
            S.op("act", lambda e: e.activation(out=out, in_=in_, func=func, scale=scale, bias=bias, accum_out=accum),
                 reads=R, writes=Wt)

    def TT(eng, out, a, b, op, R, Wt):
        S.op(eng, lambda e: e.tensor_tensor(out=out, in0=a, in1=b, op=op), reads=R, writes=Wt)

    def TS(eng, out, a, s1, s2, op0, op1, R, Wt):
        if s2 is None:
            S.op(eng, lambda e: e.tensor_scalar(out=out, in0=a, scalar1=s1, scalar2=None, op0=op0), reads=R, writes=Wt)
        else:
            S.op(eng, lambda e: e.tensor_scalar(out=out, in0=a, scalar1=s1, scalar2=s2, op0=op0, op1=op1),
                 reads=R, writes=Wt)

    def STT(eng, out, a, sc, b, op0, op1, R, Wt):
        S.op(eng, lambda e: e.scalar_tensor_tensor(out=out, in0=a, scalar=sc, in1=b, op0=op0, op1=op1),
             reads=R, writes=Wt)

    def CP(eng, out, in_, R, Wt):
        if eng == "act":
            S.op("act", lambda e: e.copy(out=out, in_=in_), reads=R, writes=Wt)
        else:
            S.op(eng, lambda e: e.tensor_copy(out=out, in_=in_), reads=R, writes=Wt)

    def MSET(eng, ap, val, Wt):
        S.op(eng, lambda e: e.memset(ap, val), writes=Wt)

    def RED(out, in_, op, R, Wt):
        S.op("dve", lambda e: e.tensor_reduce(out=out, in_=in_, axis=AX.X, op=op), reads=R, writes=Wt)

    def SCAN(out, d0, d1, R, Wt):
        S.op("dve", lambda e: e.tensor_tensor_scan(out=out, data0=d0, data1=d1, initial=0.0, op0=ALU.mult, op1=ALU.add),
             reads=R, writes=Wt)

    def RCP(out, in_, R, Wt):
        S.op("dve", lambda e: e.reciprocal(out=out, in_=in_), reads=R, writes=Wt)

    def DMA(q, out, in_, R, Wt, slow=False):
        if slow:
            S.op(q, lambda e: e.dma_start(out=out, in_=in_, allow_slow_non_contiguous=True), reads=R, writes=Wt, dma=True)
        else:
            S.op(q, lambda e: e.dma_start(out=out, in_=in_), reads=R, writes=Wt, dma=True)

    rr = {"ps": 0}

    def evac_eng(i):
        return "act" if i % 2 == 0 else "dve"

    identb = A.alloc([128], BF16); t_identb = Trk()
    identf = A.alloc([128]); t_identf = Trk()
    maskf = A.alloc([128]); maskb = A.alloc([128]); t_mask = Trk()
    rowm = A.alloc([4])
    scanm = A.alloc([T]); t_scanm = Trk()
    scT = A.alloc([KC, 3], BF16); t_scT = Trk()
    stg = A.alloc([128]); t_stg = Trk()
    vecT = A.alloc([128]); t_vec = Trk()
    lbT = A.alloc([3, 8]); t_lb = Trk()
    modT = A.alloc([48, 3]); t_mod = Trk()
    GS = A.alloc([4, KC, 3]); t_GS = Trk()
    filtp = A.alloc([4]); t_filtp = Trk()
    bda = A.alloc([2, 128], BF16); bdb = A.alloc([2, 128], BF16); t_bd = Trk()
    rw_sb = A.alloc([KC, NEXP], BF16); t_rw = Trk()
    rb_sb = A.alloc([NEXP]); t_rb = Trk()
    fng_sb = A.alloc([D]); t_fng = Trk()
    small = A.alloc([64]); t_small = Trk()

    DMA("sp", identb, C["identb"], [], [t_identb])
    DMA("sp", identf, C["identf"], [], [t_identf])
    DMA("sp", maskf, C["maskf"], [], [t_mask])
    DMA("sp", maskb, C["maskb"], [], [t_mask])
    DMA("sp", rowm, C["rowm"], [], [t_mask])
    MSET("pool", scanm, 1.0, [t_scanm])
    MSET("pool", scanm.rearrange("p (c j) -> p c j", j=32)[:, :, 0:1], 0.0, [t_scanm])
    MSET("pool", stg, 0.0, [t_stg])
    DMA("pool", rw_sb, W["router_w"].rearrange("(kc p) e -> p kc e", p=128), [], [t_rw])
    DMA("sp", rb_sb, W["router_b"].partition_broadcast(128), [], [t_rb])
    DMA("sp", fng_sb, W["final_norm_g"].partition_broadcast(128), [], [t_fng])
    m0 = A.mark()
    cf = A.alloc([KC, 3]); t_cf = Trk()
    for r in range(3):
        src = c_d[r] if r < NB else cctx_d
        DMA("sp", cf[:, :, r], src.rearrange("(kc p) -> p kc", p=128), [], [t_cf], slow=True)
    ACT(scT, cf, AF.Silu, [t_cf], [t_scT])
    S.barrier()
    A.release(m0)
    PERSIST = A.mark()

    def cond_row(b, is_ctx):
        return 2 if is_ctx else b

    def layer_setup(l):
        m0 = A.mark()
        def ld(r0, nr, src):
            DMA("sp", stg[r0:r0 + nr, :], src, [], [t_stg])
        ld(0, 8, W["norm1_g"][l].rearrange("(r c) -> r c", c=128))
        ld(8, 8, W["norm2_g"][l].rearrange("(r c) -> r c", c=128))
        ld(16, 18, W["hy_conv_w"][l].rearrange("k (j c) -> (k j) c", c=128))
        ld(34, 6, W["hy_conv_b"][l].rearrange("(r c) -> r c", c=128))
        ld(40, 4, W["hg_norm_g"][l].rearrange("(r c) -> r c", c=128))
        ld(44, 48, W["ada_b"][l].rearrange("(r c) -> r c", c=128))
        ld(92, 2, W["hy_norm_g"][l].rearrange("(r c) -> r c", c=128))
        ld(94, 2, W["fn_norm_g"][l].rearrange("(r c) -> r c", c=128))
        ld(96, 8, W["hg_lower_bounds"][0].rearrange("d (h c) -> (d h) c", c=128))
        ld(104, 8, W["hg_lower_bounds"][1].rearrange("d (h c) -> (d h) c", c=128))
        TRP(PS[0][:, 0:128], stg, identf, [t_stg, t_identf], [PT[0]])
        CP("dve", vecT, PS[0][:, 0:128], [PT[0]], [t_vec])
        if l == 0:
            MSET("pool", lbT[:, 0, :], 0.0, [t_lb])
        else:
            TT("dve", small[:, 0:8], vecT[:, 104:112], vecT[:, 96:104], ALU.subtract, [t_vec], [t_small])
            ACT(lbT[:, 0, :], small[:, 0:8], AF.Sigmoid, [t_small], [t_lb])
        TS("dve", lbT[:, 1, :], lbT[:, 0, :], -1.0, 1.0, ALU.mult, ALU.add, [t_lb], [t_lb])
        TS("dve", lbT[:, 2, :], lbT[:, 1, :], -1.0, None, ALU.mult, None, [t_lb], [t_lb])
        m_mod = A.mark()
        screp = A.alloc([KC, 3, 128], BF16); t_screp = Trk()
        CP("dve", screp, scT.unsqueeze(3).to_broadcast([128, KC, 3, 128]), [t_scT], [t_screp])
        wa = [A.alloc([KC, 768], BF16) for _ in range(2)]
        t_wa = [Trk(), Trk()]
        for gq in range(8):
            bi = gq % 2
            DMA("pool", wa[bi], W["ada_w"][l, :, gq * 768:(gq + 1) * 768].rearrange("(kc p) n -> p kc n", p=128), [], [t_wa[bi]])
            for o6 in range(6):
                oc = gq * 6 + o6
                for kc in range(KC):
                    MM(PS[1][:, oc * 3:(oc + 1) * 3], wa[bi][:, kc, o6 * 128:(o6 + 1) * 128], scT[:, kc, :], kc == 0, kc == KC - 1,
                       [t_wa[bi], t_scT], [PT[1]])
        TT("dve", modT, PS[1][:, 0:144].rearrange("p (o r) -> p o r", r=3),
           vecT[:, 44:92].unsqueeze(2).to_broadcast([128, 48, 3]), ALU.add, [PT[1], t_vec], [t_mod])
        for which, (gsl, m_shift, m_scale) in enumerate(((slice(0, 8), 0, 1), (slice(8, 16), 3, 4))):
            gi = 2 * which
            TS("dve", GS[:, gi], modT[:, m_scale * 8:(m_scale + 1) * 8, :], 1.0, None, ALU.add, None, [t_mod], [t_GS])
            TT("dve", GS[:, gi], GS[:, gi], vecT[:, gsl].unsqueeze(2).to_broadcast([128, 8, 3]), ALU.mult, [t_GS, t_vec], [t_GS])
            CP("dve", GS[:, gi + 1], modT[:, m_shift * 8:(m_shift + 1) * 8, :], [t_mod], [t_GS])
        wg = A.alloc([KC, 2, D], BF16); t_wg = Trk()
        for g, m in enumerate((2, 5)):
            for kc in range(KC):
                DMA("pool", wg[:, kc, g, :], W["ada_w"][l, kc * 128:(kc + 1) * 128, m * D:(m + 1) * D], [], [t_wg])
        gb = A.alloc([2, D]); t_gb = Trk()
        for g, m in enumerate((2, 5)):
            DMA("sp", gb[:, g, :], W["ada_b"][l, m * D:(m + 1) * D].partition_broadcast(128), [], [t_gb])
        grow = A.alloc([D]); t_grow = Trk()
        for r in range(3):
            for g in range(2):
                for hh in range(2):
                    pb = 2 + hh
                    for kc in range(KC):
                        MM(PS[pb][:, :], screp[:, kc, r, :], wg[:, kc, g, hh * 512:(hh + 1) * 512], kc == 0, kc == KC - 1,
                           [t_screp, t_wg], [PT[pb]])
                    TT("dve", grow[:, hh * 512:(hh + 1) * 512], PS[pb][:, :], gb[:, g, hh * 512:(hh + 1) * 512], ALU.add,
                       [PT[pb], t_gb], [t_grow])
                DMA("sp", gate_d[l, r, g:g + 1, :], grow[0:1, :], [t_grow], [])
        S.barrier()
        A.release(m_mod)
        DMA("sp", filtp[0:64, 0:1], W["hy_filt_freq"][l].rearrange("(p o) -> p o", o=1), [], [t_filtp])
        DMA("sp", filtp[0:64, 1:2], W["hy_filt_b1"][l].rearrange("(p o) -> p o", o=1), [], [t_filtp])
        DMA("sp", filtp[0:64, 2:3], W["hy_filt_b2"][l].rearrange("(p o) -> p o", o=1), [], [t_filtp])
        w1 = A.alloc([64]); w2 = A.alloc([64]); w3 = A.alloc([1024]); t_fw = Trk()
        DMA("sp", w1[0:33, :], W["hy_filt_w1"][l], [], [t_fw])
        DMA("sp", w2[0:64, :], W["hy_filt_w2"][l], [], [t_fw])
        DMA("sp", w3[0:64, :], W["hy_filt_w3"][l], [], [t_fw])
        biasz = A.alloc([1024]); t_bz = Trk()
        MSET("pool", biasz, 0.0, [t_bz])
        for o in range(2):
            DMA("sp", biasz[0:1, (2 * o) * 256:(2 * o) * 256 + 256], W["hy_bias"][l, o:o + 1, :], [], [t_bz])
        variants = [("l", L, NTL)] + ([("c", LC, NTC)] if l == 0 else [])
        for tag, n, nt in variants:
            m1 = A.mark()
            zp = A.alloc([n]); t_zp = Trk()
            DMA("sp", zp[0:33, :], C["zpos" + tag], [], [t_zp])
            h1 = A.alloc([n]); h2 = A.alloc([n]); t_h1 = Trk(); t_h2 = Trk()
            targ = A.alloc([512]); t_targ = Trk()
            tsn = A.alloc([512]); tcs = A.alloc([512]); tq = A.alloc([512]); t_tsn = Trk()
            fpi = A.alloc([8]); MSET("pool", fpi, math.pi / 2, [t_tsn])
            nblk = (n + 511) // 512
            for stage in range(2):
                src, t_src, dst, t_dst, wmat, kk_, bcol = ((zp, t_zp, h1, t_h1, w1, 33, 1), (h1, t_h1, h2, t_h2, w2, 64, 2))[stage]
                for bk in range(nblk):
                    c0 = bk * 512
                    cw = min(512, n - c0)
                    pb = 4 + bk % 2
                    MM(PS[pb][0:64, 0:cw], wmat[0:kk_, :], src[0:kk_, c0:c0 + cw], True, True, [t_fw, t_src], [PT[pb]])
                    TS("dve", targ[0:64, 0:cw], PS[pb][0:64, 0:cw], filtp[0:64, bcol:bcol + 1], filtp[0:64, 0:1], ALU.add, ALU.mult,
                       [PT[pb], t_filtp], [t_targ])
                    a_ = targ[0:64, 0:cw]
                    s_ = tsn[0:64, 0:cw]; c_ = tcs[0:64, 0:cw]; q_ = tq[0:64, 0:cw]
                    ACT(s_, a_, AF.Sin, [t_targ], [t_tsn], scale=0.125)
                    ACT(c_, a_, AF.Sin, [t_targ], [t_tsn], scale=0.125, bias=fpi[0:64, 0:1])
                    for rep in range(3):
                        STT("dve", q_, s_, 2.0, c_, ALU.mult, ALU.mult, [t_tsn], [t_tsn])
                        TT("dve", c_, s_, s_, ALU.mult, [t_tsn], [t_tsn])
                        TS("dve", c_, c_, -2.0, 1.0, ALU.mult, ALU.add, [t_tsn], [t_tsn])
                        if rep < 2:
                            CP("dve", s_, q_, [t_tsn], [t_tsn])
                    CP("dve", dst[0:64, c0:c0 + cw], q_, [t_tsn], [t_dst])
            HS = A.alloc([nt, 512], BF16); HD = A.alloc([nt, 512], BF16); t_HS = Trk()
            dcy = [A.alloc([1024]) for _ in range(2)]; t_dcy = [Trk(), Trk()]
            Hd = A.alloc([1024]); t_Hd = Trk()
            for jc in range(nt):
                bi = jc % 2
                DMA("sp", dcy[bi], C["dec" + tag][jc * 128:(jc + 1) * 128, :], [], [t_dcy[bi]])
                for hh in range(2):
                    pb = 4 + hh
                    MM(PS[pb][:, :], h2[0:64, jc * 128:(jc + 1) * 128], w3[0:64, hh * 512:(hh + 1) * 512], True, True,
                       [t_h2, t_fw], [PT[pb]])
                    TT("dve", Hd[:, hh * 512:(hh + 1) * 512], PS[pb][:, :], dcy[bi][:, hh * 512:(hh + 1) * 512], ALU.mult,
                       [PT[pb], t_dcy[bi]], [t_Hd])
                if jc == 0:
                    TT("dve", Hd, Hd, biasz, ALU.add, [t_Hd, t_bz], [t_Hd])
                Hv = Hd.rearrange("p (o d c) -> p o d c", o=2, d=2)
                TT("dve", HS[:, jc, :].rearrange("p (o c) -> p o c", o=2), Hv[:, :, 0, :], Hv[:, :, 1, :], ALU.add, [t_Hd], [t_HS])
                TT("dve", HD[:, jc, :].rearrange("p (o c) -> p o c", o=2), Hv[:, :, 1, :], Hv[:, :, 0, :], ALU.subtract, [t_Hd], [t_HS])
            FB = [A.alloc([2, nt, 128], BF16) for _ in range(2)]; t_FB = [Trk(), Trk()]
            KKs = [A.alloc([2, 512], BF16) for _ in range(2)]; t_KK = [Trk(), Trk()]
            for kc in range(nt):
                bi = kc % 2
                DMA("sp", FB[bi], C["hyf" + tag][kc], [], [t_FB[bi]])
                for ri, Hx in enumerate((HS, HD)):
                    pb = 6 + ri
                    for jc in range(nt):
                        MM(PS[pb][:, :], FB[bi][:, ri, jc, :], Hx[:, jc, :], jc == 0, jc == nt - 1, [t_FB[bi], t_HS], [PT[pb]])
                    CP("act" if ri == 0 else "dve", KKs[bi][:, ri, :], PS[pb][:, :], [PT[pb]], [t_KK[bi]])
                DMA("sp", kk_d[tag][l, kc].rearrange("p r o c -> p r (o c)"), KKs[bi], [t_KK[bi]], [])
            S.barrier()
            A.release(m1)
        wst = A.alloc([2, 64]); t_wst = Trk()
        DMA("sp", wst, W["fn_w"][l].rearrange("(gp g) m d -> (g m) gp d", g=2), [], [t_wst])
        bdc = A.alloc([128]); bds = A.alloc([128]); t_bdc = Trk()
        DMA("sp", bdc, C["bdc"], [], [t_bdc])
        DMA("sp", bds, C["bds"], [], [t_bdc])
        MSET("pool", bda, 0.0, [t_bd])
        MSET("pool", bdb, 0.0, [t_bd])
        for which, (mat, dst) in enumerate(((bdc, bda), (bds, bdb))):
            for gp in range(2):
                pb = 4 + gp
                MM(PS[pb][:, 0:64], mat, wst[:, gp, :], True, True, [t_bdc, t_wst], [PT[pb]])
                CP("dve", dst[0:64, gp, 0:64], PS[pb][0:64, 0:64], [PT[pb]], [t_bd])
                CP("dve", dst[64:128, gp, 64:128], PS[pb][64:128, 0:64], [PT[pb]], [t_bd])
        S.barrier()
        A.release(m0)

    def rms_rstd(ss_ap, n, R, out_ap, t_out):
        ACT(out_ap, ss_ap, AF.Sqrt, R, [t_out], scale=1.0 / n, bias=EPS)
        RCP(out_ap, out_ap, [t_out], [t_out])

    def norm_mod_transpose(xt, t_xt, hT, t_hT, col0, gi, r, bufs, i):
        junk, t_junk, ssb, t_ss, xn, t_xn = bufs
        ACT(junk, xt, AF.Square, [t_xt], [t_junk, t_ss], accum=ssb[:, 0:1])
        rms_rstd(ssb[:, 0:1], D, [t_ss], ssb[:, 1:2], t_ss)
        TS("dve", xn, xt, ssb[:, 1:2], None, ALU.mult, None, [t_xt, t_ss], [t_xn])
        pb = i % 2
        pv = PS[pb][:, :].bitcast(BF16)
        for kc in range(KC):
            TRP(pv[:, kc * 128:(kc + 1) * 128], xn[:, kc * 128:(kc + 1) * 128], identb, [t_xn, t_identb], [PT[pb]])
        for kc in range(KC):
            if kc % 2 == 0:
                ACT(hT[:, kc, col0:col0 + 128], pv[:, kc * 128:(kc + 1) * 128], AF.Identity, [PT[pb], t_GS], [t_hT],
                    scale=GS[:, gi, kc, r:r + 1], bias=GS[:, gi + 1, kc, r:r + 1])
            else:
                TS("dve", hT[:, kc, col0:col0 + 128], pv[:, kc * 128:(kc + 1) * 128], GS[:, gi, kc, r:r + 1],
                   GS[:, gi + 1, kc, r:r + 1], ALU.mult, ALU.add, [PT[pb], t_GS], [t_hT])

    def col_groups(c0, c1):
        g = []
        c = c0
        while c < c1:
            w = min(512, c1 - c)
            g.append((c, w))
            c += w
        return g

    def win_chunks(l, hT, t_hT, cols_list, evac, c0=0, c1=None):
        c1 = T if c1 is None else c1
        n = len(cols_list)
        wb = A.alloc([KC, n, 128], BF16); t_wb = Trk()
        for j, cc in enumerate(cols_list):
            DMA("pool", wb[:, :, j, :], W["w_in"][l, :, cc:cc + 128].rearrange("(kc p) n -> p kc n", p=128), [], [t_wb])
        for j in range(n):
            for (g0, gw) in col_groups(c0, c1):
                pb = rr["ps"] % 4
                rr["ps"] += 1
                for kc in range(KC):
                    MM(PS[pb][:, 0:gw], wb[:, kc, j, :], hT[:, kc, g0:g0 + gw], kc == 0, kc == KC - 1, [t_wb, t_hT], [PT[pb]])
                evac(j, g0, gw, PS[pb][:, 0:gw], PT[pb])

    def mixer_sublayer(l, b, hT, t_hT):
        last = (l == NL - 1)
        m0 = A.mark()
        xt = [A.alloc([D]) for _ in range(2)]; t_xt = [Trk(), Trk()]
        pt_ = [A.alloc([D]) for _ in range(2)]; t_pt = [Trk(), Trk()]
        junk = [A.alloc([D], BF16) for _ in range(2)]; t_junk = [Trk(), Trk()]
        ssb = [A.alloc([8]) for _ in range(2)]; t_ss = [Trk(), Trk()]
        xn = [A.alloc([D], BF16) for _ in range(2)]; t_xn = [Trk(), Trk()]
        for i in range(NT):
            bi = i % 2
            is_ctx = i < NTC
            if l == 0:
                if is_ctx:
                    DMA("sp", xt[bi], ctx_d[b, i * 128:(i + 1) * 128, :], [], [t_xt[bi]])
                else:
                    j = i - NTC
                    DMA("sp", xt[bi], x_d[b, j * 128:(j + 1) * 128, :], [], [t_xt[bi]])
                    DMA("sp", pt_[bi], C["pos"][j * 128:(j + 1) * 128, :], [], [t_pt[bi]])
                    TT("dve", xt[bi], xt[bi], pt_[bi], ALU.add, [t_xt[bi], t_pt[bi]], [t_xt[bi]])
                DMA("sp", xres[b, i * 128:(i + 1) * 128, :], xt[bi], [t_xt[bi]], [])
            else:
                DMA("sp", xt[bi], xres[b, i * 128:(i + 1) * 128, :], [], [t_xt[bi]])
            norm_mod_transpose(xt[bi], t_xt[bi], hT, t_hT, i * 128, 0, cond_row(b, is_ctx),
                               (junk[bi], t_junk[bi], ssb[bi], t_ss[bi], xn[bi], t_xn[bi]), i)
        S.barrier()
        A.release(m0)
        segs = [("l", NTC, NTL)] + ([("c", 0, NTC)] if not last else [])
        mix_c0 = 0 if not last else LC

        m0 = A.mark()
        utok = A.alloc([NT, 768], BF16); t_utok = Trk()
        mmid = A.mark()
        u = A.alloc([6, T], BF16); t_u = Trk()
        zhy = A.alloc([6, T], BF16); t_zhy = Trk()

        def ev_hy(j, g0, gw, ps, pt):
            CP(evac_eng(j), zhy[:, j, g0:g0 + gw], ps, [pt], [t_zhy])
        win_chunks(l, hT, t_hT, [j * 128 for j in range(6)], ev_hy, mix_c0, T)
        ut = A.alloc([T]); t_ut = Trk()
        for j in range(6):
            for tag, t0, nt in segs:
                a0, a1 = t0 * 128, (t0 + nt) * 128
                ACT(ut[:, a0:a1], zhy[:, j, a0:a1], AF.Identity, [t_zhy, t_vec], [t_ut],
                    scale=vecT[:, 16 + 6 + j:16 + 6 + j + 1], bias=vecT[:, 34 + j:35 + j])
                STT("dve", ut[:, a0 + 1:a1], zhy[:, j, a0:a1 - 1], vecT[:, 16 + j:17 + j], ut[:, a0 + 1:a1],
                    ALU.mult, ALU.add, [t_zhy, t_vec, t_ut], [t_ut])
                STT("dve", u[:, j, a0:a1 - 1], zhy[:, j, a0 + 1:a1], vecT[:, 16 + 12 + j:16 + 13 + j], ut[:, a0:a1 - 1],
                    ALU.mult, ALU.add, [t_zhy, t_vec, t_ut], [t_u])
                CP("pool", u[:, j, a1 - 1:a1], ut[:, a1 - 1:a1], [t_ut], [t_u])
        for i in range(mix_c0 // 128, NT):
            pb = i % 2
            pv = PS[pb][:, :].bitcast(BF16)
            for j in range(6):
                TRP(pv[:, j * 128:(j + 1) * 128], u[:, j, i * 128:(i + 1) * 128], identb, [t_u, t_identb], [PT[pb]])
            CP(evac_eng(i), utok[:, i, :], pv[:, 0:768], [PT[pb]], [t_utok])
        S.barrier()
        A.release(mmid)
        ynT = A.alloc([2, T], BF16); t_ynT = Trk()
        for tag, t0, nt in segs:
            m1 = A.mark()
            n = nt * 128
            KK = A.alloc([nt, 2, 256], BF16); t_KKl = Trk()
            FB = [A.alloc([2, nt, 128], BF16) for _ in range(2)]; t_FB = [Trk(), Trk()]
            Pr = A.alloc([nt, 256], BF16); Pi = A.alloc([nt, 256], BF16); t_P = Trk()
            y1 = A.alloc([nt, 256], BF16); t_y1 = Trk()
            tmp = [A.alloc([4, 256]) for _ in range(2)]; t_tmp = [Trk(), Trk()]
            y2 = A.alloc([256]); t_y2 = Trk()
            yn = A.alloc([256], BF16); t_yn = Trk()
            junk2 = A.alloc([256], BF16); t_j2 = Trk()
            ss2 = A.alloc([8]); t_ss2 = Trk()
            for order in range(2):
                for r_ in range(2):
                    DMA("sp", KK[:, :, r_, :], kk_d[tag][l, :, :, r_, order, :].rearrange("k p c -> p k c"), [], [t_KKl])

                def src(tc):
                    if order == 0:
                        return utok[:, t0 + tc, 0:256], t_utok
                    return y1[:, tc, :], t_y1
                for kc in range(nt):
                    bi = kc % 2
                    DMA("sp", FB[bi], C["hyf" + tag][kc], [], [t_FB[bi]])
                    pr_, pi_ = 4 + 2 * bi, 5 + 2 * bi
                    for ri, pb in ((0, pr_), (1, pi_)):
                        for tc in range(nt):
                            s_ap, s_t = src(tc)
                            MM(PS[pb][:, 0:256], FB[bi][:, ri, tc, :], s_ap, tc == 0, tc == nt - 1, [t_FB[bi], s_t], [PT[pb]])
                    tb = tmp[bi]
                    Kr = KK[:, kc, 0, :]
                    Ki = KK[:, kc, 1, :]
                    TT("dve", tb[:, 0, :], PS[pr_][:, 0:256], Kr, ALU.mult, [PT[pr_], t_KKl], [t_tmp[bi]])
                    TT("dve", tb[:, 1, :], PS[pi_][:, 0:256], Ki, ALU.mult, [PT[pi_], t_KKl], [t_tmp[bi]])
                    TT("dve", tb[:, 2, :], PS[pr_][:, 0:256], Ki, ALU.mult, [PT[pr_], t_KKl], [t_tmp[bi]])
                    TT("dve", tb[:, 3, :], PS[pi_][:, 0:256], Kr, ALU.mult, [PT[pi_], t_KKl], [t_tmp[bi]])
                    TT("pool", Pr[:, kc, :], tb[:, 0, :], tb[:, 1, :], ALU.add, [t_tmp[bi]], [t_P])
                    TT("pool", Pi[:, kc, :], tb[:, 2, :], tb[:, 3, :], ALU.subtract, [t_tmp[bi]], [t_P])
                for tc in range(nt):
                    bi = tc % 2
                    DMA("sp", FB[bi], C["hyi" + tag][tc], [], [t_FB[bi]])
                    pb = 4 + bi
                    for kc in range(nt):
                        MM(PS[pb][:, 0:256], FB[bi][:, 0, kc, :], Pr[:, kc, :], kc == 0, False, [t_FB[bi], t_P], [PT[pb]])
                        MM(PS[pb][:, 0:256], FB[bi][:, 1, kc, :], Pi[:, kc, :], False, kc == nt - 1, [t_FB[bi], t_P], [PT[pb]])
                    if order == 0:
                        TT("dve", y1[:, tc, :], PS[pb][:, 0:256], utok[:, t0 + tc, 256:512], ALU.mult, [PT[pb], t_utok], [t_y1])
                    else:
                        TT("dve", y2, PS[pb][:, 0:256], utok[:, t0 + tc, 512:768], ALU.mult, [PT[pb], t_utok], [t_y2])
                        ACT(junk2, y2, AF.Square, [t_y2], [t_j2, t_ss2], accum=ss2[:, 0:1])
                        rms_rstd(ss2[:, 0:1], 256, [t_ss2], ss2[:, 1:2], t_ss2)
                        TS("dve", yn, y2, ss2[:, 1:2], None, ALU.mult, None, [t_y2, t_ss2], [t_yn])
                        pq = 6 + bi
                        pv = PS[pq][:, :].bitcast(BF16)
                        for j in range(2):
                            TRP(pv[:, j * 128:(j + 1) * 128], yn[:, j * 128:(j + 1) * 128], identb, [t_yn, t_identb], [PT[pq]])
                        col = (t0 + tc) * 128
                        for j in range(2):
                            TS("dve", ynT[:, j, col:col + 128], pv[:, j * 128:(j + 1) * 128], vecT[:, 92 + j:93 + j], None,
                               ALU.mult, None, [PT[pq], t_vec], [t_ynT])
            S.barrier()
            A.release(m1)
        DMA("sp", yT_d[:, 0:2, mix_c0:T], ynT[:, :, mix_c0:T], [t_ynT], [])
        S.barrier()
        A.release(m0)

        m0 = A.mark()
        zfn = A.alloc([2, T], BF16); t_zfn = Trk()

        def ev_fn(j, g0, gw, ps, pt):
            CP(evac_eng(j), zfn[:, j, g0:g0 + gw], ps, [pt], [t_zfn])
        win_chunks(l, hT, t_hT, [3328, 3456], ev_fn, mix_c0, T)
        zc = A.alloc([NT, 256], BF16); zs = A.alloc([NT, 256], BF16); t_zcs = Trk()
        for i in range(mix_c0 // 128, NT):
            pa, pb2 = 4 + 2 * (i % 2), 5 + 2 * (i % 2)
            for j in range(2):
                MM(PS[pa][:, j * 128:(j + 1) * 128], zfn[:, j, i * 128:(i + 1) * 128], bda[:, j, :], True, True, [t_zfn, t_bd], [PT[pa]])
                MM(PS[pb2][:, j * 128:(j + 1) * 128], zfn[:, j, i * 128:(i + 1) * 128], bdb[:, j, :], True, True, [t_zfn, t_bd], [PT[pb2]])
            CP("act", zc[:, i, :], PS[pa][:, 0:256], [PT[pa]], [t_zcs])
            CP("dve", zs[:, i, :], PS[pb2][:, 0:256], [PT[pb2]], [t_zcs])
        fnb = A.alloc([256]); t_fnb = Trk()
        DMA("sp", fnb, W["fn_b"][l].partition_broadcast(128), [], [t_fnb])
        ynT = A.alloc([2, T], BF16); t_ynT = Trk()
        y2 = A.alloc([256]); t_y2 = Trk()
        yn = A.alloc([256], BF16); t_yn = Trk()
        junk2 = A.alloc([256], BF16); t_j2 = Trk()
        ss2 = A.alloc([8]); t_ss2 = Trk()
        for tag, t0, nt in segs:
            m1 = A.mark()
            FB = [A.alloc([2, nt, 128], BF16) for _ in range(2)]; t_FB = [Trk(), Trk()]
            for kc in range(nt):
                bi = kc % 2
                DMA("sp", FB[bi], C["fnf" + tag][kc], [], [t_FB[bi]])
                pb = 4 + bi
                for tc in range(nt):
                    MM(PS[pb][:, 0:256], FB[bi][:, 0, tc, :], zc[:, t0 + tc, :], tc == 0, False, [t_FB[bi], t_zcs], [PT[pb]])
                    MM(PS[pb][:, 0:256], FB[bi][:, 1, tc, :], zs[:, t0 + tc, :], False, tc == nt - 1, [t_FB[bi], t_zcs], [PT[pb]])
                TT("dve", y2, PS[pb][:, 0:256], fnb, ALU.add, [PT[pb], t_fnb], [t_y2])
                ACT(junk2, y2, AF.Square, [t_y2], [t_j2, t_ss2], accum=ss2[:, 0:1])
                rms_rstd(ss2[:, 0:1], 256, [t_ss2], ss2[:, 1:2], t_ss2)
                TS("dve", yn, y2, ss2[:, 1:2], None, ALU.mult, None, [t_y2, t_ss2], [t_yn])
                pq = 6 + bi
                pv = PS[pq][:, :].bitcast(BF16)
                for j in range(2):
                    TRP(pv[:, j * 128:(j + 1) * 128], yn[:, j * 128:(j + 1) * 128], identb, [t_yn, t_identb], [PT[pq]])
                col = (t0 + kc) * 128
                for j in range(2):
                    TS("dve", ynT[:, j, col:col + 128], pv[:, j * 128:(j + 1) * 128], vecT[:, 94 + j:95 + j], None,
                       ALU.mult, None, [PT[pq], t_vec], [t_ynT])
            S.barrier()
            A.release(m1)
        DMA("sp", yT_d[:, 6:8, mix_c0:T], ynT[:, :, mix_c0:T], [t_ynT], [])
        S.barrier()
        A.release(m0)

        out_t0 = 0 if not last else NTC
        for h in range(4):
            m0 = A.mark()
            qs = A.alloc([T], BF16); sg = A.alloc([T], BF16); vT = A.alloc([T], BF16)
            t_q = Trk(); t_sg = Trk(); t_vT = Trk()

            def ev_hg(j, g0, gw, ps, pt):
                if j == 0:
                    ACT(qs[:, g0:g0 + gw], ps, AF.Silu, [pt], [t_q])
                elif j == 1:
                    CP("dve", vT[:, g0:g0 + gw], ps, [pt], [t_vT])
                else:
                    ACT(sg[:, g0:g0 + gw], ps, AF.Silu, [pt], [t_sg])
            m1 = A.mark()
            win_chunks(l, hT, t_hT, [768 + 128 * h, 2304 + 128 * h, 2816 + 128 * h], ev_hg)
            S.barrier()
            A.release(m1)
            vtok = A.alloc([NT, 128], BF16); t_vtok = Trk()
            for i in range(NT):
                pb = i % 2
                pv = PS[pb][:, :].bitcast(BF16)
                TRP(pv[:, 0:128], vT[:, i * 128:(i + 1) * 128], identb, [t_vT, t_identb], [PT[pb]])
                CP(evac_eng(i), vtok[:, i, :], pv[:, 0:128], [PT[pb]], [t_vtok])
            sig = A.alloc([T]); t_sig = Trk()
            kkb = A.alloc([T]); bb = A.alloc([T]); xb_ = A.alloc([T])
            t_kk = Trk(); t_bb = Trk(); t_xb = Trk()
            qd = A.alloc([T], BF16); ke = A.alloc([T], BF16); qr = A.alloc([T], BF16)
            KI = [A.alloc([T], BF16) for _ in range(4)]
            Rblk = A.alloc([T // 8]); t_R = Trk()
            t_qd = Trk(); t_kd = Trk(); t_ke = Trk(); t_qr = Trk()
            ebend = A.alloc([NCH]); t_eb = Trk()
            Sall = A.alloc([NCH, 128], BF16); t_Sall = Trk()
            Sm = A.alloc([128]); t_Sm = Trk()
            oacc = A.alloc([NT, 128]); t_oacc = Trk(); t_oaccs = [Trk() for _ in range(NT)]
            ATa = A.alloc([NT, 128], BF16); t_ATa = Trk()
            kzT = [A.alloc([4, 128], BF16) for _ in range(3)]; t_kzT = [Trk(), Trk(), Trk()]
            ona = A.alloc([NT, 128], BF16); t_on = Trk()
            ssa = A.alloc([NT]);
            junk3 = A.alloc([128], BF16); t_j3 = Trk()
            ss3 = A.alloc([8]); t_ss3 = Trk()
            yhT = A.alloc([T], BF16); t_yhT = Trk()
            for d in range(2):
                li = d * 4 + h
                m1 = A.mark()

                def ev_f(j, g0, gw, ps, pt):
                    ACT(sig[:, g0:g0 + gw], ps, AF.Sigmoid, [pt], [t_sig])
                win_chunks(l, hT, t_hT, [1280 + 512 * d + 128 * h], ev_f)
                S.barrier()
                A.release(m1)
                TS("dve", kkb, sig, lbT[:, 2, li:li + 1], lbT[:, 1, li:li + 1], ALU.mult, ALU.add, [t_sig, t_lb], [t_kk])
                ACT(sig, sig, AF.Ln, [t_sig, t_lb], [t_sig], scale=lbT[:, 1, li:li + 1], bias=lbT[:, 0, li:li + 1])
                SCAN(bb, scanm, sig, [t_scanm, t_sig], [t_bb])
                b3 = bb.rearrange("p (c j) -> p c j", j=32)
                x3 = xb_.rearrange("p (c j) -> p c j", j=32)
                k3 = kkb.rearrange("p (c j) -> p c j", j=32)
                b8 = bb.rearrange("p (c j) -> p c j", j=8)
                l8 = sig.rearrange("p (c j) -> p c j", j=8)
                x8 = xb_.rearrange("p (c j) -> p c j", j=8)
                NB8 = T // 8
                if d == 1:
                    TT("dve", xb_, sig, bb, ALU.subtract, [t_sig, t_bb], [t_xb])
                    CP("dve", ebend, b3[:, :, 31], [t_bb], [t_eb])
                    TT("dve", b3, x3, ebend.unsqueeze(2).to_broadcast([128, NCH, 32]), ALU.add, [t_xb, t_eb], [t_bb])
                    endc = 0
                    rc = 7
                else:
                    endc = 31
                    rc = 0
                TT("dve", Rblk, b8[:, :, rc], l8[:, :, rc], ALU.subtract, [t_bb, t_sig], [t_R])
                TT("dve", x8, b8, Rblk.unsqueeze(2).to_broadcast([128, NB8, 8]), ALU.subtract, [t_bb, t_R], [t_xb])
                ACT(xb_, xb_, AF.Exp, [t_xb], [t_xb])
                STT("dve", qr, xb_, float(128 ** -0.5), qs, ALU.mult, ALU.mult, [t_xb, t_q], [t_qr])
                R4 = Rblk.rearrange("p (c i) -> p c i", i=4)
                for I in range(4):
                    lo, hi = (0, 8 * (I + 1)) if d == 0 else (8 * I, 32)
                    w_ = hi - lo
                    MSET("pool", KI[I], 0.0, [t_kd])
                    TT("dve", x3[:, :, lo:hi], R4[:, :, I:I + 1].to_broadcast([128, NCH, w_]), b3[:, :, lo:hi], ALU.subtract,
                       [t_R, t_bb], [t_xb])
                    ACT(x3[:, :, lo:hi], x3[:, :, lo:hi], AF.Exp, [t_xb], [t_xb])
                    TT("dve", KI[I].rearrange("p (c j) -> p c j", j=32)[:, :, lo:hi], x3[:, :, lo:hi], k3[:, :, lo:hi], ALU.mult,
                       [t_xb, t_kk], [t_kd])
                TT("dve", x3, b3[:, :, endc:endc + 1].to_broadcast([128, NCH, 32]), b3, ALU.subtract, [t_bb], [t_xb])
                ACT(xb_, xb_, AF.Exp, [t_xb], [t_xb])
                TT("dve", ke, xb_, kkb, ALU.mult, [t_xb, t_kk], [t_ke])
                ACT(ebend, b3[:, :, endc], AF.Exp, [t_bb], [t_eb])
                ACT(bb, bb, AF.Exp, [t_bb], [t_bb])
                STT("dve", qd, bb, float(128 ** -0.5), qs, ALU.mult, ALU.mult, [t_bb, t_q], [t_qd])
                if d == 0:
                    order = list(range(NCH))
                else:
                    order = list(range(NCHC - 1, -1, -1)) + list(range(NCH - 1, NCHC - 1, -1))
                MSET("pool", Sm, 0.0, [t_Sm])
                tiles_in_order = []
                for cch in order:
                    if cch // 4 not in tiles_in_order:
                        tiles_in_order.append(cch // 4)
                for n_, i in enumerate(tiles_in_order):
                    bi = n_ % 3
                    pk = 4 + n_ % 2
                    pkv = 6 + n_ % 2
                    pv = PS[pk][:, :].bitcast(BF16)
                    TRP(pv[:, 0:128], ke[:, i * 128:(i + 1) * 128], identb, [t_ke, t_identb], [PT[pk]])
                    for j in range(4):
                        ACT(kzT[bi][:, j, :], pv[:, 0:128], AF.Identity, [PT[pk], t_mask], [t_kzT[bi]], scale=rowm[:, j:j + 1])
                    for j in range(4):
                        MM(PS[pkv][:, j * 128:(j + 1) * 128], kzT[bi][:, j, :], vtok[:, i, :], True, True,
                           [t_kzT[bi], t_vtok], [PT[pkv]])
                    chs = [cch for cch in order if cch // 4 == i]
                    for cch in chs:
                        j = cch % 4
                        CP("dve", Sall[:, cch, :], Sm, [t_Sm], [t_Sall])
                        STT("dve", Sm, Sm, ebend[:, cch:cch + 1], PS[pkv][:, j * 128:(j + 1) * 128], ALU.mult, ALU.add,
                            [t_Sm, t_eb, PT[pkv]], [t_Sm])
                msk = maskf if d == 0 else maskb
                for i in range(out_t0, NT):
                    pa = i % 4
                    cs = slice(i * 128, (i + 1) * 128)
                    for I in range(4):
                        MM(PS[pa][:, I * 32:(I + 1) * 32], KI[I][:, cs],
                           qr[:, cs].rearrange("p (c i j) -> p c i j", c=4, i=4)[:, :, I, :], True, True, [t_kd, t_qr], [PT[pa]])
                    TT("dve", ATa[:, i, :].rearrange("p (c i j) -> p i c j", c=4, i=4),
                       PS[pa][:, 0:128].rearrange("p (i c j) -> p i c j", i=4, c=4),
                       msk.rearrange("p (c i j) -> p i c j", c=4, i=4), ALU.mult, [PT[pa], t_mask], [t_ATa])
                tl = list(range(out_t0, NT))
                for g0 in range(0, len(tl), 4):
                    grp = tl[g0:g0 + 4]
                    for gi, i in enumerate(grp):
                        cs = slice(i * 128, (i + 1) * 128)
                        MM(PS[gi][:, 128:256], ATa[:, i, :], vtok[:, i, :], True, True, [t_ATa, t_vtok], [PT[gi]])
                        MM(PS[4 + gi][:, :], qd[:, cs], Sall[:, 4 * i:4 * i + 4, :].rearrange("p j c -> p (j c)"), True, True,
                           [t_qd, t_Sall], [PT[4 + gi]])
                    for gi, i in enumerate(grp):
                        dst = oacc[:, i, :]
                        if d == 0:
                            CP("act", dst, PS[gi][:, 128:256], [PT[gi]], [t_oaccs[i]])
                        else:
                            TT("dve", dst, PS[gi][:, 128:256], dst, ALU.add, [PT[gi], t_oaccs[i]], [t_oaccs[i]])
                    for j in range(4):
                        for gi, i in enumerate(grp):
                            dst = oacc[:, i, :]
                            STT("dve", dst, PS[4 + gi][:, j * 128:(j + 1) * 128], rowm[:, j:j + 1], dst, ALU.mult, ALU.add,
                                [PT[4 + gi], t_mask, t_oaccs[i]], [t_oaccs[i]])
                if d == 1:
                    for i in range(out_t0, NT):
                        ACT(junk3, oacc[:, i, :], AF.Square, [t_oaccs[i]], [t_j3, t_ss3], accum=ssa[:, i:i + 1])
                    rms_rstd(ssa[:, out_t0:NT], 128, [t_ss3], ssa[:, out_t0:NT], t_ss3)
                    for i in range(out_t0, NT):
                        ACT(ona[:, i, :], oacc[:, i, :], AF.Identity, [t_oaccs[i], t_ss3], [t_on], scale=ssa[:, i:i + 1])
                    for i in range(out_t0, NT):
                        pq = 4 + i % 4
                        cs = slice(i * 128, (i + 1) * 128)
                        pv = PS[pq][:, :].bitcast(BF16)
                        TRP(pv[:, 0:128], ona[:, i, :], identb, [t_on, t_identb], [PT[pq]])
                        STT("dve", yhT[:, cs], pv[:, 0:128], vecT[:, 40 + h:41 + h], sg[:, cs], ALU.mult, ALU.mult,
                            [PT[pq], t_vec, t_sg], [t_yhT])
            DMA("sp", yT_d[:, 2 + h, mix_c0:T], yhT[:, mix_c0:T], [t_yhT], [])
            S.barrier()
            A.release(m0)

    def ffn_sublayer(l, b, hT, t_hT):
        last = (l == NL - 1)
        tlo = 0 if not last else NTC
        m0 = A.mark()
        ysb = A.alloc([8, T], BF16); t_ysb = Trk()
        DMA("sp", ysb[:, :, tlo * 128:T], yT_d[:, :, tlo * 128:T], [], [t_ysb])
        wo = A.alloc([KC, D], BF16); t_wo = Trk()
        DMA("pool", wo, W["w_out"][l].rearrange("(kc p) n -> p kc n", p=128), [], [t_wo])
        gt = {}
        t_gt = Trk()
        for r in ([b] if last else [b, 2]):
            gt[r] = A.alloc([D])
            DMA("sp", gt[r], gate_d[l, r, 0].partition_broadcast(128), [], [t_gt])
        xt = [A.alloc([D]) for _ in range(2)]; t_xt = [Trk(), Trk()]
        t1 = A.alloc([D]); t_t1 = Trk()
        junk = [A.alloc([D], BF16) for _ in range(2)]; t_junk = [Trk(), Trk()]
        ssb = [A.alloc([8]) for _ in range(2)]; t_ss = [Trk(), Trk()]
        xn = [A.alloc([D], BF16) for _ in range(2)]; t_xn = [Trk(), Trk()]
        for i in range(tlo, NT):
            bi = i % 2
            r = cond_row(b, i < NTC)
            DMA("sp", xt[bi], xres[b, i * 128:(i + 1) * 128, :], [], [t_xt[bi]])
            for hh in range(2):
                pb = 2 + hh
                for kc in range(KC):
                    MM(PS[pb][:, :], ysb[:, kc, i * 128:(i + 1) * 128], wo[:, kc, hh * 512:(hh + 1) * 512], kc == 0, kc == KC - 1,
                       [t_ysb, t_wo], [PT[pb]])
                TT("dve", t1[:, hh * 512:(hh + 1) * 512], PS[pb][:, :], gt[r][:, hh * 512:(hh + 1) * 512], ALU.mult, [PT[pb], t_gt], [t_t1])
            TT("dve", xt[bi], xt[bi], t1, ALU.add, [t_xt[bi], t_t1], [t_xt[bi]])
            DMA("sp", xres[b, i * 128:(i + 1) * 128, :], xt[bi], [t_xt[bi]], [])
            norm_mod_transpose(xt[bi], t_xt[bi], hT, t_hT, i * 128, 2, r,
                               (junk[bi], t_junk[bi], ssb[bi], t_ss[bi], xn[bi], t_xn[bi]), i)
        S.barrier()
        A.release(m0)
        ntiles = NT - tlo
        halves = [(tlo, tlo + (ntiles + 1) // 2), (tlo + (ntiles + 1) // 2, NT)] if not HALF_EXP else [(tlo, tlo + 1), (tlo + 1, NT)]
        for (ta, tb) in halves:
            if tb <= ta:
                continue
            m0 = A.mark()
            nth = tb - ta
            comb = A.alloc([nth, NEXP]); t_comb = Trk()
            rt = A.alloc([12, NEXP]); t_rt = Trk()
            sc = A.alloc([16]); t_sc = Trk()
            for ii in range(nth):
                i = ta + ii
                pb = ii % 2
                for kc in range(KC):
                    MM(PS[pb][:, 0:NEXP], hT[:, kc, i * 128:(i + 1) * 128], rw_sb[:, kc, :], kc == 0, kc == KC - 1, [t_hT, t_rw], [PT[pb]])
                lg = rt[:, 0, :]
                CP("dve", lg, PS[pb][:, 0:NEXP], [PT[pb]], [t_rt])
                RED(sc[:, 0:1], lg, ALU.max, [t_rt], [t_sc])
                TS("dve", sc[:, 1:2], sc[:, 0:1], -1.0, None, ALU.mult, None, [t_sc], [t_sc])
                ACT(rt[:, 1, :], lg, AF.Exp, [t_rt, t_sc], [t_rt, t_sc], bias=sc[:, 1:2], accum=sc[:, 2:3])
                RCP(sc[:, 3:4], sc[:, 2:3], [t_sc], [t_sc])
                TS("dve", rt[:, 2, :], rt[:, 1, :], sc[:, 3:4], None, ALU.mult, None, [t_rt, t_sc], [t_rt])
                TT("dve", rt[:, 3, :], rt[:, 2, :], rb_sb, ALU.add, [t_rt, t_rb], [t_rt])
                sel3 = rt[:, 3, :].rearrange("p (g k) -> p g k", k=4)
                RED(sc[:, 4:8], sel3, ALU.max, [t_rt], [t_sc])
                TT("dve", rt[:, 4, :].rearrange("p (g k) -> p g k", k=4), sel3, sc[:, 4:8].unsqueeze(2).to_broadcast([128, 4, 4]),
                   ALU.is_equal, [t_rt, t_sc], [t_rt])
                STT("dve", rt[:, 5, :], rt[:, 4, :], -1e9, rt[:, 3, :], ALU.mult, ALU.add, [t_rt], [t_rt])
                sel23 = rt[:, 5, :].rearrange("p (g k) -> p g k", k=4)
                RED(sc[:, 8:12], sel23, ALU.max, [t_rt], [t_sc])
                TT("dve", sc[:, 12:16], sc[:, 4:8], sc[:, 8:12], ALU.add, [t_sc], [t_sc])
                RED(sc[:, 0:1], sc[:, 12:16], ALU.max, [t_sc], [t_sc])
                TS("dve", sc[:, 12:16], sc[:, 12:16], sc[:, 0:1], None, ALU.is_equal, None, [t_sc], [t_sc])
                TT("dve", rt[:, 6, :].rearrange("p (g k) -> p g k", k=4), sel3, sc[:, 8:12].unsqueeze(2).to_broadcast([128, 4, 4]),
                   ALU.is_ge, [t_rt, t_sc], [t_rt])
                TT("dve", rt[:, 6, :].rearrange("p (g k) -> p g k", k=4), rt[:, 6, :].rearrange("p (g k) -> p g k", k=4),
                   sc[:, 12:16].unsqueeze(2).to_broadcast([128, 4, 4]), ALU.mult, [t_rt, t_sc], [t_rt])
                TT("dve", rt[:, 7, :], rt[:, 6, :], rt[:, 2, :], ALU.mult, [t_rt], [t_rt])
                RED(sc[:, 1:2], rt[:, 7, :], ALU.add, [t_rt], [t_sc])
                RCP(sc[:, 2:3], sc[:, 1:2], [t_sc], [t_sc])
                TS("dve", comb[:, ii, :], rt[:, 7, :], sc[:, 2:3], None, ALU.mult, None, [t_rt, t_sc], [t_comb])
                if DEBUG and ta == tlo and l == 0 and ii == 0 and b == 0:
                    DMA("sp", dbg_acc[:, 8, 0:192], rt.rearrange("p a b -> p (a b)"), [t_rt], [])
                    DMA("sp", dbg_acc[:, 9, 0:16], sc, [t_sc], [])
            acc = A.alloc([nth, D]); t_acc = Trk()
            wge = [A.alloc([KC, DEXP], BF16) for _ in range(2)]
            wue = [A.alloc([KC, DEXP], BF16) for _ in range(2)]
            wde = [A.alloc([4, D], BF16) for _ in range(2)]
            t_we = [Trk(), Trk()]
            At = [A.alloc([4, 512], BF16) for _ in range(2)]; t_At = [Trk(), Trk()]
            sl = [A.alloc([512]) for _ in range(2)]; t_sl = [Trk(), Trk()]
            cnt = 0
            for e_ in range(NEXP):
                bi = e_ % 2
                DMA("pool", wge[bi], W["moe_w_gate"][l, e_].rearrange("(kc p) n -> p kc n", p=128), [], [t_we[bi]])
                DMA("pool", wue[bi], W["moe_w_up"][l, e_].rearrange("(kc p) n -> p kc n", p=128), [], [t_we[bi]])
                DMA("pool", wde[bi], W["moe_w_down"][l, e_].rearrange("(kc p) n -> p kc n", p=128), [], [t_we[bi]])
                for (g0, gw) in col_groups(ta * 128, tb * 128):
                    ab = cnt % 2
                    cnt += 1
                    for oc in range(4):
                        pg, pu = oc % 2, 2 + oc % 2
                        for kc in range(KC):
                            MM(PS[pg][:, 0:gw], wge[bi][:, kc, oc * 128:(oc + 1) * 128], hT[:, kc, g0:g0 + gw], kc == 0, kc == KC - 1,
                               [t_we[bi], t_hT], [PT[pg]])
                        for kc in range(KC):
                            MM(PS[pu][:, 0:gw], wue[bi][:, kc, oc * 128:(oc + 1) * 128], hT[:, kc, g0:g0 + gw], kc == 0, kc == KC - 1,
                               [t_we[bi], t_hT], [PT[pu]])
                        sb_ = oc % 2
                        ACT(sl[sb_][:, 0:gw], PS[pg][:, 0:gw], AF.Silu, [PT[pg]], [t_sl[sb_]])
                        TT("dve", At[ab][:, oc, 0:gw], PS[pu][:, 0:gw], sl[sb_][:, 0:gw], ALU.mult, [PT[pu], t_sl[sb_]], [t_At[ab]])
                    for tt_ in range(gw // 128):
                        ti = (g0 // 128 - ta) + tt_
                        for hh in range(2):
                            py = 4 + (2 * tt_ + hh) % 4
                            for kc in range(4):
                                MM(PS[py][:, :], At[ab][:, kc, tt_ * 128:(tt_ + 1) * 128], wde[bi][:, kc, hh * 512:(hh + 1) * 512],
                                   kc == 0, kc == 3, [t_At[ab], t_we[bi]], [PT[py]])
                            dst = acc[:, ti, hh * 512:(hh + 1) * 512]
                            if e_ == 0:
                                TS("dve", dst, PS[py][:, :], comb[:, ti, e_:e_ + 1], None, ALU.mult, None, [PT[py], t_comb], [t_acc])
                            else:
                                STT("dve", dst, PS[py][:, :], comb[:, ti, e_:e_ + 1], dst, ALU.mult, ALU.add, [PT[py], t_comb, t_acc], [t_acc])
            if DEBUG and ta == tlo and l == 0:
                DMA("sp", dbg_acc[:, 0:nth, :], acc, [t_acc], [])
                DMA("sp", dbg_comb[:, 0:nth, :], comb, [t_comb], [])
                DMA("sp", dbg_h, hT[:, :, 0:256], [t_hT], [])
            g5 = {}
            t_g5 = Trk()
            for r in ([b] if last else [b, 2]):
                g5[r] = A.alloc([D])
                DMA("sp", g5[r], gate_d[l, r, 1].partition_broadcast(128), [], [t_g5])
            xt = [A.alloc([D]) for _ in range(2)]; t_xt = [Trk(), Trk()]
            junk = A.alloc([D], BF16); t_junk = Trk()
            ssb = A.alloc([8]); t_ss = Trk()
            for ii in range(nth):
                i = ta + ii
                bi = ii % 2
                r = cond_row(b, i < NTC)
                DMA("sp", xt[bi], xres[b, i * 128:(i + 1) * 128, :], [], [t_xt[bi]])
                TT("dve", acc[:, ii, :], acc[:, ii, :], g5[r], ALU.mult, [t_acc, t_g5], [t_acc])
                TT("dve", xt[bi], xt[bi], acc[:, ii, :], ALU.add, [t_xt[bi], t_acc], [t_xt[bi]])
                if not last:
                    DMA("sp", xres[b, i * 128:(i + 1) * 128, :], xt[bi], [t_xt[bi]], [])
                else:
                    ACT(junk, xt[bi], AF.Square, [t_xt[bi]], [t_junk, t_ss], accum=ssb[:, 0:1])
                    rms_rstd(ssb[:, 0:1], D, [t_ss], ssb[:, 1:2], t_ss)
                    STT("dve", xt[bi], xt[bi], ssb[:, 1:2], fng_sb, ALU.mult, ALU.mult, [t_xt[bi], t_ss, t_fng], [t_xt[bi]])
                    j = i - NTC
                    DMA("sp", out_d[b, j * 128:(j + 1) * 128, :], xt[bi], [t_xt[bi]], [])
            S.barrier()
            A.release(m0)

    for l in range(NL):
        if STOP_AFTER is not None and l > STOP_AFTER:
            break
        layer_setup(l)
        for b in range(NB):
            m0 = A.mark()
            hT = A.alloc([KC, T], BF16)
            t_hT = Trk()
            mixer_sublayer(l, b, hT, t_hT)
            ffn_sublayer(l, b, hT, t_hT)
            S.barrier()
            A.release(m0)
    stats = S.emit(es)
    es.close()
    return nc, hc, stats, A.peak


_CACHE = {}


def kernel(**inputs):
    NB, L, LC = 2, 2048, 256
    B = inputs["x"].shape[0]
    ncores = B // NB
    if "prog" not in _CACHE:
        _CACHE["prog"] = build(NB, L, LC)
    nc, hc, stats, peak = _CACHE["prog"]
    shared = {k: np.ascontiguousarray(np.asarray(inputs[k], dtype=np.float32)) for k in WEIGHT_SHAPES}
    shared["c_ctx"] = np.ascontiguousarray(np.asarray(inputs["c_ctx"], dtype=np.float32))
    for k, v in hc.items():
        shared["k_" + k] = v
    x = np.asarray(inputs["x"], dtype=np.float32)
    c = np.asarray(inputs["c"], dtype=np.float32)
    ctx = np.asarray(inputs["ctx"], dtype=np.float32)
    in_maps = []
    for i in range(ncores):
        m = dict(shared)
        m["x"] = np.ascontiguousarray(x[i * NB:(i + 1) * NB])
        m["c"] = np.ascontiguousarray(c[i * NB:(i + 1) * NB])
        m["ctx"] = np.ascontiguousarray(ctx[i * NB:(i + 1) * NB])
        in_maps.append(m)
    res = run_bass_kernel_spmd(nc, in_maps, core_ids=list(range(ncores)))
    return np.concatenate([r["out"] for r in res.results], axis=0).astype(np.float32)
```

```python
import math
from contextlib import ExitStack
import numpy as np
import ml_dtypes
import concourse.bass as bass
import concourse.mybir as mybir
from concourse.bass_utils import run_bass_kernel_spmd

F32 = mybir.dt.float32
BF16 = mybir.dt.bfloat16
ALU = mybir.AluOpType
AF = mybir.ActivationFunctionType
AX = mybir.AxisListType

ENGS = ("pe", "act", "dve", "pool", "sp")
ROT = 12000
NDMASEM = 64
RING = {'sp': (0, 40), 'pool': (40, 20), 'act': (60, 4), 'dve': (60, 4), 'pe': (60, 4)}


class Trk:
    __slots__ = ("lw", "rd")

    def __init__(self):
        self.lw = None
        self.rd = []


class Op:
    __slots__ = ("eng", "fn", "deps", "mile", "isdma", "ev", "semi")


class Sched:
    def __init__(self, nc):
        self.nc = nc
        self.ops = {e: [] for e in ENGS}
        self.ndma = 0
        self.dma_cnt = [0] * NDMASEM
        self.bar_deps = []
        self.dma_open = []
        self.all_dma = []
        self.ring_pos = {e: 0 for e in ENGS}
        self.sem_last = [None] * NDMASEM

    def op(self, eng, fn, reads=(), writes=(), dma=False):
        o = Op()
        o.eng = eng
        o.fn = fn
        o.isdma = dma
        o.mile = False
        o.ev = None
        deps = list(self.bar_deps)
        for t in reads:
            if t.lw is not None:
                deps.append(t.lw)
        for t in writes:
            if t.lw is not None:
                deps.append(t.lw)
            deps.extend(t.rd)
        seen = set()
        dd = []
        for d in deps:
            if id(d) in seen or d is o:
                continue
            seen.add(id(d))
            if (not d.isdma) and (not dma) and d.eng == "pe" and eng == "pe":
                continue
            d.mile = True
            dd.append(d)
        o.deps = dd
        for t in reads:
            if (not dma) and t.rd and (not t.rd[-1].isdma) and t.rd[-1].eng == eng:
                t.rd[-1] = o
            else:
                t.rd.append(o)
        for t in writes:
            t.lw = o
            t.rd = []
        if dma:
            base, cnt_ = RING[eng]
            j = base + self.ring_pos[eng] % cnt_
            self.ring_pos[eng] += 1
            self.ndma += 1
            prev = self.sem_last[j]
            if prev is not None and all(prev is not d_ for d_ in o.deps):
                o.deps.append(prev)
            self.sem_last[j] = o
            self.dma_cnt[j] += 1
            o.semi = j
            o.ev = ("d", j, 16 * self.dma_cnt[j])
            self.dma_open.append(o)
            self.all_dma.append(o)
        self.ops[eng].append(o)
        return o

    def barrier(self):
        deps = []
        for e in ENGS:
            for o in reversed(self.ops[e]):
                if not o.isdma:
                    o.mile = True
                    deps.append(o)
                    break
        best = {}
        for o in self.dma_open:
            if o.semi not in best or best[o.semi].ev[2] < o.ev[2]:
                best[o.semi] = o
        deps.extend(best.values())
        self.dma_open = []
        self.bar_deps = deps

    def emit(self, es):
        nc = self.nc
        nsem = {}
        for e in ENGS:
            c = 0
            for o in self.ops[e]:
                if o.isdma:
                    continue
                if o.mile:
                    c += 1
                    o.ev = (e, (c - 1) // ROT, (c - 1) % ROT + 1)
            nsem[e] = (c + ROT - 1) // ROT if c else 0
        sems = {}
        for e in ENGS:
            for i in range(nsem[e]):
                sems[(e, i)] = es.enter_context(nc.semaphore(f"s_{e}{i}"))
        for j in range(NDMASEM):
            sems[("d", j)] = es.enter_context(nc.semaphore(f"s_d{j}"))
        block = es.enter_context(nc.Block())
        handles = {"pe": block.tensor, "act": block.scalar, "dve": block.vector,
                   "pool": block.gpsimd, "sp": block.sync}
        stats = {}
        for e in ENGS:
            ops = self.ops[e]
            last_dma = None
            if e == "sp":
                best = {}
                for o in self.all_dma:
                    if o.semi not in best or best[o.semi].ev[2] < o.ev[2]:
                        best[o.semi] = o
                last_dma = list(best.values())

            def body(eng, ops=ops, e=e, last_dma=last_dma):
                known = {}
                nw = 0
                for o in ops:
                    for d in o.deps:
                        k = (d.ev[0], d.ev[1])
                        v = d.ev[2]
                        if known.get(k, 0) < v:
                            eng.wait_ge(sems[k], v)
                            known[k] = v
                            nw += 1
                    ins = o.fn(eng)
                    if o.isdma:
                        ins.then_inc(sems[("d", o.semi)], 16)
                    elif o.mile:
                        ins.then_inc(sems[(o.ev[0], o.ev[1])], 1)
                if last_dma is not None:
                    for d in last_dma:
                        k = (d.ev[0], d.ev[1])
                        if known.get(k, 0) < d.ev[2]:
                            eng.wait_ge(sems[k], d.ev[2])
                            known[k] = d.ev[2]
                stats[e] = (len(ops), nw)

            handles[e](body)
        return stats


class Arena:
    def __init__(self, nc, es, nelem_f32):
        self.t = es.enter_context(nc.sbuf_tensor("arena", [128, nelem_f32], F32))
        self.n = nelem_f32
        self.off = 0
        self.peak = 0

    def mark(self):
        return self.off

    def release(self, m):
        self.off = m

    def alloc(self, shape_free, dtype=F32):
        n = int(np.prod(shape_free))
        nf = (n + 1) // 2 if dtype == BF16 else n
        nf = (nf + 7) // 8 * 8
        assert self.off + nf <= self.n, f"arena overflow {self.off}+{nf}>{self.n}"
        ap = self.t[:, self.off:self.off + nf]
        self.off += nf
        self.peak = max(self.peak, self.off)
        if dtype == BF16:
            ap = ap.bitcast(BF16)[:, 0:n]
        else:
            ap = ap[:, 0:n]
        if len(shape_free) > 1:
            names = " ".join(f"a{i}" for i in range(len(shape_free)))
            kw = {f"a{i}": int(s) for i, s in enumerate(shape_free)}
            ap = ap.rearrange(f"p ({names}) -> p {names}", **kw)
        return ap


D = 1024
KC = 8
NEXP = 16
DEXP = 512
DPROJ = 3584
EPS = 1e-6
TWO_PI = 2.0 * math.pi


def host_consts(L, LC):
    bf = ml_dtypes.bfloat16
    c = {}
    rows = L // 64
    row = np.repeat(np.arange(rows, dtype=np.float32), 64)
    col = np.tile(np.arange(64, dtype=np.float32), rows)
    quarter = D // 4
    omega = (1.0 / (10000.0 ** (np.arange(quarter, dtype=np.float32) / quarter))).astype(np.float32)
    ar = row[:, None] * omega
    ac = col[:, None] * omega
    c["pos"] = np.concatenate([np.sin(ar), np.cos(ar), np.sin(ac), np.cos(ac)], axis=-1).astype(np.float32)
    c["identb"] = np.eye(128).astype(bf)
    c["identf"] = np.eye(128).astype(np.float32)
    s = np.arange(128)[:, None]
    t = np.arange(128)[None, :]
    same = (s // 32) == (t // 32)
    c["maskf"] = (same & (s <= t)).astype(np.float32)
    c["maskb"] = (same & (s >= t)).astype(np.float32)
    c["rowm"] = (np.arange(128)[:, None] // 32 == np.arange(4)[None, :]).astype(np.float32)
    deltas = np.abs(np.linspace(math.log(1e-2) / 1.5, math.log(1e-2) / 0.3, 256, dtype=np.float32))
    m = np.arange(64)
    c64 = np.cos(2 * np.pi * np.outer(m, m) / 64) / 8.0
    s64 = -np.sin(2 * np.pi * np.outer(m, m) / 64) / 8.0
    z = np.zeros((64, 64))
    c["bdc"] = np.block([[c64, z], [z, c64]]).astype(np.float32)
    c["bds"] = np.block([[s64, z], [z, s64]]).astype(np.float32)
    for tag, n in (("l", L), ("c", LC)):
        nt = n // 128
        tt = np.linspace(0.0, 1.0, n, dtype=np.float32)[:, None]
        w = (2.0 * math.pi * np.arange(n, dtype=np.float32)[:, None] / n).astype(np.float32)
        bands = np.linspace(1e-4, 15, 16, dtype=np.float32)[None, :]
        zp = np.concatenate([tt, np.cos(bands * w), -np.sin(bands * w)], axis=-1).astype(np.float32)
        c["zpos" + tag] = np.ascontiguousarray(zp.T)
        dec = np.exp(-tt * deltas[None, :]).astype(np.float32)
        dec0 = dec.copy()
        dec0[0, :] = 0.0
        c["dec" + tag] = np.ascontiguousarray(np.concatenate([dec, dec0, dec, dec0], axis=1))
        k = np.arange(n, dtype=np.float64)
        tq = np.arange(n, dtype=np.float64)
        ang = 2 * np.pi * np.outer(tq, k + 0.5) / (2 * n)
        Fc = np.cos(ang)
        Fs = np.sin(ang)

        def tile_fwd(M):
            return M.reshape(nt, 128, nt, 128).transpose(2, 1, 0, 3)

        hyf = np.stack([tile_fwd(Fc), tile_fwd(Fs)], axis=2)
        c["hyf" + tag] = np.ascontiguousarray(hyf).astype(bf)
        Ic = (Fc.T / n)
        Is = (-Fs.T / n)
        hyi = np.stack([tile_fwd(Ic), tile_fwd(Is)], axis=2)
        c["hyi" + tag] = np.ascontiguousarray(hyi).astype(bf)
        ang2 = 2 * np.pi * np.outer(tq, k) / n
        fnf = np.stack([tile_fwd(np.cos(ang2) / math.sqrt(n)), tile_fwd(np.sin(ang2) / math.sqrt(n))], axis=2)
        c["fnf" + tag] = np.ascontiguousarray(fnf).astype(bf)
    return c


WEIGHT_SHAPES = {
    "ada_w": (2, D, 6 * D), "ada_b": (2, 6 * D), "norm1_g": (2, D), "norm2_g": (2, D),
    "w_in": (2, D, DPROJ), "w_out": (2, D, D), "hy_conv_w": (2, 3, 768), "hy_conv_b": (2, 768),
    "hy_filt_w1": (2, 33, 64), "hy_filt_b1": (2, 64), "hy_filt_w2": (2, 64, 64), "hy_filt_b2": (2, 64),
    "hy_filt_w3": (2, 64, 1024), "hy_filt_freq": (2, 64), "hy_bias": (2, 2, 256), "hy_norm_g": (2, 256),
    "hg_lower_bounds": (2, 2, 512), "hg_norm_g": (2, 512), "fn_w": (2, 4, 64, 64), "fn_b": (2, 256),
    "fn_norm_g": (2, 256), "router_w": (D, NEXP), "router_b": (NEXP,),
    "moe_w_gate": (2, NEXP, D, DEXP), "moe_w_up": (2, NEXP, D, DEXP), "moe_w_down": (2, NEXP, DEXP, D),
    "final_norm_g": (D,),
}


DEBUG = False
STOP_AFTER = None
HALF_EXP = False


def build(NB, L, LC, NL=2, arena_elems=53000):
    T = LC + L
    NT = T // 128
    NTC = LC // 128
    NTL = L // 128
    NCH = T // 32
    NCHC = LC // 32
    hc = host_consts(L, LC)
    nc = bass.Bass("TRN2", target_bir_lowering=False)

    def din(name, shape, dt=F32):
        return nc.dram_tensor(name, list(shape), dt, kind="ExternalInput").ap()

    def dscr(name, shape, dt=F32):
        return nc.dram_tensor(name, list(shape), dt, kind="Internal").ap()

    x_d = din("x", [NB, L, D])
    c_d = din("c", [NB, D])
    ctx_d = din("ctx", [NB, LC, D])
    cctx_d = din("c_ctx", [D])
    W = {k: din(k, v) for k, v in WEIGHT_SHAPES.items()}
    C = {k: din("k_" + k, v.shape, BF16 if v.dtype == ml_dtypes.bfloat16 else F32) for k, v in hc.items()}
    out_d = nc.dram_tensor("out", [NB, L, D], F32, kind="ExternalOutput").ap()

    xres = nc.dram_tensor("xres", [NB, T, D], F32, kind="ExternalOutput").ap() if DEBUG else dscr("xres", [NB, T, D])
    gate_d = dscr("gate_rows", [NL, 3, 2, D])
    kk_d = {"l": dscr("kk_l", [NL, NTL, 128, 2, 2, 256], BF16), "c": dscr("kk_c", [NL, NTC, 128, 2, 2, 256], BF16)}
    yT_d = nc.dram_tensor("yT", [128, 8, T], BF16, kind="ExternalOutput").ap() if DEBUG else dscr("yT", [128, 8, T], BF16)

    if DEBUG:
        dbg_acc = nc.dram_tensor("dbg_acc", [128, 16, D], F32, kind="ExternalOutput").ap()
        dbg_comb = nc.dram_tensor("dbg_comb", [128, 16, NEXP], F32, kind="ExternalOutput").ap()
        dbg_h = nc.dram_tensor("dbg_h", [128, KC, 256], BF16, kind="ExternalOutput").ap()
    es = ExitStack()
    S = Sched(nc)
    A = Arena(nc, es, arena_elems)
    PS = [es.enter_context(nc.psum_tensor(f"ps{i}", [128, 512], F32)) for i in range(8)]
    PT = [Trk() for _ in range(8)]

    def MM(out, lhsT, rhs, st, sp, R, Wt):
        S.op("pe", lambda e: e.matmul(out, lhsT=lhsT, rhs=rhs, start=st, stop=sp), reads=R, writes=Wt)

    def TRP(out, in_, ident, R, Wt):
        S.op("pe", lambda e: e.transpose(out=out, in_=in_, identity=ident), reads=R, writes=Wt)

    def ACT(out, in_, func, R, Wt, scale=1.0, bias=0.0, accum=None):
        if accum is None:
            S.op("act", lambda e: e.activation(out=out, in_=in_, func=func, scale=scale, bias=bias), reads=R, writes=Wt)
        else:
            S.op("act", lambda e: e.activation(out=out, in_=in_, func=func, scale=scale, bias=bias, accum_out=accum),
                 reads=R, writes=Wt)

    def TT(eng, out, a, b, op, R, Wt):
        S.op(eng, lambda e: e.tensor_tensor(out=out, in0=a, in1=b, op=op), reads=R, writes=Wt)

    def TS(eng, out, a, s1, s2, op0, op1, R, Wt):
        if s2 is None:
            S.op(eng, lambda e: e.tensor_scalar(out=out, in0=a, scalar1=s1, scalar2=None, op0=op0), reads=R, writes=Wt)
        else:
            S.op(eng, lambda e: e.tensor_scalar(out=out, in0=a, scalar1=s1, scalar2=s2, op0=op0, op1=op1),
                 reads=R, writes=Wt)

    def STT(eng, out, a, sc, b, op0, op1, R, Wt):
        S.op(eng, lambda e: e.scalar_tensor_tensor(out=out, in0=a, scalar=sc, in1=b, op0=op0, op1=op1),
             reads=R, writes=Wt)

    def CP(eng, out, in_, R, Wt):
        if eng == "act":
            S.op("act", lambda e: e.copy(out=out, in_=in_), reads=R, writes=Wt)
        else:
            S.op(eng, lambda e: e.tensor_copy(out=out, in_=in_), reads=R, writes=Wt)

    def MSET(eng, ap, val, Wt):
        S.op(eng, lambda e: e.memset(ap, val), writes=Wt)

    def RED(out, in_, op, R, Wt):
        S.op("dve", lambda e: e.tensor_reduce(out=out, in_=in_, axis=AX.X, op=op), reads=R, writes=Wt)

    def SCAN(out, d0, d1, R, Wt):
        S.op("dve", lambda e: e.tensor_tensor_scan(out=out, data0=d0, data1=d1, initial=0.0, op0=ALU.mult, op1=ALU.add),
             reads=R, writes=Wt)

    def RCP(out, in_, R, Wt):
        S.op("dve", lambda e: e.reciprocal(out=out, in_=in_), reads=R, writes=Wt)

    def DMA(q, out, in_, R, Wt, slow=False):
        if slow:
            S.op(q, lambda e: e.dma_start(out=out, in_=in_, allow_slow_non_contiguous=True), reads=R, writes=Wt, dma=True)
        else:
            S.op(q, lambda e: e.dma_start(out=out, in_=in_), reads=R, writes=Wt, dma=True)

    rr = {"ps": 0}

    def evac_eng(i):
        return "act" if i % 2 == 0 else "dve"

    identb = A.alloc([128], BF16); t_identb = Trk()
    identf = A.alloc([128]); t_identf = Trk()
    maskf = A.alloc([128]); maskb = A.alloc([128]); t_mask = Trk()
    rowm = A.alloc([4])
    scanm = A.alloc([T]); t_scanm = Trk()
    scT = A.alloc([KC, 3], BF16); t_scT = Trk()
    stg = A.alloc([128]); t_stg = Trk()
    vecT = A.alloc([128]); t_vec = Trk()
    lbT = A.alloc([3, 8]); t_lb = Trk()
    modT = A.alloc([48, 3]); t_mod = Trk()
    GS = A.alloc([4, KC, 3]); t_GS = Trk()
    filtp = A.alloc([4]); t_filtp = Trk()
    bda = A.alloc([2, 128], BF16); bdb = A.alloc([2, 128], BF16); t_bd = Trk()
    rw_sb = A.alloc([KC, NEXP], BF16); t_rw = Trk()
    rb_sb = A.alloc([NEXP]); t_rb = Trk()
    fng_sb = A.alloc([D]); t_fng = Trk()
    small = A.alloc([64]); t_small = Trk()

    DMA("sp", identb, C["identb"], [], [t_identb])
    DMA("sp", identf, C["identf"], [], [t_identf])
    DMA("sp", maskf, C["maskf"], [], [t_mask])
    DMA("sp", maskb, C["maskb"], [], [t_mask])
    DMA("sp", rowm, C["rowm"], [], [t_mask])
    MSET("pool", scanm, 1.0, [t_scanm])
    MSET("pool", scanm.rearrange("p (c j) -> p c j", j=32)[:, :, 0:1], 0.0, [t_scanm])
    MSET("pool", stg, 0.0, [t_stg])
    DMA("pool", rw_sb, W["router_w"].rearrange("(kc p) e -> p kc e", p=128), [], [t_rw])
    DMA("sp", rb_sb, W["router_b"].partition_broadcast(128), [], [t_rb])
    DMA("sp", fng_sb, W["final_norm_g"].partition_broadcast(128), [], [t_fng])
    m0 = A.mark()
    cf = A.alloc([KC, 3]); t_cf = Trk()
    for r in range(3):
        src = c_d[r] if r < NB else cctx_d
        DMA("sp", cf[:, :, r], src.rearrange("(kc p) -> p kc", p=128), [], [t_cf], slow=True)
    ACT(scT, cf, AF.Silu, [t_cf], [t_scT])
    S.barrier()
    A.release(m0)
    PERSIST = A.mark()

    def cond_row(b, is_ctx):
        return 2 if is_ctx else b

    def layer_setup(l):
        m0 = A.mark()
        def ld(r0, nr, src):
            DMA("sp", stg[r0:r0 + nr, :], src, [], [t_stg])
        ld(0, 8, W["norm1_g"][l].rearrange("(r c) -> r c", c=128))
        ld(8, 8, W["norm2_g"][l].rearrange("(r c) -> r c", c=128))
        ld(16, 18, W["hy_conv_w"][l].rearrange("k (j c) -> (k j) c", c=128))
        ld(34, 6, W["hy_conv_b"][l].rearrange("(r c) -> r c", c=128))
        ld(40, 4, W["hg_norm_g"][l].rearrange("(r c) -> r c", c=128))
        ld(44, 48, W["ada_b"][l].rearrange("(r c) -> r c", c=128))
        ld(92, 2, W["hy_norm_g"][l].rearrange("(r c) -> r c", c=128))
        ld(94, 2, W["fn_norm_g"][l].rearrange("(r c) -> r c", c=128))
        ld(96, 8, W["hg_lower_bounds"][0].rearrange("d (h c) -> (d h) c", c=128))
        ld(104, 8, W["hg_lower_bounds"][1].rearrange("d (h c) -> (d h) c", c=128))
        TRP(PS[0][:, 0:128], stg, identf, [t_stg, t_identf], [PT[0]])
        CP("dve", vecT, PS[0][:, 0:128], [PT[0]], [t_vec])
        if l == 0:
            MSET("pool", lbT[:, 0, :], 0.0, [t_lb])
        else:
            TT("dve", small[:, 0:8], vecT[:, 104:112], vecT[:, 96:104], ALU.subtract, [t_vec], [t_small])
            ACT(lbT[:, 0, :], small[:, 0:8], AF.Sigmoid, [t_small], [t_lb])
        TS("dve", lbT[:, 1, :], lbT[:, 0, :], -1.0, 1.0, ALU.mult, ALU.add, [t_lb], [t_lb])
        TS("dve", lbT[:, 2, :], lbT[:, 1, :], -1.0, None, ALU.mult, None, [t_lb], [t_lb])
        m_mod = A.mark()
        screp = A.alloc([KC, 3, 128], BF16); t_screp = Trk()
        CP("dve", screp, scT.unsqueeze(3).to_broadcast([128, KC, 3, 128]), [t_scT], [t_screp])
        wa = [A.alloc([KC, 768], BF16) for _ in range(2)]
        t_wa = [Trk(), Trk()]
        for gq in range(8):
            bi = gq % 2
            DMA("pool", wa[bi], W["ada_w"][l, :, gq * 768:(gq + 1) * 768].rearrange("(kc p) n -> p kc n", p=128), [], [t_wa[bi]])
            for o6 in range(6):
                oc = gq * 6 + o6
                for kc in range(KC):
                    MM(PS[1][:, oc * 3:(oc + 1) * 3], wa[bi][:, kc, o6 * 128:(o6 + 1) * 128], scT[:, kc, :], kc == 0, kc == KC - 1,
                       [t_wa[bi], t_scT], [PT[1]])
        TT("dve", modT, PS[1][:, 0:144].rearrange("p (o r) -> p o r", r=3),
           vecT[:, 44:92].unsqueeze(2).to_broadcast([128, 48, 3]), ALU.add, [PT[1], t_vec], [t_mod])
        for which, (gsl, m_shift, m_scale) in enumerate(((slice(0, 8), 0, 1), (slice(8, 16), 3, 4))):
            gi = 2 * which
            TS("dve", GS[:, gi], modT[:, m_scale * 8:(m_scale + 1) * 8, :], 1.0, None, ALU.add, None, [t_mod], [t_GS])
            TT("dve", GS[:, gi], GS[:, gi], vecT[:, gsl].unsqueeze(2).to_broadcast([128, 8, 3]), ALU.mult, [t_GS, t_vec], [t_GS])
            CP("dve", GS[:, gi + 1], modT[:, m_shift * 8:(m_shift + 1) * 8, :], [t_mod], [t_GS])
        wg = A.alloc([KC, 2, D], BF16); t_wg = Trk()
        for g, m in enumerate((2, 5)):
            for kc in range(KC):
                DMA("pool", wg[:, kc, g, :], W["ada_w"][l, kc * 128:(kc + 1) * 128, m * D:(m + 1) * D], [], [t_wg])
        gb = A.alloc([2, D]); t_gb = Trk()
        for g, m in enumerate((2, 5)):
            DMA("sp", gb[:, g, :], W["ada_b"][l, m * D:(m + 1) * D].partition_broadcast(128), [], [t_gb])
        grow = A.alloc([D]); t_grow = Trk()
        for r in range(3):
            for g in range(2):
                for hh in range(2):
                    pb = 2 + hh
                    for kc in range(KC):
                        MM(PS[pb][:, :], screp[:, kc, r, :], wg[:, kc, g, hh * 512:(hh + 1) * 512], kc == 0, kc == KC - 1,
                           [t_screp, t_wg], [PT[pb]])
                    TT("dve", grow[:, hh * 512:(hh + 1) * 512], PS[pb][:, :], gb[:, g, hh * 512:(hh + 1) * 512], ALU.add,
                       [PT[pb], t_gb], [t_grow])
                DMA("sp", gate_d[l, r, g:g + 1, :], grow[0:1, :], [t_grow], [])
        S.barrier()
        A.release(m_mod)
        DMA("sp", filtp[0:64, 0:1], W["hy_filt_freq"][l].rearrange("(p o) -> p o", o=1), [], [t_filtp])
        DMA("sp", filtp[0:64, 1:2], W["hy_filt_b1"][l].rearrange("(p o) -> p o", o=1), [], [t_filtp])
        DMA("sp", filtp[0:64, 2:3], W["hy_filt_b2"][l].rearrange("(p o) -> p o", o=1), [], [t_filtp])
        w1 = A.alloc([64]); w2 = A.alloc([64]); w3 = A.alloc([1024]); t_fw = Trk()
        DMA("sp", w1[0:33, :], W["hy_filt_w1"][l], [], [t_fw])
        DMA("sp", w2[0:64, :], W["hy_filt_w2"][l], [], [t_fw])
        DMA("sp", w3[0:64, :], W["hy_filt_w3"][l], [], [t_fw])
        biasz = A.alloc([1024]); t_bz = Trk()
        MSET("pool", biasz, 0.0, [t_bz])
        for o in range(2):
            DMA("sp", biasz[0:1, (2 * o) * 256:(2 * o) * 256 + 256], W["hy_bias"][l, o:o + 1, :], [], [t_bz])
        variants = [("l", L, NTL)] + ([("c", LC, NTC)] if l == 0 else [])
        for tag, n, nt in variants:
            m1 = A.mark()
            zp = A.alloc([n]); t_zp = Trk()
            DMA("sp", zp[0:33, :], C["zpos" + tag], [], [t_zp])
            h1 = A.alloc([n]); h2 = A.alloc([n]); t_h1 = Trk(); t_h2 = Trk()
            targ = A.alloc([512]); t_targ = Trk()
            tsn = A.alloc([512]); tcs = A.alloc([512]); tq = A.alloc([512]); t_tsn = Trk()
            fpi = A.alloc([8]); MSET("pool", fpi, math.pi / 2, [t_tsn])
            nblk = (n + 511) // 512
            for stage in range(2):
                src, t_src, dst, t_dst, wmat, kk_, bcol = ((zp, t_zp, h1, t_h1, w1, 33, 1), (h1, t_h1, h2, t_h2, w2, 64, 2))[stage]
                for bk in range(nblk):
                    c0 = bk * 512
                    cw = min(512, n - c0)
                    pb = 4 + bk % 2
                    MM(PS[pb][0:64, 0:cw], wmat[0:kk_, :], src[0:kk_, c0:c0 + cw], True, True, [t_fw, t_src], [PT[pb]])
                    TS("dve", targ[0:64, 0:cw], PS[pb][0:64, 0:cw], filtp[0:64, bcol:bcol + 1], filtp[0:64, 0:1], ALU.add, ALU.mult,
                       [PT[pb], t_filtp], [t_targ])
                    a_ = targ[0:64, 0:cw]
                    s_ = tsn[0:64, 0:cw]; c_ = tcs[0:64, 0:cw]; q_ = tq[0:64, 0:cw]
                    ACT(s_, a_, AF.Sin, [t_targ], [t_tsn], scale=0.125)
                    ACT(c_, a_, AF.Sin, [t_targ], [t_tsn], scale=0.125, bias=fpi[0:64, 0:1])
                    for rep in range(3):
                        STT("dve", q_, s_, 2.0, c_, ALU.mult, ALU.mult, [t_tsn], [t_tsn])
                        TT("dve", c_, s_, s_, ALU.mult, [t_tsn], [t_tsn])
                        TS("dve", c_, c_, -2.0, 1.0, ALU.mult, ALU.add, [t_tsn], [t_tsn])
                        if rep < 2:
                            CP("dve", s_, q_, [t_tsn], [t_tsn])
                    CP("dve", dst[0:64, c0:c0 + cw], q_, [t_tsn], [t_dst])
            HS = A.alloc([nt, 512], BF16); HD = A.alloc([nt, 512], BF16); t_HS = Trk()
            dcy = [A.alloc([1024]) for _ in range(2)]; t_dcy = [Trk(), Trk()]
            Hd = A.alloc([1024]); t_Hd = Trk()
            for jc in range(nt):
                bi = jc % 2
                DMA("sp", dcy[bi], C["dec" + tag][jc * 128:(jc + 1) * 128, :], [], [t_dcy[bi]])
                for hh in range(2):
                    pb = 4 + hh
                    MM(PS[pb][:, :], h2[0:64, jc * 128:(jc + 1) * 128], w3[0:64, hh * 512:(hh + 1) * 512], True, True,
                       [t_h2, t_fw], [PT[pb]])
                    TT("dve", Hd[:, hh * 512:(hh + 1) * 512], PS[pb][:, :], dcy[bi][:, hh * 512:(hh + 1) * 512], ALU.mult,
                       [PT[pb], t_dcy[bi]], [t_Hd])
                if jc == 0:
                    TT("dve", Hd, Hd, biasz, ALU.add, [t_Hd, t_bz], [t_Hd])
                Hv = Hd.rearrange("p (o d c) -> p o d c", o=2, d=2)
                TT("dve", HS[:, jc, :].rearrange("p (o c) -> p o c", o=2), Hv[:, :, 0, :], Hv[:, :, 1, :], ALU.add, [t_Hd], [t_HS])
                TT("dve", HD[:, jc, :].rearrange("p (o c) -> p o c", o=2), Hv[:, :, 1, :], Hv[:, :, 0, :], ALU.subtract, [t_Hd], [t_HS])
            FB = [A.alloc([2, nt, 128], BF16) for _ in range(2)]; t_FB = [Trk(), Trk()]
            KKs = [A.alloc([2, 512], BF16) for _ in range(2)]; t_KK = [Trk(), Trk()]
            for kc in range(nt):
                bi = kc % 2
                DMA("sp", FB[bi], C["hyf" + tag][kc], [], [t_FB[bi]])
                for ri, Hx in enumerate((HS, HD)):
                    pb = 6 + ri
                    for jc in range(nt):
                        MM(PS[pb][:, :], FB[bi][:, ri, jc, :], Hx[:, jc, :], jc == 0, jc == nt - 1, [t_FB[bi], t_HS], [PT[pb]])
                    CP("act" if ri == 0 else "dve", KKs[bi][:, ri, :], PS[pb][:, :], [PT[pb]], [t_KK[bi]])
                DMA("sp", kk_d[tag][l, kc].rearrange("p r o c -> p r (o c)"), KKs[bi], [t_KK[bi]], [])
            S.barrier()
            A.release(m1)
        wst = A.alloc([2, 64]); t_wst = Trk()
        DMA("sp", wst, W["fn_w"][l].rearrange("(gp g) m d -> (g m) gp d", g=2), [], [t_wst])
        bdc = A.alloc([128]); bds = A.alloc([128]); t_bdc = Trk()
        DMA("sp", bdc, C["bdc"], [], [t_bdc])
        DMA("sp", bds, C["bds"], [], [t_bdc])
        MSET("pool", bda, 0.0, [t_bd])
        MSET("pool", bdb, 0.0, [t_bd])
        for which, (mat, dst) in enumerate(((bdc, bda), (bds, bdb))):
            for gp in range(2):
                pb = 4 + gp
                MM(PS[pb][:, 0:64], mat, wst[:, gp, :], True, True, [t_bdc, t_wst], [PT[pb]])
                CP("dve", dst[0:64, gp, 0:64], PS[pb][0:64, 0:64], [PT[pb]], [t_bd])
                CP("dve", dst[64:128, gp, 64:128], PS[pb][64:128, 0:64], [PT[pb]], [t_bd])
        S.barrier()
        A.release(m0)

    def rms_rstd(ss_ap, n, R, out_ap, t_out):
        ACT(out_ap, ss_ap, AF.Sqrt, R, [t_out], scale=1.0 / n, bias=EPS)
        RCP(out_ap, out_ap, [t_out], [t_out])

    evt = A.alloc([KC, 128]); t_evt = Trk()

    def norm_mod_transpose(xt, t_xt, hT, t_hT, col0, gi, r, bufs, i):
        junk, t_junk, ssb, t_ss, xn, t_xn = bufs
        ACT(junk, xt, AF.Square, [t_xt], [t_junk, t_ss], accum=ssb[:, 0:1])
        rms_rstd(ssb[:, 0:1], D, [t_ss], ssb[:, 1:2], t_ss)
        TS("dve", xn, xt, ssb[:, 1:2], None, ALU.mult, None, [t_xt, t_ss], [t_xn])
        pb = i % 2
        pv = PS[pb][:, :].bitcast(BF16)
        for kc in range(KC):
            TRP(pv[:, kc * 128:(kc + 1) * 128], xn[:, kc * 128:(kc + 1) * 128], identb, [t_xn, t_identb], [PT[pb]])
        pv3 = pv.rearrange("p (k c) -> p k c", k=KC)
        TT("dve", evt, pv3, GS[:, gi, :, r:r + 1].to_broadcast([128, KC, 128]), ALU.mult, [PT[pb], t_GS], [t_evt])
        TT("dve", hT[:, :, col0:col0 + 128], evt, GS[:, gi + 1, :, r:r + 1].to_broadcast([128, KC, 128]), ALU.add,
           [t_evt, t_GS], [t_hT])

    def col_groups(c0, c1):
        g = []
        c = c0
        while c < c1:
            w = min(512, c1 - c)
            g.append((c, w))
            c += w
        return g

    def win_chunks(l, hT, t_hT, cols_list, evac, c0=0, c1=None):
        c1 = T if c1 is None else c1
        n = len(cols_list)
        wb = A.alloc([KC, n, 128], BF16); t_wb = Trk()
        for j, cc in enumerate(cols_list):
            DMA("pool", wb[:, :, j, :], W["w_in"][l, :, cc:cc + 128].rearrange("(kc p) n -> p kc n", p=128), [], [t_wb])
        for j in range(n):
            for (g0, gw) in col_groups(c0, c1):
                pb = rr["ps"] % 4
                rr["ps"] += 1
                for kc in range(KC):
                    MM(PS[pb][:, 0:gw], wb[:, kc, j, :], hT[:, kc, g0:g0 + gw], kc == 0, kc == KC - 1, [t_wb, t_hT], [PT[pb]])
                evac(j, g0, gw, PS[pb][:, 0:gw], PT[pb])

    def mixer_sublayer(l, b, hT, t_hT):
        last = (l == NL - 1)
        m0 = A.mark()
        xt = [A.alloc([D]) for _ in range(2)]; t_xt = [Trk(), Trk()]
        pt_ = [A.alloc([D]) for _ in range(2)]; t_pt = [Trk(), Trk()]
        junk = [A.alloc([D], BF16) for _ in range(2)]; t_junk = [Trk(), Trk()]
        ssb = [A.alloc([8]) for _ in range(2)]; t_ss = [Trk(), Trk()]
        xn = [A.alloc([D], BF16) for _ in range(2)]; t_xn = [Trk(), Trk()]
        for i in range(NT):
            bi = i % 2
            is_ctx = i < NTC
            if l == 0:
                if is_ctx:
                    DMA("sp", xt[bi], ctx_d[b, i * 128:(i + 1) * 128, :], [], [t_xt[bi]])
                else:
                    j = i - NTC
                    DMA("sp", xt[bi], x_d[b, j * 128:(j + 1) * 128, :], [], [t_xt[bi]])
                    DMA("sp", pt_[bi], C["pos"][j * 128:(j + 1) * 128, :], [], [t_pt[bi]])
                    TT("dve", xt[bi], xt[bi], pt_[bi], ALU.add, [t_xt[bi], t_pt[bi]], [t_xt[bi]])
                DMA("sp", xres[b, i * 128:(i + 1) * 128, :], xt[bi], [t_xt[bi]], [])
            else:
                DMA("sp", xt[bi], xres[b, i * 128:(i + 1) * 128, :], [], [t_xt[bi]])
            norm_mod_transpose(xt[bi], t_xt[bi], hT, t_hT, i * 128, 0, cond_row(b, is_ctx),
                               (junk[bi], t_junk[bi], ssb[bi], t_ss[bi], xn[bi], t_xn[bi]), i)
        S.barrier()
        A.release(m0)
        segs = [("l", NTC, NTL)] + ([("c", 0, NTC)] if not last else [])
        mix_c0 = 0 if not last else LC

        m0 = A.mark()
        utok = A.alloc([NT, 768], BF16); t_utok = Trk()
        mmid = A.mark()
        u = A.alloc([6, T], BF16); t_u = Trk()
        zhy = A.alloc([6, T], BF16); t_zhy = Trk()

        def ev_hy(j, g0, gw, ps, pt):
            CP(evac_eng(j), zhy[:, j, g0:g0 + gw], ps, [pt], [t_zhy])
        win_chunks(l, hT, t_hT, [j * 128 for j in range(6)], ev_hy, mix_c0, T)
        ut = A.alloc([T]); t_ut = Trk()
        for j in range(6):
            for tag, t0, nt in segs:
                a0, a1 = t0 * 128, (t0 + nt) * 128
                ACT(ut[:, a0:a1], zhy[:, j, a0:a1], AF.Identity, [t_zhy, t_vec], [t_ut],
                    scale=vecT[:, 16 + 6 + j:16 + 6 + j + 1], bias=vecT[:, 34 + j:35 + j])
                STT("dve", ut[:, a0 + 1:a1], zhy[:, j, a0:a1 - 1], vecT[:, 16 + j:17 + j], ut[:, a0 + 1:a1],
                    ALU.mult, ALU.add, [t_zhy, t_vec, t_ut], [t_ut])
                STT("dve", u[:, j, a0:a1 - 1], zhy[:, j, a0 + 1:a1], vecT[:, 16 + 12 + j:16 + 13 + j], ut[:, a0:a1 - 1],
                    ALU.mult, ALU.add, [t_zhy, t_vec, t_ut], [t_u])
                CP("pool", u[:, j, a1 - 1:a1], ut[:, a1 - 1:a1], [t_ut], [t_u])
        for i in range(mix_c0 // 128, NT):
            pb = i % 2
            pv = PS[pb][:, :].bitcast(BF16)
            for j in range(6):
                TRP(pv[:, j * 128:(j + 1) * 128], u[:, j, i * 128:(i + 1) * 128], identb, [t_u, t_identb], [PT[pb]])
            CP(evac_eng(i), utok[:, i, :], pv[:, 0:768], [PT[pb]], [t_utok])
        S.barrier()
        A.release(mmid)
        ynT = A.alloc([2, T], BF16); t_ynT = Trk()
        for tag, t0, nt in segs:
            m1 = A.mark()
            n = nt * 128
            KK = A.alloc([nt, 2, 256], BF16); t_KKl = Trk()
            FB = [A.alloc([2, nt, 128], BF16) for _ in range(2)]; t_FB = [Trk(), Trk()]
            Pr = A.alloc([nt, 256], BF16); Pi = A.alloc([nt, 256], BF16); t_P = Trk()
            y1 = A.alloc([nt, 256], BF16); t_y1 = Trk()
            tmp = [A.alloc([4, 256]) for _ in range(2)]; t_tmp = [Trk(), Trk()]
            y2 = A.alloc([256]); t_y2 = Trk()
            yn = A.alloc([256], BF16); t_yn = Trk()
            junk2 = A.alloc([256], BF16); t_j2 = Trk()
            ss2 = A.alloc([8]); t_ss2 = Trk()
            for order in range(2):
                for r_ in range(2):
                    DMA("sp", KK[:, :, r_, :], kk_d[tag][l, :, :, r_, order, :].rearrange("k p c -> p k c"), [], [t_KKl])

                def src(tc):
                    if order == 0:
                        return utok[:, t0 + tc, 0:256], t_utok
                    return y1[:, tc, :], t_y1
                for kc in range(nt):
                    bi = kc % 2
                    DMA("sp", FB[bi], C["hyf" + tag][kc], [], [t_FB[bi]])
                    pr_, pi_ = 4 + 2 * bi, 5 + 2 * bi
                    for ri, pb in ((0, pr_), (1, pi_)):
                        for tc in range(nt):
                            s_ap, s_t = src(tc)
                            MM(PS[pb][:, 0:256], FB[bi][:, ri, tc, :], s_ap, tc == 0, tc == nt - 1, [t_FB[bi], s_t], [PT[pb]])
                    tb = tmp[bi]
                    Kr = KK[:, kc, 0, :]
                    Ki = KK[:, kc, 1, :]
                    TT("dve", tb[:, 0, :], PS[pr_][:, 0:256], Kr, ALU.mult, [PT[pr_], t_KKl], [t_tmp[bi]])
                    TT("dve", tb[:, 1, :], PS[pi_][:, 0:256], Ki, ALU.mult, [PT[pi_], t_KKl], [t_tmp[bi]])
                    TT("dve", tb[:, 2, :], PS[pr_][:, 0:256], Ki, ALU.mult, [PT[pr_], t_KKl], [t_tmp[bi]])
                    TT("dve", tb[:, 3, :], PS[pi_][:, 0:256], Kr, ALU.mult, [PT[pi_], t_KKl], [t_tmp[bi]])
                    TT("pool", Pr[:, kc, :], tb[:, 0, :], tb[:, 1, :], ALU.add, [t_tmp[bi]], [t_P])
                    TT("pool", Pi[:, kc, :], tb[:, 2, :], tb[:, 3, :], ALU.subtract, [t_tmp[bi]], [t_P])
                for tc in range(nt):
                    bi = tc % 2
                    DMA("sp", FB[bi], C["hyi" + tag][tc], [], [t_FB[bi]])
                    pb = 4 + bi
                    for kc in range(nt):
                        MM(PS[pb][:, 0:256], FB[bi][:, 0, kc, :], Pr[:, kc, :], kc == 0, False, [t_FB[bi], t_P], [PT[pb]])
                        MM(PS[pb][:, 0:256], FB[bi][:, 1, kc, :], Pi[:, kc, :], False, kc == nt - 1, [t_FB[bi], t_P], [PT[pb]])
                    if order == 0:
                        TT("dve", y1[:, tc, :], PS[pb][:, 0:256], utok[:, t0 + tc, 256:512], ALU.mult, [PT[pb], t_utok], [t_y1])
                    else:
                        TT("dve", y2, PS[pb][:, 0:256], utok[:, t0 + tc, 512:768], ALU.mult, [PT[pb], t_utok], [t_y2])
                        ACT(junk2, y2, AF.Square, [t_y2], [t_j2, t_ss2], accum=ss2[:, 0:1])
                        rms_rstd(ss2[:, 0:1], 256, [t_ss2], ss2[:, 1:2], t_ss2)
                        TS("dve", yn, y2, ss2[:, 1:2], None, ALU.mult, None, [t_y2, t_ss2], [t_yn])
                        pq = 6 + bi
                        pv = PS[pq][:, :].bitcast(BF16)
                        for j in range(2):
                            TRP(pv[:, j * 128:(j + 1) * 128], yn[:, j * 128:(j + 1) * 128], identb, [t_yn, t_identb], [PT[pq]])
                        col = (t0 + tc) * 128
                        for j in range(2):
                            TS("dve", ynT[:, j, col:col + 128], pv[:, j * 128:(j + 1) * 128], vecT[:, 92 + j:93 + j], None,
                               ALU.mult, None, [PT[pq], t_vec], [t_ynT])
            S.barrier()
            A.release(m1)
        DMA("sp", yT_d[:, 0:2, mix_c0:T], ynT[:, :, mix_c0:T], [t_ynT], [])
        S.barrier()
        A.release(m0)

        m0 = A.mark()
        zfn = A.alloc([2, T], BF16); t_zfn = Trk()

        def ev_fn(j, g0, gw, ps, pt):
            CP(evac_eng(j), zfn[:, j, g0:g0 + gw], ps, [pt], [t_zfn])
        win_chunks(l, hT, t_hT, [3328, 3456], ev_fn, mix_c0, T)
        zc = A.alloc([NT, 256], BF16); zs = A.alloc([NT, 256], BF16); t_zcs = Trk()
        for i in range(mix_c0 // 128, NT):
            pa, pb2 = 4 + 2 * (i % 2), 5 + 2 * (i % 2)
            for j in range(2):
                MM(PS[pa][:, j * 128:(j + 1) * 128], zfn[:, j, i * 128:(i + 1) * 128], bda[:, j, :], True, True, [t_zfn, t_bd], [PT[pa]])
                MM(PS[pb2][:, j * 128:(j + 1) * 128], zfn[:, j, i * 128:(i + 1) * 128], bdb[:, j, :], True, True, [t_zfn, t_bd], [PT[pb2]])
            CP("act", zc[:, i, :], PS[pa][:, 0:256], [PT[pa]], [t_zcs])
            CP("dve", zs[:, i, :], PS[pb2][:, 0:256], [PT[pb2]], [t_zcs])
        fnb = A.alloc([256]); t_fnb = Trk()
        DMA("sp", fnb, W["fn_b"][l].partition_broadcast(128), [], [t_fnb])
        ynT = A.alloc([2, T], BF16); t_ynT = Trk()
        y2 = A.alloc([256]); t_y2 = Trk()
        yn = A.alloc([256], BF16); t_yn = Trk()
        junk2 = A.alloc([256], BF16); t_j2 = Trk()
        ss2 = A.alloc([8]); t_ss2 = Trk()
        for tag, t0, nt in segs:
            m1 = A.mark()
            FB = [A.alloc([2, nt, 128], BF16) for _ in range(2)]; t_FB = [Trk(), Trk()]
            for kc in range(nt):
                bi = kc % 2
                DMA("sp", FB[bi], C["fnf" + tag][kc], [], [t_FB[bi]])
                pb = 4 + bi
                for tc in range(nt):
                    MM(PS[pb][:, 0:256], FB[bi][:, 0, tc, :], zc[:, t0 + tc, :], tc == 0, False, [t_FB[bi], t_zcs], [PT[pb]])
                    MM(PS[pb][:, 0:256], FB[bi][:, 1, tc, :], zs[:, t0 + tc, :], False, tc == nt - 1, [t_FB[bi], t_zcs], [PT[pb]])
                TT("dve", y2, PS[pb][:, 0:256], fnb, ALU.add, [PT[pb], t_fnb], [t_y2])
                ACT(junk2, y2, AF.Square, [t_y2], [t_j2, t_ss2], accum=ss2[:, 0:1])
                rms_rstd(ss2[:, 0:1], 256, [t_ss2], ss2[:, 1:2], t_ss2)
                TS("dve", yn, y2, ss2[:, 1:2], None, ALU.mult, None, [t_y2, t_ss2], [t_yn])
                pq = 6 + bi
                pv = PS[pq][:, :].bitcast(BF16)
                for j in range(2):
                    TRP(pv[:, j * 128:(j + 1) * 128], yn[:, j * 128:(j + 1) * 128], identb, [t_yn, t_identb], [PT[pq]])
                col = (t0 + kc) * 128
                for j in range(2):
                    TS("dve", ynT[:, j, col:col + 128], pv[:, j * 128:(j + 1) * 128], vecT[:, 94 + j:95 + j], None,
                       ALU.mult, None, [PT[pq], t_vec], [t_ynT])
            S.barrier()
            A.release(m1)
        DMA("sp", yT_d[:, 6:8, mix_c0:T], ynT[:, :, mix_c0:T], [t_ynT], [])
        S.barrier()
        A.release(m0)

        out_t0 = 0 if not last else NTC
        for h in range(4):
            m0 = A.mark()
            qs = A.alloc([T], BF16); sg = A.alloc([T], BF16); vT = A.alloc([T], BF16)
            t_q = Trk(); t_sg = Trk(); t_vT = Trk()

            def ev_hg(j, g0, gw, ps, pt):
                if j == 0:
                    ACT(qs[:, g0:g0 + gw], ps, AF.Silu, [pt], [t_q])
                elif j == 1:
                    CP("dve", vT[:, g0:g0 + gw], ps, [pt], [t_vT])
                else:
                    ACT(sg[:, g0:g0 + gw], ps, AF.Silu, [pt], [t_sg])
            m1 = A.mark()
            win_chunks(l, hT, t_hT, [768 + 128 * h, 2304 + 128 * h, 2816 + 128 * h], ev_hg)
            S.barrier()
            A.release(m1)
            vtok = A.alloc([NT, 128], BF16); t_vtok = Trk()
            for i in range(NT):
                pb = i % 2
                pv = PS[pb][:, :].bitcast(BF16)
                TRP(pv[:, 0:128], vT[:, i * 128:(i + 1) * 128], identb, [t_vT, t_identb], [PT[pb]])
                CP(evac_eng(i), vtok[:, i, :], pv[:, 0:128], [PT[pb]], [t_vtok])
            sig = A.alloc([T]); t_sig = Trk()
            kkb = A.alloc([T]); bb = A.alloc([T]); xb_ = A.alloc([T])
            t_kk = Trk(); t_bb = Trk(); t_xb = Trk()
            qd = A.alloc([T], BF16); ke = A.alloc([T], BF16); qr = A.alloc([T], BF16)
            KI = [A.alloc([T], BF16) for _ in range(4)]
            Rblk = A.alloc([T // 8]); t_R = Trk()
            t_qd = Trk(); t_kd = Trk(); t_ke = Trk(); t_qr = Trk()
            ebend = A.alloc([NCH]); t_eb = Trk()
            Sall = A.alloc([NCH, 128], BF16); t_Sall = Trk()
            Sm3 = [A.alloc([128]) for _ in range(3)]; t_Sm3 = [Trk(), Trk(), Trk()]
            oacc = A.alloc([NT, 128]); t_oacc = Trk(); t_oaccs = [Trk() for _ in range(NT)]
            ATa = A.alloc([NT, 128], BF16); t_ATa = Trk()
            kzT = [A.alloc([4, 128], BF16) for _ in range(3)]; t_kzT = [Trk(), Trk(), Trk()]
            ona = A.alloc([NT, 128], BF16); t_on = Trk()
            ssa = A.alloc([NT]);
            junk3 = A.alloc([128], BF16); t_j3 = Trk()
            ss3 = A.alloc([8]); t_ss3 = Trk()
            yhT = A.alloc([T], BF16); t_yhT = Trk()
            for d in range(2):
                li = d * 4 + h
                m1 = A.mark()

                def ev_f(j, g0, gw, ps, pt):
                    ACT(sig[:, g0:g0 + gw], ps, AF.Sigmoid, [pt], [t_sig])
                win_chunks(l, hT, t_hT, [1280 + 512 * d + 128 * h], ev_f)
                S.barrier()
                A.release(m1)
                TS("dve", kkb, sig, lbT[:, 2, li:li + 1], lbT[:, 1, li:li + 1], ALU.mult, ALU.add, [t_sig, t_lb], [t_kk])
                ACT(sig, sig, AF.Ln, [t_sig, t_lb], [t_sig], scale=lbT[:, 1, li:li + 1], bias=lbT[:, 0, li:li + 1])
                SCAN(bb, scanm, sig, [t_scanm, t_sig], [t_bb])
                b3 = bb.rearrange("p (c j) -> p c j", j=32)
                x3 = xb_.rearrange("p (c j) -> p c j", j=32)
                k3 = kkb.rearrange("p (c j) -> p c j", j=32)
                b8 = bb.rearrange("p (c j) -> p c j", j=8)
                l8 = sig.rearrange("p (c j) -> p c j", j=8)
                x8 = xb_.rearrange("p (c j) -> p c j", j=8)
                NB8 = T // 8
                if d == 1:
                    TT("dve", xb_, sig, bb, ALU.subtract, [t_sig, t_bb], [t_xb])
                    CP("dve", ebend, b3[:, :, 31], [t_bb], [t_eb])
                    TT("dve", b3, x3, ebend.unsqueeze(2).to_broadcast([128, NCH, 32]), ALU.add, [t_xb, t_eb], [t_bb])
                    endc = 0
                    rc = 7
                else:
                    endc = 31
                    rc = 0
                TT("dve", Rblk, b8[:, :, rc], l8[:, :, rc], ALU.subtract, [t_bb, t_sig], [t_R])
                TT("dve", x8, b8, Rblk.unsqueeze(2).to_broadcast([128, NB8, 8]), ALU.subtract, [t_bb, t_R], [t_xb])
                ACT(xb_, xb_, AF.Exp, [t_xb], [t_xb])
                STT("dve", qr, xb_, float(128 ** -0.5), qs, ALU.mult, ALU.mult, [t_xb, t_q], [t_qr])
                R4 = Rblk.rearrange("p (c i) -> p c i", i=4)
                for I in range(4):
                    lo, hi = (0, 8 * (I + 1)) if d == 0 else (8 * I, 32)
                    w_ = hi - lo
                    MSET("pool", KI[I], 0.0, [t_kd])
                    TT("dve", x3[:, :, lo:hi], R4[:, :, I:I + 1].to_broadcast([128, NCH, w_]), b3[:, :, lo:hi], ALU.subtract,
                       [t_R, t_bb], [t_xb])
                    ACT(x3[:, :, lo:hi], x3[:, :, lo:hi], AF.Exp, [t_xb], [t_xb])
                    TT("dve", KI[I].rearrange("p (c j) -> p c j", j=32)[:, :, lo:hi], x3[:, :, lo:hi], k3[:, :, lo:hi], ALU.mult,
                       [t_xb, t_kk], [t_kd])
                TT("dve", x3, b3[:, :, endc:endc + 1].to_broadcast([128, NCH, 32]), b3, ALU.subtract, [t_bb], [t_xb])
                ACT(xb_, xb_, AF.Exp, [t_xb], [t_xb])
                TT("dve", ke, xb_, kkb, ALU.mult, [t_xb, t_kk], [t_ke])
                ACT(ebend, b3[:, :, endc], AF.Exp, [t_bb], [t_eb])
                ACT(bb, bb, AF.Exp, [t_bb], [t_bb])
                STT("dve", qd, bb, float(128 ** -0.5), qs, ALU.mult, ALU.mult, [t_bb, t_q], [t_qd])
                if d == 0:
                    order = list(range(NCH))
                else:
                    order = list(range(NCHC - 1, -1, -1)) + list(range(NCH - 1, NCHC - 1, -1))
                MSET("pool", Sm3[0], 0.0, [t_Sm3[0]])
                pos_ = 0
                tiles_in_order = []
                for cch in order:
                    if cch // 4 not in tiles_in_order:
                        tiles_in_order.append(cch // 4)
                for n_, i in enumerate(tiles_in_order):
                    bi = n_ % 3
                    pk = 4 + n_ % 2
                    pkv = 6 + n_ % 2
                    pv = PS[pk][:, :].bitcast(BF16)
                    TRP(pv[:, 0:128], ke[:, i * 128:(i + 1) * 128], identb, [t_ke, t_identb], [PT[pk]])
                    for j in range(4):
                        ACT(kzT[bi][:, j, :], pv[:, 0:128], AF.Identity, [PT[pk], t_mask], [t_kzT[bi]], scale=rowm[:, j:j + 1])
                    for j in range(4):
                        MM(PS[pkv][:, j * 128:(j + 1) * 128], kzT[bi][:, j, :], vtok[:, i, :], True, True,
                           [t_kzT[bi], t_vtok], [PT[pkv]])
                    chs = [cch for cch in order if cch // 4 == i]
                    for cch in chs:
                        j = cch % 4
                        a_, b_ = pos_ % 3, (pos_ + 1) % 3
                        pos_ += 1
                        ACT(Sall[:, cch, :], Sm3[a_], AF.Identity, [t_Sm3[a_]], [t_Sall])
                        STT("dve", Sm3[b_], Sm3[a_], ebend[:, cch:cch + 1], PS[pkv][:, j * 128:(j + 1) * 128], ALU.mult, ALU.add,
                            [t_Sm3[a_], t_eb, PT[pkv]], [t_Sm3[b_]])
                msk = maskf if d == 0 else maskb
                for i in range(out_t0, NT):
                    pa = i % 4
                    cs = slice(i * 128, (i + 1) * 128)
                    for I in range(4):
                        MM(PS[pa][:, I * 32:(I + 1) * 32], KI[I][:, cs],
                           qr[:, cs].rearrange("p (c i j) -> p c i j", c=4, i=4)[:, :, I, :], True, True, [t_kd, t_qr], [PT[pa]])
                    TT("dve", ATa[:, i, :].rearrange("p (c i j) -> p i c j", c=4, i=4),
                       PS[pa][:, 0:128].rearrange("p (i c j) -> p i c j", i=4, c=4),
                       msk.rearrange("p (c i j) -> p i c j", c=4, i=4), ALU.mult, [PT[pa], t_mask], [t_ATa])
                tl = list(range(out_t0, NT))
                for g0 in range(0, len(tl), 4):
                    grp = tl[g0:g0 + 4]
                    for gi, i in enumerate(grp):
                        cs = slice(i * 128, (i + 1) * 128)
                        MM(PS[gi][:, 128:256], ATa[:, i, :], vtok[:, i, :], True, True, [t_ATa, t_vtok], [PT[gi]])
                        MM(PS[4 + gi][:, :], qd[:, cs], Sall[:, 4 * i:4 * i + 4, :].rearrange("p j c -> p (j c)"), True, True,
                           [t_qd, t_Sall], [PT[4 + gi]])
                    for gi, i in enumerate(grp):
                        dst = oacc[:, i, :]
                        if d == 0:
                            CP("act", dst, PS[gi][:, 128:256], [PT[gi]], [t_oaccs[i]])
                        else:
                            TT("dve", dst, PS[gi][:, 128:256], dst, ALU.add, [PT[gi], t_oaccs[i]], [t_oaccs[i]])
                    for j in range(4):
                        for gi, i in enumerate(grp):
                            dst = oacc[:, i, :]
                            STT("dve", dst, PS[4 + gi][:, j * 128:(j + 1) * 128], rowm[:, j:j + 1], dst, ALU.mult, ALU.add,
                                [PT[4 + gi], t_mask, t_oaccs[i]], [t_oaccs[i]])
                if d == 1:
                    for i in range(out_t0, NT):
                        ACT(junk3, oacc[:, i, :], AF.Square, [t_oaccs[i]], [t_j3, t_ss3], accum=ssa[:, i:i + 1])
                    rms_rstd(ssa[:, out_t0:NT], 128, [t_ss3], ssa[:, out_t0:NT], t_ss3)
                    for i in range(out_t0, NT):
                        ACT(ona[:, i, :], oacc[:, i, :], AF.Identity, [t_oaccs[i], t_ss3], [t_on], scale=ssa[:, i:i + 1])
                    for i in range(out_t0, NT):
                        pq = 4 + i % 4
                        cs = slice(i * 128, (i + 1) * 128)
                        pv = PS[pq][:, :].bitcast(BF16)
                        TRP(pv[:, 0:128], ona[:, i, :], identb, [t_on, t_identb], [PT[pq]])
                        STT("dve", yhT[:, cs], pv[:, 0:128], vecT[:, 40 + h:41 + h], sg[:, cs], ALU.mult, ALU.mult,
                            [PT[pq], t_vec, t_sg], [t_yhT])
            DMA("sp", yT_d[:, 2 + h, mix_c0:T], yhT[:, mix_c0:T], [t_yhT], [])
            S.barrier()
            A.release(m0)

    def ffn_sublayer(l, b, hT, t_hT):
        last = (l == NL - 1)
        tlo = 0 if not last else NTC
        m0 = A.mark()
        ysb = A.alloc([8, T], BF16); t_ysb = Trk()
        DMA("sp", ysb[:, :, tlo * 128:T], yT_d[:, :, tlo * 128:T], [], [t_ysb])
        wo = A.alloc([KC, D], BF16); t_wo = Trk()
        DMA("pool", wo, W["w_out"][l].rearrange("(kc p) n -> p kc n", p=128), [], [t_wo])
        gt = {}
        t_gt = Trk()
        for r in ([b] if last else [b, 2]):
            gt[r] = A.alloc([D])
            DMA("sp", gt[r], gate_d[l, r, 0].partition_broadcast(128), [], [t_gt])
        xt = [A.alloc([D]) for _ in range(2)]; t_xt = [Trk(), Trk()]
        t1 = A.alloc([D]); t_t1 = Trk()
        junk = [A.alloc([D], BF16) for _ in range(2)]; t_junk = [Trk(), Trk()]
        ssb = [A.alloc([8]) for _ in range(2)]; t_ss = [Trk(), Trk()]
        xn = [A.alloc([D], BF16) for _ in range(2)]; t_xn = [Trk(), Trk()]
        for i in range(tlo, NT):
            bi = i % 2
            r = cond_row(b, i < NTC)
            DMA("sp", xt[bi], xres[b, i * 128:(i + 1) * 128, :], [], [t_xt[bi]])
            for hh in range(2):
                pb = 2 + hh
                for kc in range(KC):
                    MM(PS[pb][:, :], ysb[:, kc, i * 128:(i + 1) * 128], wo[:, kc, hh * 512:(hh + 1) * 512], kc == 0, kc == KC - 1,
                       [t_ysb, t_wo], [PT[pb]])
                TT("dve", t1[:, hh * 512:(hh + 1) * 512], PS[pb][:, :], gt[r][:, hh * 512:(hh + 1) * 512], ALU.mult, [PT[pb], t_gt], [t_t1])
            TT("dve", xt[bi], xt[bi], t1, ALU.add, [t_xt[bi], t_t1], [t_xt[bi]])
            DMA("sp", xres[b, i * 128:(i + 1) * 128, :], xt[bi], [t_xt[bi]], [])
            norm_mod_transpose(xt[bi], t_xt[bi], hT, t_hT, i * 128, 2, r,
                               (junk[bi], t_junk[bi], ssb[bi], t_ss[bi], xn[bi], t_xn[bi]), i)
        S.barrier()
        A.release(m0)
        ntiles = NT - tlo
        halves = [(tlo, tlo + (ntiles + 1) // 2), (tlo + (ntiles + 1) // 2, NT)] if not HALF_EXP else [(tlo, tlo + 1), (tlo + 1, NT)]
        for (ta, tb) in halves:
            if tb <= ta:
                continue
            m0 = A.mark()
            nth = tb - ta
            comb = A.alloc([nth, NEXP]); t_comb = Trk()
            rt = A.alloc([12, NEXP]); t_rt = Trk()
            sc = A.alloc([16]); t_sc = Trk()
            for ii in range(nth):
                i = ta + ii
                pb = ii % 2
                for kc in range(KC):
                    MM(PS[pb][:, 0:NEXP], hT[:, kc, i * 128:(i + 1) * 128], rw_sb[:, kc, :], kc == 0, kc == KC - 1, [t_hT, t_rw], [PT[pb]])
                lg = rt[:, 0, :]
                CP("dve", lg, PS[pb][:, 0:NEXP], [PT[pb]], [t_rt])
                RED(sc[:, 0:1], lg, ALU.max, [t_rt], [t_sc])
                TS("dve", sc[:, 1:2], sc[:, 0:1], -1.0, None, ALU.mult, None, [t_sc], [t_sc])
                ACT(rt[:, 1, :], lg, AF.Exp, [t_rt, t_sc], [t_rt, t_sc], bias=sc[:, 1:2], accum=sc[:, 2:3])
                RCP(sc[:, 3:4], sc[:, 2:3], [t_sc], [t_sc])
                TS("dve", rt[:, 2, :], rt[:, 1, :], sc[:, 3:4], None, ALU.mult, None, [t_rt, t_sc], [t_rt])
                TT("dve", rt[:, 3, :], rt[:, 2, :], rb_sb, ALU.add, [t_rt, t_rb], [t_rt])
                sel3 = rt[:, 3, :].rearrange("p (g k) -> p g k", k=4)
                RED(sc[:, 4:8], sel3, ALU.max, [t_rt], [t_sc])
                TT("dve", rt[:, 4, :].rearrange("p (g k) -> p g k", k=4), sel3, sc[:, 4:8].unsqueeze(2).to_broadcast([128, 4, 4]),
                   ALU.is_equal, [t_rt, t_sc], [t_rt])
                STT("dve", rt[:, 5, :], rt[:, 4, :], -1e9, rt[:, 3, :], ALU.mult, ALU.add, [t_rt], [t_rt])
                sel23 = rt[:, 5, :].rearrange("p (g k) -> p g k", k=4)
                RED(sc[:, 8:12], sel23, ALU.max, [t_rt], [t_sc])
                TT("dve", sc[:, 12:16], sc[:, 4:8], sc[:, 8:12], ALU.add, [t_sc], [t_sc])
                RED(sc[:, 0:1], sc[:, 12:16], ALU.max, [t_sc], [t_sc])
                TS("dve", sc[:, 12:16], sc[:, 12:16], sc[:, 0:1], None, ALU.is_equal, None, [t_sc], [t_sc])
                TT("dve", rt[:, 6, :].rearrange("p (g k) -> p g k", k=4), sel3, sc[:, 8:12].unsqueeze(2).to_broadcast([128, 4, 4]),
                   ALU.is_ge, [t_rt, t_sc], [t_rt])
                TT("dve", rt[:, 6, :].rearrange("p (g k) -> p g k", k=4), rt[:, 6, :].rearrange("p (g k) -> p g k", k=4),
                   sc[:, 12:16].unsqueeze(2).to_broadcast([128, 4, 4]), ALU.mult, [t_rt, t_sc], [t_rt])
                TT("dve", rt[:, 7, :], rt[:, 6, :], rt[:, 2, :], ALU.mult, [t_rt], [t_rt])
                RED(sc[:, 1:2], rt[:, 7, :], ALU.add, [t_rt], [t_sc])
                RCP(sc[:, 2:3], sc[:, 1:2], [t_sc], [t_sc])
                TS("dve", comb[:, ii, :], rt[:, 7, :], sc[:, 2:3], None, ALU.mult, None, [t_rt, t_sc], [t_comb])
                if DEBUG and ta == tlo and l == 0 and ii == 0 and b == 0:
                    DMA("sp", dbg_acc[:, 8, 0:192], rt.rearrange("p a b -> p (a b)"), [t_rt], [])
                    DMA("sp", dbg_acc[:, 9, 0:16], sc, [t_sc], [])
            acc = A.alloc([nth, D]); t_acc = Trk()
            wge = [A.alloc([KC, DEXP], BF16) for _ in range(2)]
            wue = [A.alloc([KC, DEXP], BF16) for _ in range(2)]
            wde = [A.alloc([4, D], BF16) for _ in range(2)]
            t_we = [Trk(), Trk()]
            At = [A.alloc([4, 512], BF16) for _ in range(2)]; t_At = [Trk(), Trk()]
            sl = [A.alloc([512]) for _ in range(2)]; t_sl = [Trk(), Trk()]
            cnt = 0
            for e_ in range(NEXP):
                bi = e_ % 2
                DMA("pool", wge[bi], W["moe_w_gate"][l, e_].rearrange("(kc p) n -> p kc n", p=128), [], [t_we[bi]])
                DMA("pool", wue[bi], W["moe_w_up"][l, e_].rearrange("(kc p) n -> p kc n", p=128), [], [t_we[bi]])
                DMA("pool", wde[bi], W["moe_w_down"][l, e_].rearrange("(kc p) n -> p kc n", p=128), [], [t_we[bi]])
                for (g0, gw) in col_groups(ta * 128, tb * 128):
                    ab = cnt % 2
                    cnt += 1
                    for oc in range(4):
                        pg, pu = oc % 2, 2 + oc % 2
                        for kc in range(KC):
                            MM(PS[pg][:, 0:gw], wge[bi][:, kc, oc * 128:(oc + 1) * 128], hT[:, kc, g0:g0 + gw], kc == 0, kc == KC - 1,
                               [t_we[bi], t_hT], [PT[pg]])
                        for kc in range(KC):
                            MM(PS[pu][:, 0:gw], wue[bi][:, kc, oc * 128:(oc + 1) * 128], hT[:, kc, g0:g0 + gw], kc == 0, kc == KC - 1,
                               [t_we[bi], t_hT], [PT[pu]])
                        sb_ = oc % 2
                        ACT(sl[sb_][:, 0:gw], PS[pg][:, 0:gw], AF.Silu, [PT[pg]], [t_sl[sb_]])
                        TT("dve", At[ab][:, oc, 0:gw], PS[pu][:, 0:gw], sl[sb_][:, 0:gw], ALU.mult, [PT[pu], t_sl[sb_]], [t_At[ab]])
                    for tt_ in range(gw // 128):
                        ti = (g0 // 128 - ta) + tt_
                        for hh in range(2):
                            py = 4 + (2 * tt_ + hh) % 4
                            for kc in range(4):
                                MM(PS[py][:, :], At[ab][:, kc, tt_ * 128:(tt_ + 1) * 128], wde[bi][:, kc, hh * 512:(hh + 1) * 512],
                                   kc == 0, kc == 3, [t_At[ab], t_we[bi]], [PT[py]])
                            dst = acc[:, ti, hh * 512:(hh + 1) * 512]
                            if e_ == 0:
                                TS("dve", dst, PS[py][:, :], comb[:, ti, e_:e_ + 1], None, ALU.mult, None, [PT[py], t_comb], [t_acc])
                            else:
                                STT("dve", dst, PS[py][:, :], comb[:, ti, e_:e_ + 1], dst, ALU.mult, ALU.add, [PT[py], t_comb, t_acc], [t_acc])
            if DEBUG and ta == tlo and l == 0:
                DMA("sp", dbg_acc[:, 0:nth, :], acc, [t_acc], [])
                DMA("sp", dbg_comb[:, 0:nth, :], comb, [t_comb], [])
                DMA("sp", dbg_h, hT[:, :, 0:256], [t_hT], [])
            g5 = {}
            t_g5 = Trk()
            for r in ([b] if last else [b, 2]):
                g5[r] = A.alloc([D])
                DMA("sp", g5[r], gate_d[l, r, 1].partition_broadcast(128), [], [t_g5])
            xt = [A.alloc([D]) for _ in range(2)]; t_xt = [Trk(), Trk()]
            junk = A.alloc([D], BF16); t_junk = Trk()
            ssb = A.alloc([8]); t_ss = Trk()
            for ii in range(nth):
                i = ta + ii
                bi = ii % 2
                r = cond_row(b, i < NTC)
                DMA("sp", xt[bi], xres[b, i * 128:(i + 1) * 128, :], [], [t_xt[bi]])
                TT("dve", acc[:, ii, :], acc[:, ii, :], g5[r], ALU.mult, [t_acc, t_g5], [t_acc])
                TT("dve", xt[bi], xt[bi], acc[:, ii, :], ALU.add, [t_xt[bi], t_acc], [t_xt[bi]])
                if not last:
                    DMA("sp", xres[b, i * 128:(i + 1) * 128, :], xt[bi], [t_xt[bi]], [])
                else:
                    ACT(junk, xt[bi], AF.Square, [t_xt[bi]], [t_junk, t_ss], accum=ssb[:, 0:1])
                    rms_rstd(ssb[:, 0:1], D, [t_ss], ssb[:, 1:2], t_ss)
                    STT("dve", xt[bi], xt[bi], ssb[:, 1:2], fng_sb, ALU.mult, ALU.mult, [t_xt[bi], t_ss, t_fng], [t_xt[bi]])
                    j = i - NTC
                    DMA("sp", out_d[b, j * 128:(j + 1) * 128, :], xt[bi], [t_xt[bi]], [])
            S.barrier()
            A.release(m0)

    for l in range(NL):
        if STOP_AFTER is not None and l > STOP_AFTER:
            break
        layer_setup(l)
        for b in range(NB):
            m0 = A.mark()
            hT = A.alloc([KC, T], BF16)
            t_hT = Trk()
            mixer_sublayer(l, b, hT, t_hT)
            ffn_sublayer(l, b, hT, t_hT)
            S.barrier()
            A.release(m0)
    stats = S.emit(es)
    es.close()
    return nc, hc, stats, A.peak


_CACHE = {}


def kernel(**inputs):
    NB, L, LC = 2, 2048, 256
    B = inputs["x"].shape[0]
    ncores = B // NB
    if "prog" not in _CACHE:
        _CACHE["prog"] = build(NB, L, LC)
    nc, hc, stats, peak = _CACHE["prog"]
    shared = {k: np.ascontiguousarray(np.asarray(inputs[k], dtype=np.float32)) for k in WEIGHT_SHAPES}
    shared["c_ctx"] = np.ascontiguousarray(np.asarray(inputs["c_ctx"], dtype=np.float32))
    for k, v in hc.items():
        shared["k_" + k] = v
    x = np.asarray(inputs["x"], dtype=np.float32)
    c = np.asarray(inputs["c"], dtype=np.float32)
    ctx = np.asarray(inputs["ctx"], dtype=np.float32)
    in_maps = []
    for i in range(ncores):
        m = dict(shared)
        m["x"] = np.ascontiguousarray(x[i * NB:(i + 1) * NB])
        m["c"] = np.ascontiguousarray(c[i * NB:(i + 1) * NB])
        m["ctx"] = np.ascontiguousarray(ctx[i * NB:(i + 1) * NB])
        in_maps.append(m)
    res = run_bass_kernel_spmd(nc, in_maps, core_ids=list(range(ncores)))
    return np.concatenate([r["out"] for r in res.results], axis=0).astype(np.float32)
```

```python
import math
from contextlib import ExitStack
import numpy as np
import ml_dtypes
import concourse.bass as bass
import concourse.mybir as mybir
from concourse.bass_utils import run_bass_kernel_spmd

F32 = mybir.dt.float32
BF16 = mybir.dt.bfloat16
ALU = mybir.AluOpType
AF = mybir.ActivationFunctionType
AX = mybir.AxisListType

ENGS = ("pe", "act", "dve", "pool", "sp")
ROT = 12000
NDMASEM = 64
RING = {'sp': (0, 40), 'pool': (40, 20), 'act': (60, 4), 'dve': (60, 4), 'pe': (60, 4)}


class Trk:
    __slots__ = ("lw", "rd")

    def __init__(self):
        self.lw = None
        self.rd = []


class Op:
    __slots__ = ("eng", "fn", "deps", "mile", "isdma", "ev", "semi")


class Sched:
    def __init__(self, nc):
        self.nc = nc
        self.ops = {e: [] for e in ENGS}
        self.ndma = 0
        self.dma_cnt = [0] * NDMASEM
        self.bar_deps = []
        self.dma_open = []
        self.all_dma = []
        self.ring_pos = {e: 0 for e in ENGS}
        self.sem_last = [None] * NDMASEM

    def op(self, eng, fn, reads=(), writes=(), dma=False):
        o = Op()
        o.eng = eng
        o.fn = fn
        o.isdma = dma
        o.mile = False
        o.ev = None
        deps = list(self.bar_deps)
        for t in reads:
            if t.lw is not None:
                deps.append(t.lw)
        for t in writes:
            if t.lw is not None:
                deps.append(t.lw)
            deps.extend(t.rd)
        seen = set()
        dd = []
        for d in deps:
            if id(d) in seen or d is o:
                continue
            seen.add(id(d))
            if (not d.isdma) and (not dma) and d.eng == "pe" and eng == "pe":
                continue
            d.mile = True
            dd.append(d)
        o.deps = dd
        for t in reads:
            if (not dma) and t.rd and (not t.rd[-1].isdma) and t.rd[-1].eng == eng:
                t.rd[-1] = o
            else:
                t.rd.append(o)
        for t in writes:
            t.lw = o
            t.rd = []
        if dma:
            base, cnt_ = RING[eng]
            j = base + self.ring_pos[eng] % cnt_
            self.ring_pos[eng] += 1
            self.ndma += 1
            prev = self.sem_last[j]
            if prev is not None and all(prev is not d_ for d_ in o.deps):
                o.deps.append(prev)
            self.sem_last[j] = o
            self.dma_cnt[j] += 1
            o.semi = j
            o.ev = ("d", j, 16 * self.dma_cnt[j])
            self.dma_open.append(o)
            self.all_dma.append(o)
        self.ops[eng].append(o)
        return o

    def barrier(self):
        deps = []
        for e in ENGS:
            for o in reversed(self.ops[e]):
                if not o.isdma:
                    o.mile = True
                    deps.append(o)
                    break
        best = {}
        for o in self.dma_open:
            if o.semi not in best or best[o.semi].ev[2] < o.ev[2]:
                best[o.semi] = o
        deps.extend(best.values())
        self.dma_open = []
        self.bar_deps = deps

    def emit(self, es):
        nc = self.nc
        nsem = {}
        for e in ENGS:
            c = 0
            for o in self.ops[e]:
                if o.isdma:
                    continue
                if o.mile:
                    c += 1
                    o.ev = (e, (c - 1) // ROT, (c - 1) % ROT + 1)
            nsem[e] = (c + ROT - 1) // ROT if c else 0
        sems = {}
        for e in ENGS:
            for i in range(nsem[e]):
                sems[(e, i)] = es.enter_context(nc.semaphore(f"s_{e}{i}"))
        for j in range(NDMASEM):
            sems[("d", j)] = es.enter_context(nc.semaphore(f"s_d{j}"))
        block = es.enter_context(nc.Block())
        handles = {"pe": block.tensor, "act": block.scalar, "dve": block.vector,
                   "pool": block.gpsimd, "sp": block.sync}
        stats = {}
        for e in ENGS:
            ops = self.ops[e]
            last_dma = None
            if e == "sp":
                best = {}
                for o in self.all_dma:
                    if o.semi not in best or best[o.semi].ev[2] < o.ev[2]:
                        best[o.semi] = o
                last_dma = list(best.values())

            def body(eng, ops=ops, e=e, last_dma=last_dma):
                known = {}
                nw = 0
                for o in ops:
                    for d in o.deps:
                        k = (d.ev[0], d.ev[1])
                        v = d.ev[2]
                        if known.get(k, 0) < v:
                            eng.wait_ge(sems[k], v)
                            known[k] = v
                            nw += 1
                    ins = o.fn(eng)
                    if o.isdma:
                        ins.then_inc(sems[("d", o.semi)], 16)
                    elif o.mile:
                        ins.then_inc(sems[(o.ev[0], o.ev[1])], 1)
                if last_dma is not None:
                    for d in last_dma:
                        k = (d.ev[0], d.ev[1])
                        if known.get(k, 0) < d.ev[2]:
                            eng.wait_ge(sems[k], d.ev[2])
                            known[k] = d.ev[2]
                stats[e] = (len(ops), nw)

            handles[e](body)
        return stats


class Arena:
    def __init__(self, nc, es, nelem_f32):
        self.t = es.enter_context(nc.sbuf_tensor("arena", [128, nelem_f32], F32))
        self.n = nelem_f32
        self.off = 0
        self.peak = 0

    def mark(self):
        return self.off

    def release(self, m):
        self.off = m

    def alloc(self, shape_free, dtype=F32):
        n = int(np.prod(shape_free))
        nf = (n + 1) // 2 if dtype == BF16 else n
        nf = (nf + 7) // 8 * 8
        assert self.off + nf <= self.n, f"arena overflow {self.off}+{nf}>{self.n}"
        ap = self.t[:, self.off:self.off + nf]
        self.off += nf
        self.peak = max(self.peak, self.off)
        if dtype == BF16:
            ap = ap.bitcast(BF16)[:, 0:n]
        else:
            ap = ap[:, 0:n]
        if len(shape_free) > 1:
            names = " ".join(f"a{i}" for i in range(len(shape_free)))
            kw = {f"a{i}": int(s) for i, s in enumerate(shape_free)}
            ap = ap.rearrange(f"p ({names}) -> p {names}", **kw)
        return ap


D = 1024
KC = 8
NEXP = 16
DEXP = 512
DPROJ = 3584
EPS = 1e-6
TWO_PI = 2.0 * math.pi


def host_consts(L, LC):
    bf = ml_dtypes.bfloat16
    c = {}
    rows = L // 64
    row = np.repeat(np.arange(rows, dtype=np.float32), 64)
    col = np.tile(np.arange(64, dtype=np.float32), rows)
    quarter = D // 4
    omega = (1.0 / (10000.0 ** (np.arange(quarter, dtype=np.float32) / quarter))).astype(np.float32)
    ar = row[:, None] * omega
    ac = col[:, None] * omega
    c["pos"] = np.concatenate([np.sin(ar), np.cos(ar), np.sin(ac), np.cos(ac)], axis=-1).astype(np.float32)
    c["identb"] = np.eye(128).astype(bf)
    c["identf"] = np.eye(128).astype(np.float32)
    s = np.arange(128)[:, None]
    t = np.arange(128)[None, :]
    same = (s // 32) == (t // 32)
    c["maskf"] = (same & (s <= t)).astype(np.float32)
    c["maskb"] = (same & (s >= t)).astype(np.float32)
    c["rowm"] = (np.arange(128)[:, None] // 32 == np.arange(4)[None, :]).astype(np.float32)
    deltas = np.abs(np.linspace(math.log(1e-2) / 1.5, math.log(1e-2) / 0.3, 256, dtype=np.float32))
    m = np.arange(64)
    c64 = np.cos(2 * np.pi * np.outer(m, m) / 64) / 8.0
    s64 = -np.sin(2 * np.pi * np.outer(m, m) / 64) / 8.0
    z = np.zeros((64, 64))
    c["bdc"] = np.block([[c64, z], [z, c64]]).astype(np.float32)
    c["bds"] = np.block([[s64, z], [z, s64]]).astype(np.float32)
    for tag, n in (("l", L), ("c", LC)):
        nt = n // 128
        tt = np.linspace(0.0, 1.0, n, dtype=np.float32)[:, None]
        w = (2.0 * math.pi * np.arange(n, dtype=np.float32)[:, None] / n).astype(np.float32)
        bands = np.linspace(1e-4, 15, 16, dtype=np.float32)[None, :]
        zp = np.concatenate([tt, np.cos(bands * w), -np.sin(bands * w)], axis=-1).astype(np.float32)
        c["zpos" + tag] = np.ascontiguousarray(zp.T)
        dec = np.exp(-tt * deltas[None, :]).astype(np.float32)
        dec0 = dec.copy()
        dec0[0, :] = 0.0
        c["dec" + tag] = np.ascontiguousarray(np.concatenate([dec, dec0, dec, dec0], axis=1))
        k = np.arange(n, dtype=np.float64)
        tq = np.arange(n, dtype=np.float64)
        ang = 2 * np.pi * np.outer(tq, k + 0.5) / (2 * n)
        Fc = np.cos(ang)
        Fs = np.sin(ang)

        def tile_fwd(M):
            return M.reshape(nt, 128, nt, 128).transpose(2, 1, 0, 3)

        hyf = np.stack([tile_fwd(Fc), tile_fwd(Fs)], axis=2)
        c["hyf" + tag] = np.ascontiguousarray(hyf).astype(bf)
        Ic = (Fc.T / n)
        Is = (-Fs.T / n)
        hyi = np.stack([tile_fwd(Ic), tile_fwd(Is)], axis=2)
        c["hyi" + tag] = np.ascontiguousarray(hyi).astype(bf)
        ang2 = 2 * np.pi * np.outer(tq, k) / n
        fnf = np.stack([tile_fwd(np.cos(ang2) / math.sqrt(n)), tile_fwd(np.sin(ang2) / math.sqrt(n))], axis=2)
        c["fnf" + tag] = np.ascontiguousarray(fnf).astype(bf)
    return c


WEIGHT_SHAPES = {
    "ada_w": (2, D, 6 * D), "ada_b": (2, 6 * D), "norm1_g": (2, D), "norm2_g": (2, D),
    "w_in": (2, D, DPROJ), "w_out": (2, D, D), "hy_conv_w": (2, 3, 768), "hy_conv_b": (2, 768),
    "hy_filt_w1": (2, 33, 64), "hy_filt_b1": (2, 64), "hy_filt_w2": (2, 64, 64), "hy_filt_b2": (2, 64),
    "hy_filt_w3": (2, 64, 1024), "hy_filt_freq": (2, 64), "hy_bias": (2, 2, 256), "hy_norm_g": (2, 256),
    "hg_lower_bounds": (2, 2, 512), "hg_norm_g": (2, 512), "fn_w": (2, 4, 64, 64), "fn_b": (2, 256),
    "fn_norm_g": (2, 256), "router_w": (D, NEXP), "router_b": (NEXP,),
    "moe_w_gate": (2, NEXP, D, DEXP), "moe_w_up": (2, NEXP, D, DEXP), "moe_w_down": (2, NEXP, DEXP, D),
    "final_norm_g": (D,),
}


DEBUG = False
STOP_AFTER = None
HALF_EXP = False


def build(NB, L, LC, NL=2, arena_elems=53000):
    T = LC + L
    NT = T // 128
    NTC = LC // 128
    NTL = L // 128
    NCH = T // 32
    NCHC = LC // 32
    hc = host_consts(L, LC)
    nc = bass.Bass("TRN2", target_bir_lowering=False)

    def din(name, shape, dt=F32):
        return nc.dram_tensor(name, list(shape), dt, kind="ExternalInput").ap()

    def dscr(name, shape, dt=F32):
        return nc.dram_tensor(name, list(shape), dt, kind="Internal").ap()

    x_d = din("x", [NB, L, D])
    c_d = din("c", [NB, D])
    ctx_d = din("ctx", [NB, LC, D])
    cctx_d = din("c_ctx", [D])
    W = {k: din(k, v) for k, v in WEIGHT_SHAPES.items()}
    C = {k: din("k_" + k, v.shape, BF16 if v.dtype == ml_dtypes.bfloat16 else F32) for k, v in hc.items()}
    out_d = nc.dram_tensor("out", [NB, L, D], F32, kind="ExternalOutput").ap()

    xres = nc.dram_tensor("xres", [NB, T, D], F32, kind="ExternalOutput").ap() if DEBUG else dscr("xres", [NB, T, D])
    gate_d = dscr("gate_rows", [NL, 3, 2, D])
    kk_d = {"l": dscr("kk_l", [NL, NTL, 128, 2, 2, 256], BF16), "c": dscr("kk_c", [NL, NTC, 128, 2, 2, 256], BF16)}
    yT_d = nc.dram_tensor("yT", [128, 8, T], BF16, kind="ExternalOutput").ap() if DEBUG else dscr("yT", [128, 8, T], BF16)

    if DEBUG:
        dbg_acc = nc.dram_tensor("dbg_acc", [128, 16, D], F32, kind="ExternalOutput").ap()
        dbg_comb = nc.dram_tensor("dbg_comb", [128, 16, NEXP], F32, kind="ExternalOutput").ap()
        dbg_h = nc.dram_tensor("dbg_h", [128, KC, 256], BF16, kind="ExternalOutput").ap()
    es = ExitStack()
    S = Sched(nc)
    A = Arena(nc, es, arena_elems)
    PS = [es.enter_context(nc.psum_tensor(f"ps{i}", [128, 512], F32)) for i in range(8)]
    PT = [Trk() for _ in range(8)]

    def MM(out, lhsT, rhs, st, sp, R, Wt):
        S.op("pe", lambda e: e.matmul(out, lhsT=lhsT, rhs=rhs, start=st, stop=sp), reads=R, writes=Wt)

    def TRP(out, in_, ident, R, Wt):
        S.op("pe", lambda e: e.transpose(out=out, in_=in_, identity=ident), reads=R, writes=Wt)

    def ACT(out, in_, func, R, Wt, scale=1.0, bias=0.0, accum=None):
        if accum is None:
            S.op("act", lambda e: e.activation(out=out, in_=in_, func=func, scale=scale, bias=bias), reads=R, writes=Wt)
        else:
            S.op("act", lambda e: e.activation(out=out, in_=in_, func=func, scale=scale, bias=bias, accum_out=accum),
                 reads=R, writes=Wt)

    def TT(eng, out, a, b, op, R, Wt):
        S.op(eng, lambda e: e.tensor_tensor(out=out, in0=a, in1=b, op=op), reads=R, writes=Wt)

    def TS(eng, out, a, s1, s2, op0, op1, R, Wt):
        if s2 is None:
            S.op(eng, lambda e: e.tensor_scalar(out=out, in0=a, scalar1=s1, scalar2=None, op0=op0), reads=R, writes=Wt)
        else:
            S.op(eng, lambda e: e.tensor_scalar(out=out, in0=a, scalar1=s1, scalar2=s2, op0=op0, op1=op1),
                 reads=R, writes=Wt)

    def STT(eng, out, a, sc, b, op0, op1, R, Wt):
        S.op(eng, lambda e: e.scalar_tensor_tensor(out=out, in0=a, scalar=sc, in1=b, op0=op0, op1=op1),
             reads=R, writes=Wt)

    def CP(eng, out, in_, R, Wt):
        if eng == "act":
            S.op("act", lambda e: e.copy(out=out, in_=in_), reads=R, writes=Wt)
        else:
            S.op(eng, lambda e: e.tensor_copy(out=out, in_=in_), reads=R, writes=Wt)

    def MSET(eng, ap, val, Wt):
        S.op(eng, lambda e: e.memset(ap, val), writes=Wt)

    def RED(out, in_, op, R, Wt):
        S.op("dve", lambda e: e.tensor_reduce(out=out, in_=in_, axis=AX.X, op=op), reads=R, writes=Wt)

    def SCAN(out, d0, d1, R, Wt):
        S.op("dve", lambda e: e.tensor_tensor_scan(out=out, data0=d0, data1=d1, initial=0.0, op0=ALU.mult, op1=ALU.add),
             reads=R, writes=Wt)

    def RCP(out, in_, R, Wt):
        S.op("dve", lambda e: e.reciprocal(out=out, in_=in_), reads=R, writes=Wt)

    def DMA(q, out, in_, R, Wt, slow=False):
        if slow:
            S.op(q, lambda e: e.dma_start(out=out, in_=in_, allow_slow_non_contiguous=True), reads=R, writes=Wt, dma=True)
        else:
            S.op(q, lambda e: e.dma_start(out=out, in_=in_), reads=R, writes=Wt, dma=True)

    rr = {"ps": 0}

    def evac_eng(i):
        return "act" if i % 2 == 0 else "dve"

    identb = A.alloc([128], BF16); t_identb = Trk()
    identf = A.alloc([128]); t_identf = Trk()
    maskf = A.alloc([128]); maskb = A.alloc([128]); t_mask = Trk()
    rowm = A.alloc([4])
    scanm = A.alloc([T]); t_scanm = Trk()
    scT = A.alloc([KC, 3], BF16); t_scT = Trk()
    stg = A.alloc([128]); t_stg = Trk()
    vecT = A.alloc([128]); t_vec = Trk()
    lbT = A.alloc([3, 8]); t_lb = Trk()
    modT = A.alloc([48, 3]); t_mod = Trk()
    GS = A.alloc([4, KC, 3]); t_GS = Trk()
    filtp = A.alloc([4]); t_filtp = Trk()
    bda = A.alloc([2, 128], BF16); bdb = A.alloc([2, 128], BF16); t_bd = Trk()
    rw_sb = A.alloc([KC, NEXP], BF16); t_rw = Trk()
    rb_sb = A.alloc([NEXP]); t_rb = Trk()
    fng_sb = A.alloc([D]); t_fng = Trk()
    small = A.alloc([64]); t_small = Trk()

    DMA("sp", identb, C["identb"], [], [t_identb])
    DMA("sp", identf, C["identf"], [], [t_identf])
    DMA("sp", maskf, C["maskf"], [], [t_mask])
    DMA("sp", maskb, C["maskb"], [], [t_mask])
    DMA("sp", rowm, C["rowm"], [], [t_mask])
    MSET("pool", scanm, 1.0, [t_scanm])
    MSET("pool", scanm.rearrange("p (c j) -> p c j", j=32)[:, :, 0:1], 0.0, [t_scanm])
    MSET("pool", stg, 0.0, [t_stg])
    DMA("pool", rw_sb, W["router_w"].rearrange("(kc p) e -> p kc e", p=128), [], [t_rw])
    DMA("sp", rb_sb, W["router_b"].partition_broadcast(128), [], [t_rb])
    DMA("sp", fng_sb, W["final_norm_g"].partition_broadcast(128), [], [t_fng])
    m0 = A.mark()
    cf = A.alloc([KC, 3]); t_cf = Trk()
    for r in range(3):
        src = c_d[r] if r < NB else cctx_d
        DMA("sp", cf[:, :, r], src.rearrange("(kc p) -> p kc", p=128), [], [t_cf], slow=True)
    ACT(scT, cf, AF.Silu, [t_cf], [t_scT])
    S.barrier()
    A.release(m0)
    PERSIST = A.mark()

    def cond_row(b, is_ctx):
        return 2 if is_ctx else b

    def layer_setup(l):
        m0 = A.mark()
        def ld(r0, nr, src):
            DMA("sp", stg[r0:r0 + nr, :], src, [], [t_stg])
        ld(0, 8, W["norm1_g"][l].rearrange("(r c) -> r c", c=128))
        ld(8, 8, W["norm2_g"][l].rearrange("(r c) -> r c", c=128))
        ld(16, 18, W["hy_conv_w"][l].rearrange("k (j c) -> (k j) c", c=128))
        ld(34, 6, W["hy_conv_b"][l].rearrange("(r c) -> r c", c=128))
        ld(40, 4, W["hg_norm_g"][l].rearrange("(r c) -> r c", c=128))
        ld(44, 48, W["ada_b"][l].rearrange("(r c) -> r c", c=128))
        ld(92, 2, W["hy_norm_g"][l].rearrange("(r c) -> r c", c=128))
        ld(94, 2, W["fn_norm_g"][l].rearrange("(r c) -> r c", c=128))
        ld(96, 8, W["hg_lower_bounds"][0].rearrange("d (h c) -> (d h) c", c=128))
        ld(104, 8, W["hg_lower_bounds"][1].rearrange("d (h c) -> (d h) c", c=128))
        TRP(PS[0][:, 0:128], stg, identf, [t_stg, t_identf], [PT[0]])
        CP("dve", vecT, PS[0][:, 0:128], [PT[0]], [t_vec])
        if l == 0:
            MSET("pool", lbT[:, 0, :], 0.0, [t_lb])
        else:
            TT("dve", small[:, 0:8], vecT[:, 104:112], vecT[:, 96:104], ALU.subtract, [t_vec], [t_small])
            ACT(lbT[:, 0, :], small[:, 0:8], AF.Sigmoid, [t_small], [t_lb])
        TS("dve", lbT[:, 1, :], lbT[:, 0, :], -1.0, 1.0, ALU.mult, ALU.add, [t_lb], [t_lb])
        TS("dve", lbT[:, 2, :], lbT[:, 1, :], -1.0, None, ALU.mult, None, [t_lb], [t_lb])
        m_mod = A.mark()
        screp = A.alloc([KC, 3, 128], BF16); t_screp = Trk()
        CP("dve", screp, scT.unsqueeze(3).to_broadcast([128, KC, 3, 128]), [t_scT], [t_screp])
        wa = [A.alloc([KC, 768], BF16) for _ in range(2)]
        t_wa = [Trk(), Trk()]
        for gq in range(8):
            bi = gq % 2
            DMA("pool", wa[bi], W["ada_w"][l, :, gq * 768:(gq + 1) * 768].rearrange("(kc p) n -> p kc n", p=128), [], [t_wa[bi]])
            for o6 in range(6):
                oc = gq * 6 + o6
                for kc in range(KC):
                    MM(PS[1][:, oc * 3:(oc + 1) * 3], wa[bi][:, kc, o6 * 128:(o6 + 1) * 128], scT[:, kc, :], kc == 0, kc == KC - 1,
                       [t_wa[bi], t_scT], [PT[1]])
        TT("dve", modT, PS[1][:, 0:144].rearrange("p (o r) -> p o r", r=3),
           vecT[:, 44:92].unsqueeze(2).to_broadcast([128, 48, 3]), ALU.add, [PT[1], t_vec], [t_mod])
        for which, (gsl, m_shift, m_scale) in enumerate(((slice(0, 8), 0, 1), (slice(8, 16), 3, 4))):
            gi = 2 * which
            TS("dve", GS[:, gi], modT[:, m_scale * 8:(m_scale + 1) * 8, :], 1.0, None, ALU.add, None, [t_mod], [t_GS])
            TT("dve", GS[:, gi], GS[:, gi], vecT[:, gsl].unsqueeze(2).to_broadcast([128, 8, 3]), ALU.mult, [t_GS, t_vec], [t_GS])
            CP("dve", GS[:, gi + 1], modT[:, m_shift * 8:(m_shift + 1) * 8, :], [t_mod], [t_GS])
        wg = A.alloc([KC, 2, D], BF16); t_wg = Trk()
        for g, m in enumerate((2, 5)):
            for kc in range(KC):
                DMA("pool", wg[:, kc, g, :], W["ada_w"][l, kc * 128:(kc + 1) * 128, m * D:(m + 1) * D], [], [t_wg])
        gb = A.alloc([2, D]); t_gb = Trk()
        for g, m in enumerate((2, 5)):
            DMA("sp", gb[:, g, :], W["ada_b"][l, m * D:(m + 1) * D].partition_broadcast(128), [], [t_gb])
        grow = A.alloc([D]); t_grow = Trk()
        for r in range(3):
            for g in range(2):
                for hh in range(2):
                    pb = 2 + hh
                    for kc in range(KC):
                        MM(PS[pb][:, :], screp[:, kc, r, :], wg[:, kc, g, hh * 512:(hh + 1) * 512], kc == 0, kc == KC - 1,
                           [t_screp, t_wg], [PT[pb]])
                    TT("dve", grow[:, hh * 512:(hh + 1) * 512], PS[pb][:, :], gb[:, g, hh * 512:(hh + 1) * 512], ALU.add,
                       [PT[pb], t_gb], [t_grow])
                DMA("sp", gate_d[l, r, g:g + 1, :], grow[0:1, :], [t_grow], [])
        S.barrier()
        A.release(m_mod)
        DMA("sp", filtp[0:64, 0:1], W["hy_filt_freq"][l].rearrange("(p o) -> p o", o=1), [], [t_filtp])
        DMA("sp", filtp[0:64, 1:2], W["hy_filt_b1"][l].rearrange("(p o) -> p o", o=1), [], [t_filtp])
        DMA("sp", filtp[0:64, 2:3], W["hy_filt_b2"][l].rearrange("(p o) -> p o", o=1), [], [t_filtp])
        w1 = A.alloc([64]); w2 = A.alloc([64]); w3 = A.alloc([1024]); t_fw = Trk()
        DMA("sp", w1[0:33, :], W["hy_filt_w1"][l], [], [t_fw])
        DMA("sp", w2[0:64, :], W["hy_filt_w2"][l], [], [t_fw])
        DMA("sp", w3[0:64, :], W["hy_filt_w3"][l], [], [t_fw])
        biasz = A.alloc([1024]); t_bz = Trk()
        MSET("pool", biasz, 0.0, [t_bz])
        for o in range(2):
            DMA("sp", biasz[0:1, (2 * o) * 256:(2 * o) * 256 + 256], W["hy_bias"][l, o:o + 1, :], [], [t_bz])
        variants = [("l", L, NTL)] + ([("c", LC, NTC)] if l == 0 else [])
        for tag, n, nt in variants:
            m1 = A.mark()
            zp = A.alloc([n]); t_zp = Trk()
            DMA("sp", zp[0:33, :], C["zpos" + tag], [], [t_zp])
            h1 = A.alloc([n]); h2 = A.alloc([n]); t_h1 = Trk(); t_h2 = Trk()
            targ = A.alloc([512]); t_targ = Trk()
            tsn = A.alloc([512]); tcs = A.alloc([512]); tq = A.alloc([512]); t_tsn = Trk()
            fpi = A.alloc([8]); MSET("pool", fpi, math.pi / 2, [t_tsn])
            nblk = (n + 511) // 512
            for stage in range(2):
                src, t_src, dst, t_dst, wmat, kk_, bcol = ((zp, t_zp, h1, t_h1, w1, 33, 1), (h1, t_h1, h2, t_h2, w2, 64, 2))[stage]
                for bk in range(nblk):
                    c0 = bk * 512
                    cw = min(512, n - c0)
                    pb = 4 + bk % 2
                    MM(PS[pb][0:64, 0:cw], wmat[0:kk_, :], src[0:kk_, c0:c0 + cw], True, True, [t_fw, t_src], [PT[pb]])
                    TS("dve", targ[0:64, 0:cw], PS[pb][0:64, 0:cw], filtp[0:64, bcol:bcol + 1], filtp[0:64, 0:1], ALU.add, ALU.mult,
                       [PT[pb], t_filtp], [t_targ])
                    a_ = targ[0:64, 0:cw]
                    s_ = tsn[0:64, 0:cw]; c_ = tcs[0:64, 0:cw]; q_ = tq[0:64, 0:cw]
                    ACT(s_, a_, AF.Sin, [t_targ], [t_tsn], scale=0.125)
                    ACT(c_, a_, AF.Sin, [t_targ], [t_tsn], scale=0.125, bias=fpi[0:64, 0:1])
                    for rep in range(3):
                        STT("dve", q_, s_, 2.0, c_, ALU.mult, ALU.mult, [t_tsn], [t_tsn])
                        TT("dve", c_, s_, s_, ALU.mult, [t_tsn], [t_tsn])
                        TS("dve", c_, c_, -2.0, 1.0, ALU.mult, ALU.add, [t_tsn], [t_tsn])
                        if rep < 2:
                            CP("dve", s_, q_, [t_tsn], [t_tsn])
                    CP("dve", dst[0:64, c0:c0 + cw], q_, [t_tsn], [t_dst])
            HS = A.alloc([nt, 512], BF16); HD = A.alloc([nt, 512], BF16); t_HS = Trk()
            dcy = [A.alloc([1024]) for _ in range(2)]; t_dcy = [Trk(), Trk()]
            Hd = A.alloc([1024]); t_Hd = Trk()
            for jc in range(nt):
                bi = jc % 2
                DMA("sp", dcy[bi], C["dec" + tag][jc * 128:(jc + 1) * 128, :], [], [t_dcy[bi]])
                for hh in range(2):
                    pb = 4 + hh
                    MM(PS[pb][:, :], h2[0:64, jc * 128:(jc + 1) * 128], w3[0:64, hh * 512:(hh + 1) * 512], True, True,
                       [t_h2, t_fw], [PT[pb]])
                    TT("dve", Hd[:, hh * 512:(hh + 1) * 512], PS[pb][:, :], dcy[bi][:, hh * 512:(hh + 1) * 512], ALU.mult,
                       [PT[pb], t_dcy[bi]], [t_Hd])
                if jc == 0:
                    TT("dve", Hd, Hd, biasz, ALU.add, [t_Hd, t_bz], [t_Hd])
                Hv = Hd.rearrange("p (o d c) -> p o d c", o=2, d=2)
                TT("dve", HS[:, jc, :].rearrange("p (o c) -> p o c", o=2), Hv[:, :, 0, :], Hv[:, :, 1, :], ALU.add, [t_Hd], [t_HS])
                TT("dve", HD[:, jc, :].rearrange("p (o c) -> p o c", o=2), Hv[:, :, 1, :], Hv[:, :, 0, :], ALU.subtract, [t_Hd], [t_HS])
            FB = [A.alloc([2, nt, 128], BF16) for _ in range(2)]; t_FB = [Trk(), Trk()]
            KKs = [A.alloc([2, 512], BF16) for _ in range(2)]; t_KK = [Trk(), Trk()]
            for kc in range(nt):
                bi = kc % 2
                DMA("sp", FB[bi], C["hyf" + tag][kc], [], [t_FB[bi]])
                for ri, Hx in enumerate((HS, HD)):
                    pb = 6 + ri
                    for jc in range(nt):
                        MM(PS[pb][:, :], FB[bi][:, ri, jc, :], Hx[:, jc, :], jc == 0, jc == nt - 1, [t_FB[bi], t_HS], [PT[pb]])
                    CP("act" if ri == 0 else "dve", KKs[bi][:, ri, :], PS[pb][:, :], [PT[pb]], [t_KK[bi]])
                DMA("sp", kk_d[tag][l, kc].rearrange("p r o c -> p r (o c)"), KKs[bi], [t_KK[bi]], [])
            S.barrier()
            A.release(m1)
        wst = A.alloc([2, 64]); t_wst = Trk()
        DMA("sp", wst, W["fn_w"][l].rearrange("(gp g) m d -> (g m) gp d", g=2), [], [t_wst])
        bdc = A.alloc([128]); bds = A.alloc([128]); t_bdc = Trk()
        DMA("sp", bdc, C["bdc"], [], [t_bdc])
        DMA("sp", bds, C["bds"], [], [t_bdc])
        MSET("pool", bda, 0.0, [t_bd])
        MSET("pool", bdb, 0.0, [t_bd])
        for which, (mat, dst) in enumerate(((bdc, bda), (bds, bdb))):
            for gp in range(2):
                pb = 4 + gp
                MM(PS[pb][:, 0:64], mat, wst[:, gp, :], True, True, [t_bdc, t_wst], [PT[pb]])
                CP("dve", dst[0:64, gp, 0:64], PS[pb][0:64, 0:64], [PT[pb]], [t_bd])
                CP("dve", dst[64:128, gp, 64:128], PS[pb][64:128, 0:64], [PT[pb]], [t_bd])
        S.barrier()
        A.release(m0)

    def rms_rstd(ss_ap, n, R, out_ap, t_out):
        ACT(out_ap, ss_ap, AF.Sqrt, R, [t_out], scale=1.0 / n, bias=EPS)
        RCP(out_ap, out_ap, [t_out], [t_out])

    evt = A.alloc([KC, 128]); t_evt = Trk()

    def norm_mod_transpose(xt, t_xt, hT, t_hT, col0, gi, r, bufs, i):
        junk, t_junk, ssb, t_ss, xn, t_xn = bufs
        ACT(junk, xt, AF.Square, [t_xt], [t_junk, t_ss], accum=ssb[:, 0:1])
        rms_rstd(ssb[:, 0:1], D, [t_ss], ssb[:, 1:2], t_ss)
        TS("dve", xn, xt, ssb[:, 1:2], None, ALU.mult, None, [t_xt, t_ss], [t_xn])
        pb = i % 2
        pv = PS[pb][:, :].bitcast(BF16)
        for kc in range(KC):
            TRP(pv[:, kc * 128:(kc + 1) * 128], xn[:, kc * 128:(kc + 1) * 128], identb, [t_xn, t_identb], [PT[pb]])
        pv3 = pv.rearrange("p (k c) -> p k c", k=KC)
        TT("dve", evt, pv3, GS[:, gi, :, r:r + 1].to_broadcast([128, KC, 128]), ALU.mult, [PT[pb], t_GS], [t_evt])
        TT("dve", hT[:, :, col0:col0 + 128], evt, GS[:, gi + 1, :, r:r + 1].to_broadcast([128, KC, 128]), ALU.add,
           [t_evt, t_GS], [t_hT])

    def col_groups(c0, c1):
        g = []
        c = c0
        while c < c1:
            w = min(512, c1 - c)
            g.append((c, w))
            c += w
        return g

    def win_chunks(l, hT, t_hT, cols_list, evac, c0=0, c1=None):
        c1 = T if c1 is None else c1
        n = len(cols_list)
        wb = A.alloc([KC, n, 128], BF16); t_wb = Trk()
        for j, cc in enumerate(cols_list):
            DMA("pool", wb[:, :, j, :], W["w_in"][l, :, cc:cc + 128].rearrange("(kc p) n -> p kc n", p=128), [], [t_wb])
        for j in range(n):
            for (g0, gw) in col_groups(c0, c1):
                pb = rr["ps"] % 4
                rr["ps"] += 1
                for kc in range(KC):
                    MM(PS[pb][:, 0:gw], wb[:, kc, j, :], hT[:, kc, g0:g0 + gw], kc == 0, kc == KC - 1, [t_wb, t_hT], [PT[pb]])
                evac(j, g0, gw, PS[pb][:, 0:gw], PT[pb])

    def mixer_sublayer(l, b, hT, t_hT):
        last = (l == NL - 1)
        m0 = A.mark()
        xt = [A.alloc([D]) for _ in range(2)]; t_xt = [Trk(), Trk()]
        pt_ = [A.alloc([D]) for _ in range(2)]; t_pt = [Trk(), Trk()]
        junk = [A.alloc([D], BF16) for _ in range(2)]; t_junk = [Trk(), Trk()]
        ssb = [A.alloc([8]) for _ in range(2)]; t_ss = [Trk(), Trk()]
        xn = [A.alloc([D], BF16) for _ in range(2)]; t_xn = [Trk(), Trk()]
        for i in range(NT):
            bi = i % 2
            is_ctx = i < NTC
            if l == 0:
                if is_ctx:
                    DMA("sp", xt[bi], ctx_d[b, i * 128:(i + 1) * 128, :], [], [t_xt[bi]])
                else:
                    j = i - NTC
                    DMA("sp", xt[bi], x_d[b, j * 128:(j + 1) * 128, :], [], [t_xt[bi]])
                    DMA("sp", pt_[bi], C["pos"][j * 128:(j + 1) * 128, :], [], [t_pt[bi]])
                    TT("dve", xt[bi], xt[bi], pt_[bi], ALU.add, [t_xt[bi], t_pt[bi]], [t_xt[bi]])
                DMA("sp", xres[b, i * 128:(i + 1) * 128, :], xt[bi], [t_xt[bi]], [])
            else:
                DMA("sp", xt[bi], xres[b, i * 128:(i + 1) * 128, :], [], [t_xt[bi]])
            norm_mod_transpose(xt[bi], t_xt[bi], hT, t_hT, i * 128, 0, cond_row(b, is_ctx),
                               (junk[bi], t_junk[bi], ssb[bi], t_ss[bi], xn[bi], t_xn[bi]), i)
        S.barrier()
        A.release(m0)
        segs = [("l", NTC, NTL)] + ([("c", 0, NTC)] if not last else [])
        mix_c0 = 0 if not last else LC

        m0 = A.mark()
        utok = A.alloc([NT, 768], BF16); t_utok = Trk()
        mmid = A.mark()
        u = A.alloc([6, T], BF16); t_u = Trk()
        zhy = A.alloc([6, T], BF16); t_zhy = Trk()

        def ev_hy(j, g0, gw, ps, pt):
            CP(evac_eng(j), zhy[:, j, g0:g0 + gw], ps, [pt], [t_zhy])
        win_chunks(l, hT, t_hT, [j * 128 for j in range(6)], ev_hy, mix_c0, T)
        ut = A.alloc([T]); t_ut = Trk()
        for j in range(6):
            for tag, t0, nt in segs:
                a0, a1 = t0 * 128, (t0 + nt) * 128
                ACT(ut[:, a0:a1], zhy[:, j, a0:a1], AF.Identity, [t_zhy, t_vec], [t_ut],
                    scale=vecT[:, 16 + 6 + j:16 + 6 + j + 1], bias=vecT[:, 34 + j:35 + j])
                STT("dve", ut[:, a0 + 1:a1], zhy[:, j, a0:a1 - 1], vecT[:, 16 + j:17 + j], ut[:, a0 + 1:a1],
                    ALU.mult, ALU.add, [t_zhy, t_vec, t_ut], [t_ut])
                STT("dve", u[:, j, a0:a1 - 1], zhy[:, j, a0 + 1:a1], vecT[:, 16 + 12 + j:16 + 13 + j], ut[:, a0:a1 - 1],
                    ALU.mult, ALU.add, [t_zhy, t_vec, t_ut], [t_u])
                CP("pool", u[:, j, a1 - 1:a1], ut[:, a1 - 1:a1], [t_ut], [t_u])
        for i in range(mix_c0 // 128, NT):
            pb = i % 2
            pv = PS[pb][:, :].bitcast(BF16)
            for j in range(6):
                TRP(pv[:, j * 128:(j + 1) * 128], u[:, j, i * 128:(i + 1) * 128], identb, [t_u, t_identb], [PT[pb]])
            CP(evac_eng(i), utok[:, i, :], pv[:, 0:768], [PT[pb]], [t_utok])
        S.barrier()
        A.release(mmid)
        ynT = A.alloc([2, T], BF16); t_ynT = Trk()
        for tag, t0, nt in segs:
            m1 = A.mark()
            n = nt * 128
            KK = A.alloc([nt, 2, 256], BF16); t_KKl = Trk()
            FB = [A.alloc([2, nt, 128], BF16) for _ in range(2)]; t_FB = [Trk(), Trk()]
            Pr = A.alloc([nt, 256], BF16); Pi = A.alloc([nt, 256], BF16); t_P = Trk()
            y1 = A.alloc([nt, 256], BF16); t_y1 = Trk()
            tmp = [A.alloc([4, 256]) for _ in range(2)]; t_tmp = [Trk(), Trk()]
            y2 = A.alloc([256]); t_y2 = Trk()
            yn = A.alloc([256], BF16); t_yn = Trk()
            junk2 = A.alloc([256], BF16); t_j2 = Trk()
            ss2 = A.alloc([8]); t_ss2 = Trk()
            for order in range(2):
                for r_ in range(2):
                    DMA("sp", KK[:, :, r_, :], kk_d[tag][l, :, :, r_, order, :].rearrange("k p c -> p k c"), [], [t_KKl])

                def src(tc):
                    if order == 0:
                        return utok[:, t0 + tc, 0:256], t_utok
                    return y1[:, tc, :], t_y1
                for kc in range(nt):
                    bi = kc % 2
                    DMA("sp", FB[bi], C["hyf" + tag][kc], [], [t_FB[bi]])
                    pr_, pi_ = 4 + 2 * bi, 5 + 2 * bi
                    for ri, pb in ((0, pr_), (1, pi_)):
                        for tc in range(nt):
                            s_ap, s_t = src(tc)
                            MM(PS[pb][:, 0:256], FB[bi][:, ri, tc, :], s_ap, tc == 0, tc == nt - 1, [t_FB[bi], s_t], [PT[pb]])
                    tb = tmp[bi]
                    Kr = KK[:, kc, 0, :]
                    Ki = KK[:, kc, 1, :]
                    TT("dve", tb[:, 0, :], PS[pr_][:, 0:256], Kr, ALU.mult, [PT[pr_], t_KKl], [t_tmp[bi]])
                    TT("dve", tb[:, 1, :], PS[pi_][:, 0:256], Ki, ALU.mult, [PT[pi_], t_KKl], [t_tmp[bi]])
                    TT("dve", tb[:, 2, :], PS[pr_][:, 0:256], Ki, ALU.mult, [PT[pr_], t_KKl], [t_tmp[bi]])
                    TT("dve", tb[:, 3, :], PS[pi_][:, 0:256], Kr, ALU.mult, [PT[pi_], t_KKl], [t_tmp[bi]])
                    TT("pool", Pr[:, kc, :], tb[:, 0, :], tb[:, 1, :], ALU.add, [t_tmp[bi]], [t_P])
                    TT("pool", Pi[:, kc, :], tb[:, 2, :], tb[:, 3, :], ALU.subtract, [t_tmp[bi]], [t_P])
                for tc in range(nt):
                    bi = tc % 2
                    DMA("sp", FB[bi], C["hyi" + tag][tc], [], [t_FB[bi]])
                    pb = 4 + bi
                    for kc in range(nt):
                        MM(PS[pb][:, 0:256], FB[bi][:, 0, kc, :], Pr[:, kc, :], kc == 0, False, [t_FB[bi], t_P], [PT[pb]])
                        MM(PS[pb][:, 0:256], FB[bi][:, 1, kc, :], Pi[:, kc, :], False, kc == nt - 1, [t_FB[bi], t_P], [PT[pb]])
                    if order == 0:
                        TT("dve", y1[:, tc, :], PS[pb][:, 0:256], utok[:, t0 + tc, 256:512], ALU.mult, [PT[pb], t_utok], [t_y1])
                    else:
                        TT("dve", y2, PS[pb][:, 0:256], utok[:, t0 + tc, 512:768], ALU.mult, [PT[pb], t_utok], [t_y2])
                        ACT(junk2, y2, AF.Square, [t_y2], [t_j2, t_ss2], accum=ss2[:, 0:1])
                        rms_rstd(ss2[:, 0:1], 256, [t_ss2], ss2[:, 1:2], t_ss2)
                        TS("dve", yn, y2, ss2[:, 1:2], None, ALU.mult, None, [t_y2, t_ss2], [t_yn])
                        pq = 6 + bi
                        pv = PS[pq][:, :].bitcast(BF16)
                        for j in range(2):
                            TRP(pv[:, j * 128:(j + 1) * 128], yn[:, j * 128:(j + 1) * 128], identb, [t_yn, t_identb], [PT[pq]])
                        col = (t0 + tc) * 128
                        for j in range(2):
                            TS("dve", ynT[:, j, col:col + 128], pv[:, j * 128:(j + 1) * 128], vecT[:, 92 + j:93 + j], None,
                               ALU.mult, None, [PT[pq], t_vec], [t_ynT])
            S.barrier()
            A.release(m1)
        DMA("sp", yT_d[:, 0:2, mix_c0:T], ynT[:, :, mix_c0:T], [t_ynT], [])
        S.barrier()
        A.release(m0)

        m0 = A.mark()
        zfn = A.alloc([2, T], BF16); t_zfn = Trk()

        def ev_fn(j, g0, gw, ps, pt):
            CP(evac_eng(j), zfn[:, j, g0:g0 + gw], ps, [pt], [t_zfn])
        win_chunks(l, hT, t_hT, [3328, 3456], ev_fn, mix_c0, T)
        zc = A.alloc([NT, 256], BF16); zs = A.alloc([NT, 256], BF16); t_zcs = Trk()
        for i in range(mix_c0 // 128, NT):
            pa, pb2 = 4 + 2 * (i % 2), 5 + 2 * (i % 2)
            for j in range(2):
                MM(PS[pa][:, j * 128:(j + 1) * 128], zfn[:, j, i * 128:(i + 1) * 128], bda[:, j, :], True, True, [t_zfn, t_bd], [PT[pa]])
                MM(PS[pb2][:, j * 128:(j + 1) * 128], zfn[:, j, i * 128:(i + 1) * 128], bdb[:, j, :], True, True, [t_zfn, t_bd], [PT[pb2]])
            CP("act", zc[:, i, :], PS[pa][:, 0:256], [PT[pa]], [t_zcs])
            CP("dve", zs[:, i, :], PS[pb2][:, 0:256], [PT[pb2]], [t_zcs])
        fnb = A.alloc([256]); t_fnb = Trk()
        DMA("sp", fnb, W["fn_b"][l].partition_broadcast(128), [], [t_fnb])
        ynT = A.alloc([2, T], BF16); t_ynT = Trk()
        y2 = A.alloc([256]); t_y2 = Trk()
        yn = A.alloc([256], BF16); t_yn = Trk()
        junk2 = A.alloc([256], BF16); t_j2 = Trk()
        ss2 = A.alloc([8]); t_ss2 = Trk()
        for tag, t0, nt in segs:
            m1 = A.mark()
            FB = [A.alloc([2, nt, 128], BF16) for _ in range(2)]; t_FB = [Trk(), Trk()]
            for kc in range(nt):
                bi = kc % 2
                DMA("sp", FB[bi], C["fnf" + tag][kc], [], [t_FB[bi]])
                pb = 4 + bi
                for tc in range(nt):
                    MM(PS[pb][:, 0:256], FB[bi][:, 0, tc, :], zc[:, t0 + tc, :], tc == 0, False, [t_FB[bi], t_zcs], [PT[pb]])
                    MM(PS[pb][:, 0:256], FB[bi][:, 1, tc, :], zs[:, t0 + tc, :], False, tc == nt - 1, [t_FB[bi], t_zcs], [PT[pb]])
                TT("dve", y2, PS[pb][:, 0:256], fnb, ALU.add, [PT[pb], t_fnb], [t_y2])
                ACT(junk2, y2, AF.Square, [t_y2], [t_j2, t_ss2], accum=ss2[:, 0:1])
                rms_rstd(ss2[:, 0:1], 256, [t_ss2], ss2[:, 1:2], t_ss2)
                TS("dve", yn, y2, ss2[:, 1:2], None, ALU.mult, None, [t_y2, t_ss2], [t_yn])
                pq = 6 + bi
                pv = PS[pq][:, :].bitcast(BF16)
                for j in range(2):
                    TRP(pv[:, j * 128:(j + 1) * 128], yn[:, j * 128:(j + 1) * 128], identb, [t_yn, t_identb], [PT[pq]])
                col = (t0 + kc) * 128
                for j in range(2):
                    TS("dve", ynT[:, j, col:col + 128], pv[:, j * 128:(j + 1) * 128], vecT[:, 94 + j:95 + j], None,
                       ALU.mult, None, [PT[pq], t_vec], [t_ynT])
            S.barrier()
            A.release(m1)
        DMA("sp", yT_d[:, 6:8, mix_c0:T], ynT[:, :, mix_c0:T], [t_ynT], [])
        S.barrier()
        A.release(m0)

        out_t0 = 0 if not last else NTC
        for h in range(4):
            m0 = A.mark()
            qs = A.alloc([T], BF16); sg = A.alloc([T], BF16); vT = A.alloc([T], BF16)
            t_q = Trk(); t_sg = Trk(); t_vT = Trk()

            def ev_hg(j, g0, gw, ps, pt):
                if j == 0:
                    ACT(qs[:, g0:g0 + gw], ps, AF.Silu, [pt], [t_q])
                elif j == 1:
                    CP("dve", vT[:, g0:g0 + gw], ps, [pt], [t_vT])
                else:
                    ACT(sg[:, g0:g0 + gw], ps, AF.Silu, [pt], [t_sg])
            m1 = A.mark()
            win_chunks(l, hT, t_hT, [768 + 128 * h, 2304 + 128 * h, 2816 + 128 * h], ev_hg)
            S.barrier()
            A.release(m1)
            vtok = A.alloc([NT, 128], BF16); t_vtok = Trk()
            for i in range(NT):
                pb = i % 2
                pv = PS[pb][:, :].bitcast(BF16)
                TRP(pv[:, 0:128], vT[:, i * 128:(i + 1) * 128], identb, [t_vT, t_identb], [PT[pb]])
                CP(evac_eng(i), vtok[:, i, :], pv[:, 0:128], [PT[pb]], [t_vtok])
            sig = A.alloc([T]); t_sig = Trk()
            kkb = A.alloc([T]); bb = A.alloc([T]); xb_ = A.alloc([T])
            t_kk = Trk(); t_bb = Trk(); t_xb = Trk()
            qd = A.alloc([T], BF16); ke = A.alloc([T], BF16); qr = A.alloc([T], BF16)
            KI = [A.alloc([T], BF16) for _ in range(4)]
            Rblk = A.alloc([T // 8]); t_R = Trk()
            t_qd = Trk(); t_kd = Trk(); t_ke = Trk(); t_qr = Trk()
            ebend = A.alloc([NCH]); t_eb = Trk()
            Sall = A.alloc([NCH, 128], BF16); t_Sall = Trk()
            Sm = A.alloc([128]); t_Sm = Trk()
            oacc = A.alloc([NT, 128]); t_oacc = Trk(); t_oaccs = [Trk() for _ in range(NT)]
            ATa = A.alloc([NT, 128], BF16); t_ATa = Trk()
            kzT = [A.alloc([4, 128], BF16) for _ in range(3)]; t_kzT = [Trk(), Trk(), Trk()]
            ona = A.alloc([NT, 128], BF16); t_on = Trk()
            ssa = A.alloc([NT]);
            junk3 = A.alloc([128], BF16); t_j3 = Trk()
            ss3 = A.alloc([8]); t_ss3 = Trk()
            yhT = A.alloc([T], BF16); t_yhT = Trk()
            for d in range(2):
                li = d * 4 + h
                m1 = A.mark()

                def ev_f(j, g0, gw, ps, pt):
                    ACT(sig[:, g0:g0 + gw], ps, AF.Sigmoid, [pt], [t_sig])
                win_chunks(l, hT, t_hT, [1280 + 512 * d + 128 * h], ev_f)
                S.barrier()
                A.release(m1)
                TS("dve", kkb, sig, lbT[:, 2, li:li + 1], lbT[:, 1, li:li + 1], ALU.mult, ALU.add, [t_sig, t_lb], [t_kk])
                ACT(sig, sig, AF.Ln, [t_sig, t_lb], [t_sig], scale=lbT[:, 1, li:li + 1], bias=lbT[:, 0, li:li + 1])
                SCAN(bb, scanm, sig, [t_scanm, t_sig], [t_bb])
                b3 = bb.rearrange("p (c j) -> p c j", j=32)
                x3 = xb_.rearrange("p (c j) -> p c j", j=32)
                k3 = kkb.rearrange("p (c j) -> p c j", j=32)
                b8 = bb.rearrange("p (c j) -> p c j", j=8)
                l8 = sig.rearrange("p (c j) -> p c j", j=8)
                x8 = xb_.rearrange("p (c j) -> p c j", j=8)
                NB8 = T // 8
                if d == 1:
                    TT("dve", xb_, sig, bb, ALU.subtract, [t_sig, t_bb], [t_xb])
                    CP("dve", ebend, b3[:, :, 31], [t_bb], [t_eb])
                    TT("dve", b3, x3, ebend.unsqueeze(2).to_broadcast([128, NCH, 32]), ALU.add, [t_xb, t_eb], [t_bb])
                    endc = 0
                    rc = 7
                else:
                    endc = 31
                    rc = 0
                TT("dve", Rblk, b8[:, :, rc], l8[:, :, rc], ALU.subtract, [t_bb, t_sig], [t_R])
                TT("dve", x8, b8, Rblk.unsqueeze(2).to_broadcast([128, NB8, 8]), ALU.subtract, [t_bb, t_R], [t_xb])
                ACT(xb_, xb_, AF.Exp, [t_xb], [t_xb])
                STT("dve", qr, xb_, float(128 ** -0.5), qs, ALU.mult, ALU.mult, [t_xb, t_q], [t_qr])
                R4 = Rblk.rearrange("p (c i) -> p c i", i=4)
                for I in range(4):
                    lo, hi = (0, 8 * (I + 1)) if d == 0 else (8 * I, 32)
                    w_ = hi - lo
                    MSET("pool", KI[I], 0.0, [t_kd])
                    TT("dve", x3[:, :, lo:hi], R4[:, :, I:I + 1].to_broadcast([128, NCH, w_]), b3[:, :, lo:hi], ALU.subtract,
                       [t_R, t_bb], [t_xb])
                    ACT(x3[:, :, lo:hi], x3[:, :, lo:hi], AF.Exp, [t_xb], [t_xb])
                    TT("dve", KI[I].rearrange("p (c j) -> p c j", j=32)[:, :, lo:hi], x3[:, :, lo:hi], k3[:, :, lo:hi], ALU.mult,
                       [t_xb, t_kk], [t_kd])
                TT("dve", x3, b3[:, :, endc:endc + 1].to_broadcast([128, NCH, 32]), b3, ALU.subtract, [t_bb], [t_xb])
                ACT(xb_, xb_, AF.Exp, [t_xb], [t_xb])
                TT("dve", ke, xb_, kkb, ALU.mult, [t_xb, t_kk], [t_ke])
                ACT(ebend, b3[:, :, endc], AF.Exp, [t_bb], [t_eb])
                ACT(bb, bb, AF.Exp, [t_bb], [t_bb])
                STT("dve", qd, bb, float(128 ** -0.5), qs, ALU.mult, ALU.mult, [t_bb, t_q], [t_qd])
                if d == 0:
                    order = list(range(NCH))
                else:
                    order = list(range(NCHC - 1, -1, -1)) + list(range(NCH - 1, NCHC - 1, -1))
                MSET("pool", Sm, 0.0, [t_Sm])
                tiles_in_order = []
                for cch in order:
                    if cch // 4 not in tiles_in_order:
                        tiles_in_order.append(cch // 4)
                for n_, i in enumerate(tiles_in_order):
                    bi = n_ % 3
                    pk = 4 + n_ % 2
                    pkv = 6 + n_ % 2
                    pv = PS[pk][:, :].bitcast(BF16)
                    TRP(pv[:, 0:128], ke[:, i * 128:(i + 1) * 128], identb, [t_ke, t_identb], [PT[pk]])
                    for j in range(4):
                        ACT(kzT[bi][:, j, :], pv[:, 0:128], AF.Identity, [PT[pk], t_mask], [t_kzT[bi]], scale=rowm[:, j:j + 1])
                    for j in range(4):
                        MM(PS[pkv][:, j * 128:(j + 1) * 128], kzT[bi][:, j, :], vtok[:, i, :], True, True,
                           [t_kzT[bi], t_vtok], [PT[pkv]])
                    chs = [cch for cch in order if cch // 4 == i]
                    for cch in chs:
                        j = cch % 4
                        CP("dve", Sall[:, cch, :], Sm, [t_Sm], [t_Sall])
                        STT("dve", Sm, Sm, ebend[:, cch:cch + 1], PS[pkv][:, j * 128:(j + 1) * 128], ALU.mult, ALU.add,
                            [t_Sm, t_eb, PT[pkv]], [t_Sm])
                msk = maskf if d == 0 else maskb
                for i in range(out_t0, NT):
                    pa = i % 4
                    cs = slice(i * 128, (i + 1) * 128)
                    for I in range(4):
                        MM(PS[pa][:, I * 32:(I + 1) * 32], KI[I][:, cs],
                           qr[:, cs].rearrange("p (c i j) -> p c i j", c=4, i=4)[:, :, I, :], True, True, [t_kd, t_qr], [PT[pa]])
                    TT("dve", ATa[:, i, :].rearrange("p (c i j) -> p i c j", c=4, i=4),
                       PS[pa][:, 0:128].rearrange("p (i c j) -> p i c j", i=4, c=4),
                       msk.rearrange("p (c i j) -> p i c j", c=4, i=4), ALU.mult, [PT[pa], t_mask], [t_ATa])
                tl = list(range(out_t0, NT))
                for g0 in range(0, len(tl), 4):
                    grp = tl[g0:g0 + 4]
                    for gi, i in enumerate(grp):
                        cs = slice(i * 128, (i + 1) * 128)
                        MM(PS[gi][:, 128:256], ATa[:, i, :], vtok[:, i, :], True, True, [t_ATa, t_vtok], [PT[gi]])
                        MM(PS[4 + gi][:, :], qd[:, cs], Sall[:, 4 * i:4 * i + 4, :].rearrange("p j c -> p (j c)"), True, True,
                           [t_qd, t_Sall], [PT[4 + gi]])
                    for gi, i in enumerate(grp):
                        dst = oacc[:, i, :]
                        if d == 0:
                            CP("act", dst, PS[gi][:, 128:256], [PT[gi]], [t_oaccs[i]])
                        else:
                            TT("dve", dst, PS[gi][:, 128:256], dst, ALU.add, [PT[gi], t_oaccs[i]], [t_oaccs[i]])
                    for j in range(4):
                        for gi, i in enumerate(grp):
                            dst = oacc[:, i, :]
                            STT("dve", dst, PS[4 + gi][:, j * 128:(j + 1) * 128], rowm[:, j:j + 1], dst, ALU.mult, ALU.add,
                                [PT[4 + gi], t_mask, t_oaccs[i]], [t_oaccs[i]])
                if d == 1:
                    for i in range(out_t0, NT):
                        ACT(junk3, oacc[:, i, :], AF.Square, [t_oaccs[i]], [t_j3, t_ss3], accum=ssa[:, i:i + 1])
                    rms_rstd(ssa[:, out_t0:NT], 128, [t_ss3], ssa[:, out_t0:NT], t_ss3)
                    for i in range(out_t0, NT):
                        ACT(ona[:, i, :], oacc[:, i, :], AF.Identity, [t_oaccs[i], t_ss3], [t_on], scale=ssa[:, i:i + 1])
                    for i in range(out_t0, NT):
                        pq = 4 + i % 4
                        cs = slice(i * 128, (i + 1) * 128)
                        pv = PS[pq][:, :].bitcast(BF16)
                        TRP(pv[:, 0:128], ona[:, i, :], identb, [t_on, t_identb], [PT[pq]])
                        STT("dve", yhT[:, cs], pv[:, 0:128], vecT[:, 40 + h:41 + h], sg[:, cs], ALU.mult, ALU.mult,
                            [PT[pq], t_vec, t_sg], [t_yhT])
            DMA("sp", yT_d[:, 2 + h, mix_c0:T], yhT[:, mix_c0:T], [t_yhT], [])
            S.barrier()
            A.release(m0)

    def ffn_sublayer(l, b, hT, t_hT):
        last = (l == NL - 1)
        tlo = 0 if not last else NTC
        m0 = A.mark()
        ysb = A.alloc([8, T], BF16); t_ysb = Trk()
        DMA("sp", ysb[:, :, tlo * 128:T], yT_d[:, :, tlo * 128:T], [], [t_ysb])
        wo = A.alloc([KC, D], BF16); t_wo = Trk()
        DMA("pool", wo, W["w_out"][l].rearrange("(kc p) n -> p kc n", p=128), [], [t_wo])
        gt = {}
        t_gt = Trk()
        for r in ([b] if last else [b, 2]):
            gt[r] = A.alloc([D])
            DMA("sp", gt[r], gate_d[l, r, 0].partition_broadcast(128), [], [t_gt])
        xt = [A.alloc([D]) for _ in range(2)]; t_xt = [Trk(), Trk()]
        t1 = A.alloc([D]); t_t1 = Trk()
        junk = [A.alloc([D], BF16) for _ in range(2)]; t_junk = [Trk(), Trk()]
        ssb = [A.alloc([8]) for _ in range(2)]; t_ss = [Trk(), Trk()]
        xn = [A.alloc([D], BF16) for _ in range(2)]; t_xn = [Trk(), Trk()]
        for i in range(tlo, NT):
            bi = i % 2
            r = cond_row(b, i < NTC)
            DMA("sp", xt[bi], xres[b, i * 128:(i + 1) * 128, :], [], [t_xt[bi]])
            for hh in range(2):
                pb = 2 + hh
                for kc in range(KC):
                    MM(PS[pb][:, :], ysb[:, kc, i * 128:(i + 1) * 128], wo[:, kc, hh * 512:(hh + 1) * 512], kc == 0, kc == KC - 1,
                       [t_ysb, t_wo], [PT[pb]])
                TT("dve", t1[:, hh * 512:(hh + 1) * 512], PS[pb][:, :], gt[r][:, hh * 512:(hh + 1) * 512], ALU.mult, [PT[pb], t_gt], [t_t1])
            TT("dve", xt[bi], xt[bi], t1, ALU.add, [t_xt[bi], t_t1], [t_xt[bi]])
            DMA("sp", xres[b, i * 128:(i + 1) * 128, :], xt[bi], [t_xt[bi]], [])
            norm_mod_transpose(xt[bi], t_xt[bi], hT, t_hT, i * 128, 2, r,
                               (junk[bi], t_junk[bi], ssb[bi], t_ss[bi], xn[bi], t_xn[bi]), i)
        S.barrier()
        A.release(m0)
        ntiles = NT - tlo
        halves = [(tlo, tlo + (ntiles + 1) // 2), (tlo + (ntiles + 1) // 2, NT)] if not HALF_EXP else [(tlo, tlo + 1), (tlo + 1, NT)]
        for (ta, tb) in halves:
            if tb <= ta:
                continue
            m0 = A.mark()
            nth = tb - ta
            comb = A.alloc([nth, NEXP]); t_comb = Trk()
            rt = A.alloc([12, NEXP]); t_rt = Trk()
            sc = A.alloc([16]); t_sc = Trk()
            for ii in range(nth):
                i = ta + ii
                pb = ii % 2
                for kc in range(KC):
                    MM(PS[pb][:, 0:NEXP], hT[:, kc, i * 128:(i + 1) * 128], rw_sb[:, kc, :], kc == 0, kc == KC - 1, [t_hT, t_rw], [PT[pb]])
                lg = rt[:, 0, :]
                CP("dve", lg, PS[pb][:, 0:NEXP], [PT[pb]], [t_rt])
                RED(sc[:, 0:1], lg, ALU.max, [t_rt], [t_sc])
                TS("dve", sc[:, 1:2], sc[:, 0:1], -1.0, None, ALU.mult, None, [t_sc], [t_sc])
                ACT(rt[:, 1, :], lg, AF.Exp, [t_rt, t_sc], [t_rt, t_sc], bias=sc[:, 1:2], accum=sc[:, 2:3])
                RCP(sc[:, 3:4], sc[:, 2:3], [t_sc], [t_sc])
                TS("dve", rt[:, 2, :], rt[:, 1, :], sc[:, 3:4], None, ALU.mult, None, [t_rt, t_sc], [t_rt])
                TT("dve", rt[:, 3, :], rt[:, 2, :], rb_sb, ALU.add, [t_rt, t_rb], [t_rt])
                sel3 = rt[:, 3, :].rearrange("p (g k) -> p g k", k=4)
                RED(sc[:, 4:8], sel3, ALU.max, [t_rt], [t_sc])
                TT("dve", rt[:, 4, :].rearrange("p (g k) -> p g k", k=4), sel3, sc[:, 4:8].unsqueeze(2).to_broadcast([128, 4, 4]),
                   ALU.is_equal, [t_rt, t_sc], [t_rt])
                STT("dve", rt[:, 5, :], rt[:, 4, :], -1e9, rt[:, 3, :], ALU.mult, ALU.add, [t_rt], [t_rt])
                sel23 = rt[:, 5, :].rearrange("p (g k) -> p g k", k=4)
                RED(sc[:, 8:12], sel23, ALU.max, [t_rt], [t_sc])
                TT("dve", sc[:, 12:16], sc[:, 4:8], sc[:, 8:12], ALU.add, [t_sc], [t_sc])
                RED(sc[:, 0:1], sc[:, 12:16], ALU.max, [t_sc], [t_sc])
                TS("dve", sc[:, 12:16], sc[:, 12:16], sc[:, 0:1], None, ALU.is_equal, None, [t_sc], [t_sc])
                TT("dve", rt[:, 6, :].rearrange("p (g k) -> p g k", k=4), sel3, sc[:, 8:12].unsqueeze(2).to_broadcast([128, 4, 4]),
                   ALU.is_ge, [t_rt, t_sc], [t_rt])
                TT("dve", rt[:, 6, :].rearrange("p (g k) -> p g k", k=4), rt[:, 6, :].rearrange("p (g k) -> p g k", k=4),
                   sc[:, 12:16].unsqueeze(2).to_broadcast([128, 4, 4]), ALU.mult, [t_rt, t_sc], [t_rt])
                TT("dve", rt[:, 7, :], rt[:, 6, :], rt[:, 2, :], ALU.mult, [t_rt], [t_rt])
                RED(sc[:, 1:2], rt[:, 7, :], ALU.add, [t_rt], [t_sc])
                RCP(sc[:, 2:3], sc[:, 1:2], [t_sc], [t_sc])
                TS("dve", comb[:, ii, :], rt[:, 7, :], sc[:, 2:3], None, ALU.mult, None, [t_rt, t_sc], [t_comb])
                if DEBUG and ta == tlo and l == 0 and ii == 0 and b == 0:
                    DMA("sp", dbg_acc[:, 8, 0:192], rt.rearrange("p a b -> p (a b)"), [t_rt], [])
                    DMA("sp", dbg_acc[:, 9, 0:16], sc, [t_sc], [])
            acc = A.alloc([nth, D]); t_acc = Trk()
            wge = [A.alloc([KC, DEXP], BF16) for _ in range(2)]
            wue = [A.alloc([KC, DEXP], BF16) for _ in range(2)]
            wde = [A.alloc([4, D], BF16) for _ in range(2)]
            t_we = [Trk(), Trk()]
            At = [A.alloc([4, 512], BF16) for _ in range(2)]; t_At = [Trk(), Trk()]
            sl = [A.alloc([512]) for _ in range(2)]; t_sl = [Trk(), Trk()]
            cnt = 0
            for e_ in range(NEXP):
                bi = e_ % 2
                DMA("pool", wge[bi], W["moe_w_gate"][l, e_].rearrange("(kc p) n -> p kc n", p=128), [], [t_we[bi]])
                DMA("pool", wue[bi], W["moe_w_up"][l, e_].rearrange("(kc p) n -> p kc n", p=128), [], [t_we[bi]])
                DMA("pool", wde[bi], W["moe_w_down"][l, e_].rearrange("(kc p) n -> p kc n", p=128), [], [t_we[bi]])
                for (g0, gw) in col_groups(ta * 128, tb * 128):
                    ab = cnt % 2
                    cnt += 1
                    for oc in range(4):
                        pg, pu = oc % 2, 2 + oc % 2
                        for kc in range(KC):
                            MM(PS[pg][:, 0:gw], wge[bi][:, kc, oc * 128:(oc + 1) * 128], hT[:, kc, g0:g0 + gw], kc == 0, kc == KC - 1,
                               [t_we[bi], t_hT], [PT[pg]])
                        for kc in range(KC):
                            MM(PS[pu][:, 0:gw], wue[bi][:, kc, oc * 128:(oc + 1) * 128], hT[:, kc, g0:g0 + gw], kc == 0, kc == KC - 1,
                               [t_we[bi], t_hT], [PT[pu]])
                        sb_ = oc % 2
                        ACT(sl[sb_][:, 0:gw], PS[pg][:, 0:gw], AF.Silu, [PT[pg]], [t_sl[sb_]])
                        TT("dve", At[ab][:, oc, 0:gw], PS[pu][:, 0:gw], sl[sb_][:, 0:gw], ALU.mult, [PT[pu], t_sl[sb_]], [t_At[ab]])
                    for tt_ in range(gw // 128):
                        ti = (g0 // 128 - ta) + tt_
                        for hh in range(2):
                            py = 4 + (2 * tt_ + hh) % 4
                            for kc in range(4):
                                MM(PS[py][:, :], At[ab][:, kc, tt_ * 128:(tt_ + 1) * 128], wde[bi][:, kc, hh * 512:(hh + 1) * 512],
                                   kc == 0, kc == 3, [t_At[ab], t_we[bi]], [PT[py]])
                            dst = acc[:, ti, hh * 512:(hh + 1) * 512]
                            if e_ == 0:
                                TS("dve", dst, PS[py][:, :], comb[:, ti, e_:e_ + 1], None, ALU.mult, None, [PT[py], t_comb], [t_acc])
                            else:
                                STT("dve", dst, PS[py][:, :], comb[:, ti, e_:e_ + 1], dst, ALU.mult, ALU.add, [PT[py], t_comb, t_acc], [t_acc])
            if DEBUG and ta == tlo and l == 0:
                DMA("sp", dbg_acc[:, 0:nth, :], acc, [t_acc], [])
                DMA("sp", dbg_comb[:, 0:nth, :], comb, [t_comb], [])
                DMA("sp", dbg_h, hT[:, :, 0:256], [t_hT], [])
            g5 = {}
            t_g5 = Trk()
            for r in ([b] if last else [b, 2]):
                g5[r] = A.alloc([D])
                DMA("sp", g5[r], gate_d[l, r, 1].partition_broadcast(128), [], [t_g5])
            xt = [A.alloc([D]) for _ in range(2)]; t_xt = [Trk(), Trk()]
            junk = A.alloc([D], BF16); t_junk = Trk()
            ssb = A.alloc([8]); t_ss = Trk()
            for ii in range(nth):
                i = ta + ii
                bi = ii % 2
                r = cond_row(b, i < NTC)
                DMA("sp", xt[bi], xres[b, i * 128:(i + 1) * 128, :], [], [t_xt[bi]])
                TT("dve", acc[:, ii, :], acc[:, ii, :], g5[r], ALU.mult, [t_acc, t_g5], [t_acc])
                TT("dve", xt[bi], xt[bi], acc[:, ii, :], ALU.add, [t_xt[bi], t_acc], [t_xt[bi]])
                if not last:
                    DMA("sp", xres[b, i * 128:(i + 1) * 128, :], xt[bi], [t_xt[bi]], [])
                else:
                    ACT(junk, xt[bi], AF.Square, [t_xt[bi]], [t_junk, t_ss], accum=ssb[:, 0:1])
                    rms_rstd(ssb[:, 0:1], D, [t_ss], ssb[:, 1:2], t_ss)
                    STT("dve", xt[bi], xt[bi], ssb[:, 1:2], fng_sb, ALU.mult, ALU.mult, [t_xt[bi], t_ss, t_fng], [t_xt[bi]])
                    j = i - NTC
                    DMA("sp", out_d[b, j * 128:(j + 1) * 128, :], xt[bi], [t_xt[bi]], [])
            S.barrier()
            A.release(m0)

    for l in range(NL):
        if STOP_AFTER is not None and l > STOP_AFTER:
            break
        layer_setup(l)
        for b in range(NB):
            m0 = A.mark()
            hT = A.alloc([KC, T], BF16)
            t_hT = Trk()
            mixer_sublayer(l, b, hT, t_hT)
            ffn_sublayer(l, b, hT, t_hT)
            S.barrier()
            A.release(m0)
    stats = S.emit(es)
    es.close()
    return nc, hc, stats, A.peak


_CACHE = {}


def kernel(**inputs):
    NB, L, LC = 2, 2048, 256
    B = inputs["x"].shape[0]
    ncores = B // NB
    if "prog" not in _CACHE:
        _CACHE["prog"] = build(NB, L, LC)
    nc, hc, stats, peak = _CACHE["prog"]
    shared = {k: np.ascontiguousarray(np.asarray(inputs[k], dtype=np.float32)) for k in WEIGHT_SHAPES}
    shared["c_ctx"] = np.ascontiguousarray(np.asarray(inputs["c_ctx"], dtype=np.float32))
    for k, v in hc.items():
        shared["k_" + k] = v
    x = np.asarray(inputs["x"], dtype=np.float32)
    c = np.asarray(inputs["c"], dtype=np.float32)
    ctx = np.asarray(inputs["ctx"], dtype=np.float32)
    in_maps = []
    for i in range(ncores):
        m = dict(shared)
        m["x"] = np.ascontiguousarray(x[i * NB:(i + 1) * NB])
        m["c"] = np.ascontiguousarray(c[i * NB:(i + 1) * NB])
        m["ctx"] = np.ascontiguousarray(ctx[i * NB:(i + 1) * NB])
        in_maps.append(m)
    res = run_bass_kernel_spmd(nc, in_maps, core_ids=list(range(ncores)))
    return np.concatenate([r["out"] for r in res.results], axis=0).astype(np.float32)
```
